# Optimizing a Trainium2 kernel written in Bass

```python
import math
import jax, jax.numpy as jnp
from jax import lax
import numpy as np

D_MODEL = 1024
BATCH = 32
SEQ = 2048
DEPTH = 1

ATT_HEAD_DIM = 64
D_ATT = D_MODEL // 2
N_ATT_HEADS = D_ATT // ATT_HEAD_DIM
MOBA_BLOCK = 256
MOBA_TOPK = 3

SSM_HEAD_DIM = 64
D_SSM = 3 * D_MODEL // 2
N_SSM_HEADS = D_SSM // SSM_HEAD_DIM
N_SSM_GROUPS = 4
SSM_HEADS_PER_GROUP = N_SSM_HEADS // N_SSM_GROUPS
D_STATE = 128
SSM_CONV = 4
SSD_CHUNK = 128
D_XBC = D_SSM + 2 * N_SSM_GROUPS * D_STATE
D_MIX = D_ATT + D_SSM
D_IN_PROJ = 3 * D_ATT + D_SSM + D_XBC + N_SSM_HEADS

D_FF = ((8 * D_MODEL // 3 + 255) // 256) * 256
FFN_CONV = 3
EPS = 1e-6
NEG_INF = -1e30

kernel_name = 'moba_ssd_hybrid_block'


def rms_norm(x, g):
    xf = x.astype(jnp.float32)
    y = xf * lax.rsqrt(jnp.mean(xf * xf, axis=-1, keepdims=True) + EPS)
    return (y * g.astype(jnp.float32)).astype(x.dtype)


def causal_dwconv(x, w, b):
    k_width, ch = w.shape
    y = lax.conv_general_dilated(
        x, w[:, None, :].astype(x.dtype), window_strides=(1,),
        padding=[(k_width - 1, 0)], dimension_numbers=('NWC', 'WIO', 'NWC'),
        feature_group_count=ch)
    return y + b.astype(x.dtype)


def alibi_slopes(n_heads):
    return jnp.asarray(2.0 ** (-8.0 * np.arange(1, n_heads + 1) / n_heads), dtype=jnp.float32)


def moba_attention(q, k, v):
    H, S, DH = q.shape
    nb = S // MOBA_BLOCK
    n_extra = max(MOBA_TOPK - nb, 0)
    n_cand = nb + n_extra
    qb = q.reshape(H, nb, MOBA_BLOCK, DH)
    kb = k.reshape(H, nb, MOBA_BLOCK, DH)
    vb = v.reshape(H, nb, MOBA_BLOCK, DH)
    k_mean = jnp.mean(kb.astype(jnp.float32), axis=2)
    pad4 = ((0, 0), (0, n_extra), (0, 0), (0, 0))
    kb_c = jnp.pad(kb, pad4)
    vb_c = jnp.pad(vb, pad4)
    k_mean_c = jnp.pad(k_mean, ((0, 0), (0, n_extra), (0, 0)))
    slopes = alibi_slopes(H)
    scale = DH ** -0.5
    offs = jnp.arange(MOBA_BLOCK)
    own_dist = (offs[:, None] - offs[None, :]).astype(jnp.float32)
    own_causal = offs[:, None] >= offs[None, :]
    head_idx = jnp.arange(H)[:, None, None]
    cand = jnp.arange(n_cand)
    rank = jnp.arange(MOBA_TOPK)

    def query_block(i):
        qi = lax.dynamic_index_in_dim(qb, i, axis=1, keepdims=False)
        ki = lax.dynamic_index_in_dim(kb, i, axis=1, keepdims=False)
        vi = lax.dynamic_index_in_dim(vb, i, axis=1, keepdims=False)
        gate = jnp.einsum('hqd,hjd->hqj', qi.astype(jnp.float32), k_mean_c)
        gate = jnp.where(cand[None, None, :] < i, gate, NEG_INF)
        _, sel = lax.top_k(gate, MOBA_TOPK)
        kg = kb_c[head_idx, sel]
        vg = vb_c[head_idx, sel]
        s_own = jnp.einsum('hqd,hkd->hqk', qi, ki).astype(jnp.float32) * scale
        s_own = s_own - slopes[:, None, None] * own_dist
        s_own = jnp.where(own_causal[None], s_own, NEG_INF)
        q_pos = i * MOBA_BLOCK + offs
        k_pos = sel[..., None] * MOBA_BLOCK + offs
        dist = (q_pos[None, :, None, None] - k_pos).astype(jnp.float32)
        s_sel = jnp.einsum('hqd,hqrkd->hqrk', qi, kg).astype(jnp.float32) * scale
        s_sel = s_sel - slopes[:, None, None, None] * dist
        s_sel = jnp.where((rank < i)[None, None, :, None], s_sel, NEG_INF)
        scores = jnp.concatenate(
            [s_own, s_sel.reshape(H, MOBA_BLOCK, MOBA_TOPK * MOBA_BLOCK)], axis=-1)
        p = jax.nn.softmax(scores, axis=-1).astype(v.dtype)
        p_own = p[..., :MOBA_BLOCK]
        p_sel = p[..., MOBA_BLOCK:].reshape(H, MOBA_BLOCK, MOBA_TOPK, MOBA_BLOCK)
        return (jnp.einsum('hqk,hkd->hqd', p_own, vi)
                + jnp.einsum('hqrk,hqrkd->hqd', p_sel, vg))

    out = lax.map(query_block, jnp.arange(nb))
    return out.transpose(1, 0, 2, 3).reshape(H, S, DH)


def ssd_scan(xs, dt, a, bm, cm):
    Bsz, S, G, K, P = xs.shape
    N = bm.shape[-1]
    nc = S // SSD_CHUNK
    L = SSD_CHUNK
    x_dt = (xs.astype(jnp.float32) * dt[..., None]).reshape(Bsz, nc, L, G, K, P)
    bc = bm.astype(jnp.float32).reshape(Bsz, nc, L, G, N)
    cc = cm.astype(jnp.float32).reshape(Bsz, nc, L, G, N)
    a_dt = (dt * a).reshape(Bsz, nc, L, G, K).transpose(0, 1, 3, 4, 2)
    a_cs = jnp.cumsum(a_dt, axis=-1)
    causal = jnp.tril(jnp.ones((L, L), dtype=bool))
    seg = a_cs[..., :, None] - a_cs[..., None, :]
    decay = jnp.where(causal, jnp.exp(jnp.where(causal, seg, 0.0)), 0.0)
    cb = jnp.einsum('bclgn,bcsgn->bcgls', cc, bc)
    y_diag = jnp.einsum('bcgls,bcgkls,bcsgkp->bclgkp', cb, decay, x_dt)
    decay_states = jnp.exp(a_cs[..., -1:] - a_cs)
    states = jnp.einsum('bclgn,bcgkl,bclgkp->bcgkpn', bc, decay_states, x_dt)
    chunk_decay = jnp.exp(a_cs[..., -1])

    def step(h, inp):
        st, dec = inp
        return h * dec[..., None, None] + st, h

    h0 = jnp.zeros((Bsz, G, K, P, N), dtype=states.dtype)
    _, prev = lax.scan(step, h0, (jnp.moveaxis(states, 1, 0), jnp.moveaxis(chunk_decay, 1, 0)))
    prev = jnp.moveaxis(prev, 0, 1)
    y_off = jnp.einsum('bclgn,bcgkpn,bcgkl->bclgkp', cc, prev, jnp.exp(a_cs))
    return (y_diag + y_off).reshape(Bsz, S, G, K, P)


def hybrid_mixer(h, w_in, ssm_conv_w, ssm_conv_b, dt_bias, a_log, d_skip,
                 attn_norm_g, ssm_norm_g, w_out):
    Bsz, S, _ = h.shape
    s_pad = ((S + MOBA_BLOCK - 1) // MOBA_BLOCK) * MOBA_BLOCK
    hp = jnp.pad(h, ((0, 0), (0, s_pad - S), (0, 0)))
    proj = hp @ w_in.astype(h.dtype)
    cuts = [D_ATT, 2 * D_ATT, 3 * D_ATT, 3 * D_ATT + D_SSM, 3 * D_ATT + D_SSM + D_XBC]
    q, k, v, z, xbc, dt_raw = jnp.split(proj, cuts, axis=-1)

    def to_heads(t):
        return t.reshape(Bsz, s_pad, N_ATT_HEADS, ATT_HEAD_DIM).transpose(0, 2, 1, 3)

    att = lax.map(lambda qkv: moba_attention(*qkv), (to_heads(q), to_heads(k), to_heads(v)))
    att = att.transpose(0, 2, 1, 3).reshape(Bsz, s_pad, D_ATT)
    att = rms_norm(att, attn_norm_g)

    xbc = jax.nn.silu(causal_dwconv(xbc, ssm_conv_w, ssm_conv_b))
    xs, bm, cm = jnp.split(xbc, [D_SSM, D_SSM + N_SSM_GROUPS * D_STATE], axis=-1)
    dt = jax.nn.softplus(dt_raw.astype(jnp.float32) + dt_bias.astype(jnp.float32))
    a = -jnp.exp(a_log.astype(jnp.float32))
    xs5 = xs.reshape(Bsz, s_pad, N_SSM_GROUPS, SSM_HEADS_PER_GROUP, SSM_HEAD_DIM)
    y = ssd_scan(xs5,
                 dt.reshape(Bsz, s_pad, N_SSM_GROUPS, SSM_HEADS_PER_GROUP),
                 a.reshape(N_SSM_GROUPS, SSM_HEADS_PER_GROUP),
                 bm.reshape(Bsz, s_pad, N_SSM_GROUPS, D_STATE),
                 cm.reshape(Bsz, s_pad, N_SSM_GROUPS, D_STATE))
    y = y + d_skip.astype(jnp.float32).reshape(N_SSM_GROUPS, SSM_HEADS_PER_GROUP)[..., None] * xs5.astype(jnp.float32)
    y = y.reshape(Bsz, s_pad, D_SSM).astype(h.dtype) * jax.nn.silu(z)
    gsz = D_SSM // N_SSM_GROUPS
    y = rms_norm(y.reshape(Bsz, s_pad, N_SSM_GROUPS, gsz),
                 ssm_norm_g.reshape(N_SSM_GROUPS, gsz)).reshape(Bsz, s_pad, D_SSM)

    mixed = jnp.concatenate([att, y], axis=-1)[:, :S]
    return mixed @ w_out.astype(h.dtype)


def conv_ffn(h, w_up, ffn_conv_w, ffn_conv_b, w_down):
    u = h @ w_up.astype(h.dtype)
    u = causal_dwconv(u, ffn_conv_w, ffn_conv_b)
    gate, val = jnp.split(u, 2, axis=-1)
    return (jax.nn.silu(gate) * val) @ w_down.astype(h.dtype)


def setup_inputs(seed: int = 0) -> dict:
    key = jax.random.key(seed)
    ks = jax.random.split(key, 17)
    f32 = jnp.float32

    def nrm(k, shape, scale):
        return jax.random.normal(k, shape, f32) * scale

    u = jax.random.uniform(ks[5], (DEPTH, N_SSM_HEADS), f32)
    dt0 = jnp.exp(u * (math.log(0.1) - math.log(0.001)) + math.log(0.001))
    dt_bias = dt0 + jnp.log(-jnp.expm1(-dt0))
    a_log = jnp.log(jax.random.uniform(ks[6], (DEPTH, N_SSM_HEADS), f32, minval=1.0, maxval=16.0))
    return {
        'x': nrm(ks[0], (BATCH, SEQ, D_MODEL), 1.0),
        'ln1_g': 1.0 + nrm(ks[1], (DEPTH, D_MODEL), 0.02),
        'w_in': nrm(ks[2], (DEPTH, D_MODEL, D_IN_PROJ), D_MODEL ** -0.5),
        'ssm_conv_w': nrm(ks[3], (DEPTH, SSM_CONV, D_XBC), SSM_CONV ** -0.5),
        'ssm_conv_b': nrm(ks[4], (DEPTH, D_XBC), 0.02),
        'dt_bias': dt_bias,
        'a_log': a_log,
        'd_skip': 1.0 + nrm(ks[7], (DEPTH, N_SSM_HEADS), 0.1),
        'attn_norm_g': 1.0 + nrm(ks[8], (DEPTH, D_ATT), 0.02),
        'ssm_norm_g': 1.0 + nrm(ks[9], (DEPTH, D_SSM), 0.02),
        'w_out': nrm(ks[10], (DEPTH, D_MIX, D_MODEL), D_MIX ** -0.5),
        'ln2_g': 1.0 + nrm(ks[11], (DEPTH, D_MODEL), 0.02),
        'w_up': nrm(ks[12], (DEPTH, D_MODEL, 2 * D_FF), D_MODEL ** -0.5),
        'ffn_conv_w': nrm(ks[13], (DEPTH, FFN_CONV, 2 * D_FF), FFN_CONV ** -0.5),
        'ffn_conv_b': nrm(ks[14], (DEPTH, 2 * D_FF), 0.02),
        'w_down': nrm(ks[15], (DEPTH, D_FF, D_MODEL), D_FF ** -0.5),
        'lnf_g': 1.0 + nrm(ks[16], (D_MODEL,), 0.02),
    }


def reference(x, ln1_g, w_in, ssm_conv_w, ssm_conv_b, dt_bias, a_log, d_skip,
              attn_norm_g, ssm_norm_g, w_out, ln2_g, w_up, ffn_conv_w, ffn_conv_b,
              w_down, lnf_g):
    for l in range(DEPTH):
        x = x + hybrid_mixer(rms_norm(x, ln1_g[l]), w_in[l], ssm_conv_w[l], ssm_conv_b[l],
                             dt_bias[l], a_log[l], d_skip[l], attn_norm_g[l],
                             ssm_norm_g[l], w_out[l])
        x = x + conv_ffn(rms_norm(x, ln2_g[l]), w_up[l], ffn_conv_w[l], ffn_conv_b[l], w_down[l])
    return rms_norm(x, lnf_g)
```

```python
import contextlib
import numpy as np
import ml_dtypes
import concourse.bass as bass
import concourse.mybir as mybir
from concourse.bass_utils import run_bass_kernel_spmd

F32 = mybir.dt.float32
BF16 = mybir.dt.bfloat16
AF = mybir.ActivationFunctionType
ALU = mybir.AluOpType
AX = mybir.AxisListType

NCORES = 8
S = 2048
D = 1024
D_ATT = 512
D_SSM = 1536
NH_SSM = 24
D_XBC = 2560
D_IN = 5656
D_FF = 2816
EPS = 1e-6
C_Q, C_K, C_V, C_Z, C_XBC, C_DT = 0, 512, 1024, 1536, 3072, 5632
BIG = 30000.0


class Tok:
    __slots__ = ("key", "val")

    def __init__(self, key, val):
        self.key, self.val = key, val


class Buf:
    __slots__ = ("name", "w", "r")

    def __init__(self, name):
        self.name, self.w, self.r = name, {}, {}


class Prog:
    def __init__(self, nc, es):
        self.nc, self.es = nc, es
        self.eng = {"pe": nc.tensor, "act": nc.scalar, "dve": nc.vector, "pool": nc.gpsimd, "sp": nc.sync}
        self.sems, self.cnt, self.seen = {}, {}, {}
        self.pending = {e: [] for e in self.eng}
        self.nbuf = 0
        for e in self.eng:
            self._sem(e)

    def _sem(self, key):
        if key not in self.sems:
            self.sems[key] = self.es.enter_context(self.nc.semaphore("s_" + key))
            self.cnt[key] = 0
        return self.sems[key]

    def buf(self, name=None):
        self.nbuf += 1
        return Buf(name or f"b{self.nbuf}")

    def bufs(self, n, name=None):
        return [self.buf(None if name is None else f"{name}{i}") for i in range(n)]

    def wait(self, eng, tok):
        if tok is None:
            return
        if self.seen.get((eng, tok.key), 0) >= tok.val:
            return
        self.eng[eng].wait_ge(self.sems[tok.key], tok.val)
        self.seen[(eng, tok.key)] = tok.val

    def _deps(self, eng, reads, writes):
        for b in reads:
            for t in b.w.values():
                self.wait(eng, t)
        for b in writes:
            for t in b.w.values():
                self.wait(eng, t)
            for t in b.r.values():
                self.wait(eng, t)

    def _mark(self, tok, reads, writes):
        for b in reads:
            b.r[tok.key] = tok
        for b in writes:
            if b.r:
                b.r = {}
                b.w = {}
            b.w[tok.key] = tok

    def op(self, eng, fn, reads=(), writes=(), sig=True):
        self._deps(eng, reads, writes)
        ins = fn(self.eng[eng])
        if not sig:
            self.pending[eng].append((tuple(reads), tuple(writes)))
            return None
        ins.then_inc(self.sems[eng], 1)
        self.cnt[eng] += 1
        tok = Tok(eng, self.cnt[eng])
        for (r, w) in self.pending[eng]:
            self._mark(tok, r, w)
        self.pending[eng] = []
        self._mark(tok, reads, writes)
        return tok

    def dma(self, q, out, in_, key, reads=(), writes=()):
        self._sem(key)
        self._deps(q, reads, writes)
        self.eng[q].dma_start(out=out, in_=in_).then_inc(self.sems[key], 16)
        self.cnt[key] += 16
        tok = Tok(key, self.cnt[key])
        self._mark(tok, reads, writes)
        return tok

    def barrier(self):
        for e in self.eng:
            assert not self.pending[e], e
        for key in self.sems:
            if self.cnt[key] > 0:
                self.wait("sp", Tok(key, self.cnt[key]))
        self.eng["sp"].sem_inc(self.sems["sp"], 1)
        self.cnt["sp"] += 1
        tok = Tok("sp", self.cnt["sp"])
        for e in self.eng:
            self.wait(e, tok)

    def finish(self):
        for key in self.sems:
            if self.cnt[key] > 0:
                self.wait("sp", Tok(key, self.cnt[key]))


class Alloc:
    def __init__(self, nc, es):
        self.nc, self.es, self.n = nc, es, 0

    def sb(self, shape, dt, name=None):
        self.n += 1
        return self.es.enter_context(self.nc.sbuf_tensor(f"t{self.n}_" + (name or "sb"), list(shape), dt))

    def ps(self, shape, dt, name=None):
        self.n += 1
        return self.es.enter_context(self.nc.psum_tensor(f"p{self.n}_" + (name or "ps"), list(shape), dt))


def phase1(P, T, nseq):
    nc = P.nc
    with contextlib.ExitStack() as es:
        A = Alloc(nc, es)
        ngroups = nseq * 4
        w_bf = A.sb([128, 8, D_IN], BF16, "w_in_bf")
        b_w = P.buf("w_in")
        for kc in range(8):
            P.dma("pool", w_bf[:, kc, :], T["w_in"][kc * 128:(kc + 1) * 128, :], "w1", writes=[b_w])
        ident = A.sb([128, 128], BF16, "ident1")
        g1 = A.sb([128, D], F32, "g1rep")
        cw = A.sb([128, 20, 4], F32, "cw1")
        cb = A.sb([128, 20], F32, "cb1")
        dtb = A.sb([128, NH_SSM], F32, "dtb")
        b_c = P.buf("consts1")
        P.dma("sp", ident[:], T["ident"], "c1", writes=[b_c])
        P.dma("sp", g1[:], T["ln1_g"], "c1", writes=[b_c])
        P.dma("sp", cw[:], T["ssm_cw"], "c1", writes=[b_c])
        P.dma("sp", cb[:], T["ssm_cb"], "c1", writes=[b_c])
        P.dma("sp", dtb[:], T["dt_bias"], "c1", writes=[b_c])

        xin = [A.sb([128, D], F32, f"xin{i}") for i in range(4)]
        b_xin = P.bufs(4, "xin")
        junk = A.sb([128, D], BF16, "junk1")
        b_junk = P.buf("junk")
        ss = A.sb([128, 4], F32, "ss")
        ssv = A.sb([128, 4], F32, "ssv")
        ssl = A.sb([128, 4], F32, "ssl")
        rstd = A.sb([128, 4], F32, "rstd")
        b_ss, b_ssv, b_ssl, b_rstd = P.buf(), P.buf(), P.buf(), P.buf()
        hn = [A.sb([128, D], BF16, f"hn{i}") for i in range(4)]
        b_hn = P.bufs(4, "hn")
        hnT = [A.sb([128, 8, 512], BF16, f"hnT{i}") for i in range(2)]
        b_hnT = P.bufs(2, "hnT")
        pt = [A.ps([128, 8, 128], BF16, f"pt{i}") for i in range(2)]
        b_pt = P.bufs(2, "pt")
        pm = [A.ps([128, 512], F32, f"pm{i}") for i in range(3)]
        b_pm = P.bufs(3, "pm")
        px = [A.ps([128, 8, 128], BF16, f"px{i}") for i in range(2)]
        b_px = P.bufs(2, "px")
        pd = A.ps([128, 512], F32, "pd")
        b_pd = P.buf("pd")
        pre = [A.sb([128, 515], F32, f"pre{i}") for i in range(2)]
        b_pre = P.bufs(2, "pre")
        acc = [A.sb([128, 512], F32, f"acc{i}") for i in range(2)]
        b_acc = P.bufs(2, "acc")
        hist = A.sb([128, 20, 3], F32, "hist")
        b_hist = P.bufs(20, "hist")
        cvo = [A.sb([128, 512], BF16, f"cvo{i}") for i in range(4)]
        b_cvo = P.bufs(4, "cvo")
        xs_tok = A.sb([128, 4, D_SSM], BF16, "xs_tok")
        b_xs_tok = P.buf("xs_tok")
        b_tok = A.sb([128, 4, 512], BF16, "b_tok")
        b_b_tok = P.buf("b_tok")
        qk_ev = [A.sb([128, 512], BF16, f"qkev{i}") for i in range(3)]
        b_qk_ev = P.bufs(3, "qkev")
        v_ev = [A.sb([128, 512], BF16, f"vev{i}") for i in range(2)]
        b_v_ev = P.bufs(2, "vev")
        sz_ev = [A.sb([128, D_SSM], BF16, f"szev{i}") for i in range(2)]
        b_sz_ev = P.bufs(2, "szev")
        ntiles = nseq * 16
        dt_all = A.sb([128, ntiles, NH_SSM], F32, "dt_all")
        dt_e = A.sb([128, ntiles, NH_SSM], F32, "dt_e")
        b_dt_all, b_dt_e = P.buf("dt_all"), P.buf("dt_e")

        cnt = {"pm": 0, "pt": 0, "px": 0, "cvo": 0, "pre": 0, "qk": 0, "v": 0, "sz": 0}

        def norm(gi):
            s, t0 = gi // 4, (gi % 4) * 512
            g = gi % 2
            for tt in range(4):
                P.dma("pool", xin[tt][:], T["x"][s, t0 + tt * 128:t0 + (tt + 1) * 128, :], f"xin{tt}",
                      writes=[b_xin[tt]])
                P.op("act", lambda e, tt=tt: e.activation(out=junk[:], in_=xin[tt][:], func=AF.Square,
                                                         accum_out=ss[:, tt:tt + 1]),
                     reads=[b_xin[tt]], writes=[b_junk, b_ss])
            P.op("dve", lambda e: e.tensor_scalar(out=ssv[:], in0=ss[:], scalar1=1.0 / D, scalar2=EPS,
                                                  op0=ALU.mult, op1=ALU.add), reads=[b_ss], writes=[b_ssv])
            P.op("act", lambda e: e.activation(out=ssl[:], in_=ssv[:], func=AF.Ln), reads=[b_ssv], writes=[b_ssl])
            P.op("act", lambda e: e.activation(out=rstd[:], in_=ssl[:], func=AF.Exp, scale=-0.5),
                 reads=[b_ssl], writes=[b_rstd])
            for tt in range(4):
                h = tt
                P.op("dve", lambda e, tt=tt, h=h: e.scalar_tensor_tensor(
                    out=hn[h][:], in0=xin[tt][:], scalar=rstd[:, tt:tt + 1], in1=g1[:], op0=ALU.mult, op1=ALU.mult),
                    reads=[b_xin[tt], b_rstd, b_c], writes=[b_hn[h]])

        def norm_pe(gi):
            g = gi % 2
            for tt in range(4):
                h = tt
                pi = cnt["pt"] % 2
                cnt["pt"] += 1
                for kc in range(8):
                    P.op("pe", lambda e, pi=pi, kc=kc, h=h: e.transpose(
                        out=pt[pi][:, kc, :], in_=hn[h][:, kc * 128:(kc + 1) * 128], identity=ident[:]),
                        reads=[b_hn[h], b_c], writes=[b_pt[pi]], sig=(kc == 7))
                P.op("act", lambda e, pi=pi, tt=tt, g=g: e.copy(
                    out=hnT[g][:, :, tt * 128:(tt + 1) * 128], in_=pt[pi][:]),
                    reads=[b_pt[pi]], writes=[b_hnT[g]])

        def mm(gi, nxt):
            s, t0 = gi // 4, (gi % 4) * 512
            g = gi % 2
            deferred = []
            units = tok_units(gi)
            post = mm_post_factory(gi, deferred)

            def flush(upto):
                while deferred and deferred[0][0] <= upto:
                    deferred.pop(0)[1]()

            for c in range(28):
                col0 = c * 128 if c < 8 else C_XBC + (c - 8) * 128
                pi = cnt["pm"] % 3
                cnt["pm"] += 1
                for kc in range(8):
                    P.op("pe", lambda e, pi=pi, kc=kc, col0=col0: e.matmul(
                        pm[pi][:], lhsT=w_bf[:, kc, col0:col0 + 128], rhs=hnT[g][:, kc, :],
                        start=(kc == 0), stop=(kc == 7)),
                        reads=[b_w, b_hnT[g]], writes=[b_pm[pi]], sig=(kc == 7))
                if c < 8:
                    qi = cnt["qk"] % 3
                    cnt["qk"] += 1
                    P.op("dve", lambda e, qi=qi, pi=pi: e.tensor_copy(out=qk_ev[qi][:], in_=pm[pi][:]),
                         reads=[b_pm[pi]], writes=[b_qk_ev[qi]])
                    P.dma("sp", T["qk"][s, 2 * c:2 * c + 2, :, t0:t0 + 512].rearrange("h d t -> (h d) t"),
                          qk_ev[qi][:], f"qko{qi}", reads=[b_qk_ev[qi]])
                else:
                    cc = c - 8
                    pr = cnt["pre"] % 2
                    cnt["pre"] += 1
                    P.op("act", lambda e, pr=pr, pi=pi: e.copy(out=pre[pr][:, 3:515], in_=pm[pi][:]),
                         reads=[b_pm[pi]], writes=[b_pre[pr]])
                    if t0 == 0:
                        P.op("dve", lambda e, pr=pr: e.memset(pre[pr][:, 0:3], 0.0), writes=[b_pre[pr]])
                    else:
                        P.op("dve", lambda e, pr=pr, cc=cc: e.tensor_copy(out=pre[pr][:, 0:3], in_=hist[:, cc, :]),
                             reads=[b_hist[cc]], writes=[b_pre[pr]])
                    P.op("act", lambda e, pr=pr, cc=cc, pi=pi: e.activation(
                        out=acc[pr][:], in_=pm[pi][:], func=AF.Identity, scale=cw[:, cc, 3:4], bias=cb[:, cc:cc + 1]),
                        reads=[b_pm[pi], b_c], writes=[b_acc[pr]])
                    for k in (2, 1, 0):
                        P.op("dve", lambda e, pr=pr, cc=cc, k=k: e.scalar_tensor_tensor(
                            out=acc[pr][:], in0=pre[pr][:, k:k + 512], scalar=cw[:, cc, k:k + 1], in1=acc[pr][:],
                            op0=ALU.mult, op1=ALU.add), reads=[b_pre[pr], b_c, b_acc[pr]], writes=[b_acc[pr]])
                    P.op("dve", lambda e, pr=pr, cc=cc: e.tensor_copy(out=hist[:, cc, :], in_=pre[pr][:, 512:515]),
                         reads=[b_pre[pr]], writes=[b_hist[cc]])
                    deferred.append((c + 1, lambda c=c, cc=cc, pr=pr: post(c, cc, pr)))
                if c >= 8:
                    units.pop(0)()
                flush(c)
            flush(10 ** 9)
            assert not units
            if nxt is not None:
                norm_pe(nxt)

        def mm_post_factory(gi, deferred):
            s, t0 = gi // 4, (gi % 4) * 512

            def post(c, cc, pr):
                    ci = cnt["cvo"] % 4
                    cnt["cvo"] += 1
                    P.op("act", lambda e, ci=ci, pr=pr: e.activation(out=cvo[ci][:], in_=acc[pr][:], func=AF.Silu),
                         reads=[b_acc[pr]], writes=[b_cvo[ci]])
                    if cc >= 12:
                        gg = (cc - 12) % 4
                        dst = T["bT"] if cc < 16 else T["cT"]
                        P.dma("sp", dst[s, gg * 128:(gg + 1) * 128, t0:t0 + 512], cvo[ci][:], f"cvo{ci}",
                              reads=[b_cvo[ci]])
                    if cc < 16:
                        def tr(ci=ci, cc=cc):
                            xi = cnt["px"] % 2
                            cnt["px"] += 1
                            for tt in range(4):
                                P.op("pe", lambda e, xi=xi, tt=tt, ci=ci: e.transpose(
                                    out=px[xi][:, tt, :], in_=cvo[ci][:, tt * 128:(tt + 1) * 128], identity=ident[:]),
                                    reads=[b_cvo[ci], b_c], writes=[b_px[xi]], sig=(tt == 3))
                            deferred.append((c + 4, lambda: tr_ev(xi, cc)))

                        def tr_ev(xi, cc):
                            if cc < 12:
                                P.op("act", lambda e, xi=xi, cc=cc: e.copy(
                                    out=xs_tok[:, :, cc * 128:(cc + 1) * 128], in_=px[xi][:, 0:4, :]),
                                    reads=[b_px[xi]], writes=[b_xs_tok])
                                if cc == 11:
                                    P.dma("sp", T["xs"][s, t0:t0 + 512, :].rearrange("(tt p) c -> p tt c", p=128),
                                          xs_tok[:], "xso", reads=[b_xs_tok])
                            else:
                                gg = cc - 12
                                P.op("act", lambda e, xi=xi, gg=gg: e.copy(
                                    out=b_tok[:, :, gg * 128:(gg + 1) * 128], in_=px[xi][:, 0:4, :]),
                                    reads=[b_px[xi]], writes=[b_b_tok])
                                if gg == 3:
                                    P.dma("sp", T["btok"][s, t0:t0 + 512, :].rearrange("(tt p) c -> p tt c", p=128),
                                          b_tok[:], "bto", reads=[b_b_tok])
                        deferred.append((c + 3, tr))
            return post

        def tok_units(gi):
            s, t0 = gi // 4, (gi % 4) * 512
            g = gi % 2
            units = []
            for tt in range(4):
                units += tok_tile(gi, s, t0, g, tt)
            return units

        def tok_tile(gi, s, t0, g, tt):
            tsl = slice(tt * 128, (tt + 1) * 128)
            tile_idx = gi * 4 + tt
            st = {}

            def u_v():
                pi = cnt["pm"] % 3
                cnt["pm"] += 1
                for kc in range(8):
                    P.op("pe", lambda e, pi=pi, kc=kc: e.matmul(
                        pm[pi][:], lhsT=hnT[g][:, kc, tsl], rhs=w_bf[:, kc, C_V:C_V + 512],
                        start=(kc == 0), stop=(kc == 7)), reads=[b_w, b_hnT[g]], writes=[b_pm[pi]], sig=(kc == 7))
                vi = cnt["v"] % 2
                cnt["v"] += 1
                P.op("dve", lambda e, vi=vi, pi=pi: e.tensor_copy(out=v_ev[vi][:], in_=pm[pi][:]),
                     reads=[b_pm[pi]], writes=[b_v_ev[vi]])
                P.dma("sp", T["v"][s, t0 + tt * 128:t0 + (tt + 1) * 128, :], v_ev[vi][:], f"vo{vi}",
                      reads=[b_v_ev[vi]])
                st["zi"] = cnt["sz"] % 2
                cnt["sz"] += 1

            def u_z(j):
                zi = st["zi"]
                pi = cnt["pm"] % 3
                cnt["pm"] += 1
                for kc in range(8):
                    P.op("pe", lambda e, pi=pi, kc=kc, j=j: e.matmul(
                        pm[pi][:], lhsT=hnT[g][:, kc, tsl], rhs=w_bf[:, kc, C_Z + j * 512:C_Z + (j + 1) * 512],
                        start=(kc == 0), stop=(kc == 7)),
                        reads=[b_w, b_hnT[g]], writes=[b_pm[pi]], sig=(kc == 7))
                P.op("act", lambda e, zi=zi, pi=pi, j=j: e.activation(
                    out=sz_ev[zi][:, j * 512:(j + 1) * 512], in_=pm[pi][:], func=AF.Silu),
                    reads=[b_pm[pi]], writes=[b_sz_ev[zi]])
                if j == 2:
                    P.dma("sp", T["sz"][s, t0 + tt * 128:t0 + (tt + 1) * 128, :], sz_ev[zi][:], f"szo{zi}",
                          reads=[b_sz_ev[zi]])

            def u_dt():
                for kc in range(8):
                    P.op("pe", lambda e, kc=kc: e.matmul(
                        pd[:, 0:NH_SSM], lhsT=hnT[g][:, kc, tsl], rhs=w_bf[:, kc, C_DT:C_DT + NH_SSM],
                        start=(kc == 0), stop=(kc == 7)), reads=[b_w, b_hnT[g]], writes=[b_pd], sig=(kc == 7))
                P.op("dve", lambda e, tile_idx=tile_idx: e.tensor_tensor(
                    out=dt_all[:, tile_idx, :], in0=pd[:, 0:NH_SSM], in1=dtb[:], op=ALU.add),
                    reads=[b_pd, b_c], writes=[b_dt_all])

            return [u_v, lambda: u_z(0), lambda: u_z(1), lambda: u_z(2), u_dt]

        norm(0)
        norm_pe(0)
        for gi in range(ngroups):
            if gi + 1 < ngroups:
                norm(gi + 1)
            mm(gi, gi + 1 if gi + 1 < ngroups else None)
        P.op("dve", lambda e: e.tensor_scalar_min(out=dt_all[:], in0=dt_all[:], scalar1=60.0),
             reads=[b_dt_all], writes=[b_dt_all])
        P.op("act", lambda e: e.activation(out=dt_e[:], in_=dt_all[:], func=AF.Exp), reads=[b_dt_all], writes=[b_dt_e])
        P.op("act", lambda e: e.activation(out=dt_all[:], in_=dt_e[:], func=AF.Ln, bias=1.0, scale=1.0),
             reads=[b_dt_e], writes=[b_dt_all])
        for s in range(nseq):
            P.dma("sp", T["dt"][s], dt_all[:, s * 16:(s + 1) * 16, :], "dto",
                  reads=[b_dt_all])
        P.barrier()


SLOPES = [2.0 ** (-(h + 1)) for h in range(8)]


def phase2a(P, T, nseq, w_out_bf=None, b_wout=None):
    nc = P.nc
    with contextlib.ExitStack() as es:
        A = Alloc(nc, es)
        ident = A.sb([128, 128], BF16, "ident2")
        cmask = A.sb([128, 2, 256], BF16, "cmask")
        ownmask = A.sb([128, 16, 8], F32, "ownmask")
        kbias = A.sb([128, 8, 2], F32, "kbias")
        pastmask = A.sb([128, 16, 8], F32, "pastmask")
        dmbase = A.sb([128, 16, 8], F32, "dmbase")
        ag = A.sb([128, D_ATT], F32, "ag")
        b_c = P.buf("consts2")
        P.dma("sp", ident[:], T["ident"], "c2", writes=[b_c])
        P.dma("sp", cmask[:], T["cmask"], "c2", writes=[b_c])
        P.dma("sp", ownmask[:], T["ownmask"], "c2", writes=[b_c])
        P.dma("sp", kbias[:], T["kbias"], "c2", writes=[b_c])
        P.dma("sp", pastmask[:], T["pastmask"], "c2", writes=[b_c])
        P.dma("sp", dmbase[:], T["dmbase"], "c2", writes=[b_c])
        P.dma("sp", ag[:], T["attn_g"], "c2", writes=[b_c])
        qa = [A.sb([128, S], BF16, f"qa{i}") for i in range(2)]
        ka = [A.sb([128, S], BF16, f"ka{i}") for i in range(2)]
        b_qa, b_ka = P.bufs(2, "qa"), P.bufs(2, "ka")
        for i in range(2):
            P.op("pool", lambda e, i=i: e.memset(qa[i][64:128, :], 0.0), writes=[b_qa[i]])
            P.op("pool", lambda e, i=i: e.memset(ka[i][64:128, :], 0.0), writes=[b_ka[i]])
        for i in range(2):
            P.dma("sp", ka[i][64:73, :], T["karows"], f"kld{i}", writes=[b_ka[i]])
        vh = [A.sb([128, 16, 65], BF16, f"vh{i}") for i in range(2)]
        b_vh = P.bufs(2, "vh")
        for i in range(2):
            P.op("pool", lambda e, i=i: e.memset(vh[i][:, :, 64:65], 1.0), writes=[b_vh[i]])
        ksum = A.sb([64, 8], F32, "ksum")
        kmT = A.sb([64, 8], BF16, "kmT")
        b_ksum, b_kmT = P.buf(), P.buf()
        Gm = A.sb([128, 16, 8], F32, "Gm")
        top8 = A.sb([128, 16, 8], F32, "top8")
        selm = A.sb([128, 16, 8], F32, "selm")
        selt = A.sb([128, 16, 8], F32, "selt")
        pen = A.sb([128, 16, 8], BF16, "pen")
        b_Gm, b_top8, b_selm, b_selt, b_pen = P.buf(), P.buf(), P.buf(), P.buf(), P.buf()
        PT = [A.sb([128, 256], BF16, f"PT{i}") for i in range(4)]
        b_PT = P.bufs(4, "PT")
        att_tok2 = [A.sb([128, 16, D_ATT], F32, f"att_tok{i}") for i in range(2)]
        b_att2 = P.bufs(2, "att_tok")
        rden = A.sb([128, 2, 2], F32, "rden")
        b_rden = P.bufs(2, "rden")
        junk = A.sb([128, D_ATT], BF16, "junk2")
        b_junk = P.buf()
        ss = A.sb([128, 16], F32, "ss2")
        ssv = A.sb([128, 16], F32, "ssv2")
        ssl = A.sb([128, 16], F32, "ssl2")
        rstd = A.sb([128, 16], F32, "rstd2")
        b_ss, b_ssv, b_ssl, b_rstd = P.buf(), P.buf(), P.buf(), P.buf()
        atn = [A.sb([128, D_ATT], BF16, f"atn{i}") for i in range(2)]
        b_atn = P.bufs(2, "atn")
        pG_ = A.ps([128, 64, 8], F32, "pG")
        pG = pG_[:, 0:16, :]
        b_pG = P.buf("pG")
        ppen = A.ps([8, 1024], BF16, "ppen")
        b_ppen = P.buf("ppen")
        pS = [A.ps([128, 512], F32, f"pS{i}") for i in range(4)]
        b_pS = P.bufs(4, "pS")
        pacc = [A.ps([128, 4, 128], F32, f"pacc{i}") for i in range(2)]
        b_pacc = P.bufs(2, "pacc")
        cnt = {"S": 0, "pen": 0, "atn": 0}

        def load_v(s, h):
            b = h % 2
            P.dma("sp", vh[b][:, :, 0:64], T["v"][s, :, h * 64:(h + 1) * 64].rearrange("(kt p) d -> p kt d", p=128),
                  f"vld{b}", writes=[b_vh[b]])

        def prep1(s, h):
            b = h % 2
            load_v(s, h)
            P.dma("sp", qa[b][0:64, :], T["qk"][s, h], f"qld{b}", writes=[b_qa[b]])
            P.dma("sp", qa[b][72:73, :], T["qrow"][h:h + 1, :], f"qld{b}", writes=[b_qa[b]])
            P.dma("sp", ka[b][0:64, :], T["qk"][s, 8 + h], f"kld{b}", writes=[b_ka[b]])
            P.op("dve", lambda e: e.tensor_reduce(out=ksum[:], in_=ka[b][0:64, :].rearrange("p (j k) -> p j k", k=256),
                                                  axis=AX.X, op=ALU.add), reads=[b_ka[b]], writes=[b_ksum])
            P.op("dve", lambda e: e.tensor_scalar(out=kmT[:], in0=ksum[:], scalar1=1.0 / 256, scalar2=None, op0=ALU.mult),
                 reads=[b_ksum], writes=[b_kmT])
            for t in range(16):
                P.op("pe", lambda e, t=t: e.matmul(pG_[:, t, :], lhsT=qa[b][0:64, t * 128:(t + 1) * 128], rhs=kmT[:],
                                                   start=True, stop=True),
                     reads=[b_qa[b], b_kmT], writes=[b_pG], sig=(t == 15))
            P.op("dve", lambda e: e.tensor_tensor(out=Gm[:], in0=pG, in1=pastmask[:], op=ALU.add),
                 reads=[b_pG, b_c], writes=[b_Gm])
            for t in range(16):
                P.op("dve", lambda e, t=t: e.max(out=top8[:, t, :], in_=Gm[:, t, :]), reads=[b_Gm], writes=[b_top8])
            P.op("dve", lambda e: e.tensor_tensor(out=selm[:], in0=Gm[:], in1=top8[:, :, 2:3].to_broadcast([128, 16, 8]),
                                                  op=ALU.is_ge), reads=[b_Gm, b_top8], writes=[b_selm])
            P.op("dve", lambda e: e.tensor_tensor(out=selm[:], in0=selm[:], in1=ownmask[:], op=ALU.max),
                 reads=[b_selm, b_c], writes=[b_selm])
            P.op("dve", lambda e: e.tensor_scalar(out=selt[:], in0=selm[:], scalar1=-1.0, scalar2=BIG,
                                                  op0=ALU.add, op1=ALU.mult), reads=[b_selm], writes=[b_selt])
            P.op("dve", lambda e: e.scalar_tensor_tensor(out=pen[:], in0=dmbase[:], scalar=-2048.0 * SLOPES[h],
                                                         in1=selt[:], op0=ALU.mult, op1=ALU.add),
                 reads=[b_selt, b_c], writes=[b_pen])

        def prep2(s, h):
            b = h % 2
            for grp in range(4):
                for j in range(4):
                    t = grp * 4 + j
                    P.op("pe", lambda e, t=t, j=j: e.transpose(
                        out=ppen[:, j * 128:(j + 1) * 128], in_=pen[:, t, :], identity=ident[:]),
                        reads=[b_pen, b_c], writes=[b_ppen], sig=(j == 3))
                P.op("dve", lambda e, grp=grp: e.tensor_copy(out=qa[b][64:72, grp * 512:(grp + 1) * 512],
                                                            in_=ppen[:, 0:512]),
                     reads=[b_ppen], writes=[b_qa[b]])

        def qk(s, h, i, kt):
            b = h % 2
            si = cnt["S"] % 4
            cnt["S"] += 1
            Sp = pS[si][:, 0:256]
            qs = slice(i * 256, (i + 1) * 256)
            ks = slice(kt * 128, (kt + 1) * 128)
            diag = (kt // 2 == i)
            P.op("pe", lambda e: e.matmul(Sp, lhsT=ka[b][:, ks], rhs=qa[b][:, qs], start=True, stop=not diag),
                 reads=[b_ka[b], b_qa[b]], writes=[b_pS[si]], sig=not diag)
            if diag:
                P.op("pe", lambda e: e.matmul(Sp, lhsT=ident[:], rhs=cmask[:, kt % 2, :], start=False, stop=True),
                     reads=[b_c], writes=[b_pS[si]])
            P.op("act", lambda e: e.activation(out=PT[si][:], in_=Sp, func=AF.Exp, scale=0.125,
                                               bias=kbias[:, h, kt % 2:kt % 2 + 1]),
                 reads=[b_pS[si], b_c], writes=[b_PT[si]])
            return si

        def pv(s, h, i, kt, si):
            last = 2 * i + 1
            a = i % 2
            for u in range(2):
                P.op("pe", lambda e, u=u: e.matmul(pacc[a][:, u, 0:65], lhsT=PT[si][:, u * 128:(u + 1) * 128],
                                                   rhs=vh[h % 2][:, kt, :], start=(kt == 0 and u == 0), stop=(kt == last),
                                                   skip_group_check=True),
                     reads=[b_PT[si], b_vh[h % 2]], writes=[b_pacc[a]], sig=(u == 1))
            if kt == last:
                P.op("dve", lambda e: e.reciprocal(out=rden[:, a, :], in_=pacc[a][:, 0:2, 64]),
                     reads=[b_pacc[a]], writes=[b_rden[a]])
                for u in range(2):
                    P.op("dve", lambda e, u=u: e.tensor_scalar(
                        out=att_tok2[s % 2][:, 2 * i + u, h * 64:(h + 1) * 64], in0=pacc[a][:, u, 0:64],
                        scalar1=rden[:, a, u:u + 1], scalar2=None, op0=ALU.mult),
                        reads=[b_pacc[a], b_rden[a]], writes=[b_att2[s % 2]])

        def main(s, h, hooks):
            steps = [(i, kt) for i in range(8) for kt in range(2 * i + 2)]
            look = 3
            sis = {}
            for n in range(min(look, len(steps))):
                sis[n] = qk(s, h, *steps[n])
            for n, (i, kt) in enumerate(steps):
                if n + look < len(steps):
                    sis[n + look] = qk(s, h, *steps[n + look])
                pv(s, h, i, kt, sis.pop(n))
                if kt == 2 * i + 1 and i in hooks:
                    hooks[i]()

        def finish_seq(s):
            att_tok, b_att = att_tok2[s % 2], b_att2[s % 2]
            for t in range(16):
                P.op("act", lambda e, t=t: e.activation(out=junk[:], in_=att_tok[:, t, :], func=AF.Square,
                                                       accum_out=ss[:, t:t + 1]), reads=[b_att], writes=[b_junk, b_ss])
            P.op("dve", lambda e: e.tensor_scalar(out=ssv[:], in0=ss[:], scalar1=1.0 / D_ATT, scalar2=EPS,
                                                  op0=ALU.mult, op1=ALU.add), reads=[b_ss], writes=[b_ssv])
            P.op("act", lambda e: e.activation(out=ssl[:], in_=ssv[:], func=AF.Ln), reads=[b_ssv], writes=[b_ssl])
            P.op("act", lambda e: e.activation(out=rstd[:], in_=ssl[:], func=AF.Exp, scale=-0.5),
                 reads=[b_ssl], writes=[b_rstd])
            for t in range(16):
                ai = cnt["atn"] % 2
                cnt["atn"] += 1
                P.op("dve", lambda e, t=t, ai=ai: e.tensor_scalar(
                    out=atn[ai][:], in0=att_tok[:, t, :], scalar1=rstd[:, t:t + 1], scalar2=None, op0=ALU.mult),
                    reads=[b_att, b_rstd], writes=[b_atn[ai]])
                P.dma("sp", T["mixed"][s, t * 128:(t + 1) * 128, 0:D_ATT], atn[ai][:], f"atno{ai}", reads=[b_atn[ai]])

        wq = []
        if w_out_bf is not None:
            gcol = A.sb([128, 16], F32, "gcol")
            b_gcol = P.buf("gcol")
            P.dma("sp", gcol[:], T["mix_g"], "gcol", writes=[b_gcol])
            stage = [A.sb([128, D], F32, f"wstage{i}") for i in range(2)]
            b_stage = P.bufs(2)

            def wload(kc):
                si = kc % 2
                P.dma("sp", stage[si][:], T["w_out"][kc * 128:(kc + 1) * 128, :], f"wst{si}", writes=[b_stage[si]])
                P.op("act", lambda e: e.activation(out=w_out_bf[:, kc, :], in_=stage[si][:], func=AF.Copy,
                                                   scale=gcol[:, kc:kc + 1]),
                     reads=[b_stage[si], b_gcol], writes=[b_wout])
            wq = [lambda kc=kc: wload(kc) for kc in range(16)]

        heads = [(s, h) for s in range(nseq) for h in range(8)]
        prep1(*heads[0])
        prep2(*heads[0])
        pending_fin = []
        for n, (s, h) in enumerate(heads):
            hooks = {}
            extra = {}
            if wq:
                for i in (0, 2, 4, 6):
                    if wq:
                        extra[i] = wq.pop(0)
            if pending_fin:
                extra[1] = pending_fin.pop(0)
            nxt = heads[n + 1] if n + 1 < len(heads) else None

            def mk(i, nxt=nxt, extra=extra):
                def f():
                    if i in extra:
                        extra[i]()
                    if nxt is not None and i == 3:
                        prep1(*nxt)
                    if nxt is not None and i == 5:
                        prep2(*nxt)
                return f
            for i in range(8):
                hooks[i] = mk(i)
            main(s, h, hooks)
            if h == 7:
                pending_fin.append(lambda s=s: finish_seq(s))
        while wq:
            wq.pop(0)()
        while pending_fin:
            pending_fin.pop(0)()
        P.barrier()


def phase2b(P, T, nseq):
    nc = P.nc
    with contextlib.ExitStack() as es:
        A = Alloc(nc, es)
        U = A.sb([128, 128], F32, "U")
        Ls = A.sb([128, 128], F32, "Ls")
        ones = A.sb([128, 128], F32, "ones")
        alog = A.sb([128, NH_SSM], F32, "alog")
        arep = A.sb([128, NH_SSM], F32, "arep")
        dsk = A.sb([128, NH_SSM], F32, "dsk")
        sg = A.sb([128, D_SSM], F32, "sg")
        b_c = P.buf("consts2b")
        P.dma("sp", U[:], T["U"], "c3", writes=[b_c])
        P.dma("sp", Ls[:], T["Ls"], "c3", writes=[b_c])
        P.dma("sp", ones[:], T["ones"], "c3", writes=[b_c])
        P.dma("sp", alog[:], T["a_log"], "c3", writes=[b_c])
        P.dma("sp", dsk[:], T["d_skip"], "c3", writes=[b_c])
        P.dma("sp", sg[:], T["ssm_g"], "c3", writes=[b_c])
        b_a = P.buf("arep")
        P.op("act", lambda e: e.activation(out=arep[:], in_=alog[:], func=AF.Exp), reads=[b_c], writes=[b_a])
        P.op("dve", lambda e: e.tensor_scalar(out=arep[:], in0=arep[:], scalar1=-1.0, scalar2=None, op0=ALU.mult),
             reads=[b_a], writes=[b_a])

        def two(shape, dt, name, n=2):
            return [A.sb(shape, dt, f"{name}{i}") for i in range(n)]
        xs = two([128, D_SSM], BF16, "xs", 3); b_xs = P.bufs(3)
        bt = two([128, 512], BF16, "bt", 3); b_bt = P.bufs(3)
        bT = two([128, 4, 128], BF16, "bT", 3); b_bT = P.bufs(3)
        cT = two([128, 4, 128], BF16, "cT", 3); b_cT = P.bufs(3)
        sz = two([128, D_SSM], BF16, "sz", 3); b_sz = P.bufs(3)
        dt = two([128, NH_SSM], F32, "dt", 3); b_dt = P.bufs(3)
        adt = two([128, NH_SSM], F32, "adt"); b_adt = P.bufs(2)
        ecs3 = two([128, 72], F32, "ecs3"); b_ecs3 = P.bufs(2)
        Dm = two([128, NH_SSM, 128], F32, "Dm"); b_Dm = P.bufs(2)
        xdtw = two([128, D_SSM], BF16, "xdtw"); b_xdtw = P.bufs(2)
        cbm = two([128, 4, 128], BF16, "cbm"); b_cbm = P.bufs(2)
        lndt = two([128, NH_SSM], F32, "lndt"); b_lndt = P.bufs(2)
        dtw = two([128, NH_SSM], F32, "dtw"); b_dtw = P.bufs(2)
        M = two([128, NH_SSM, 128], BF16, "M")
        b_M = [P.bufs(8), P.bufs(8)]
        E = two([128, 3, 128], BF16, "E"); b_E = P.bufs(2)
        hst = A.sb([128, D_SSM], F32, "hst"); b_h = P.bufs(4, "h")
        hbf = A.sb([128, D_SSM], BF16, "hbf"); b_hbf = P.bufs(4, "hbf")
        yo = A.sb([128, 384], F32, "yo"); b_yo = P.buf()
        yall2 = two([128, D_SSM], F32, "yall"); b_y2 = [P.bufs(4), P.bufs(4)]
        junk = A.sb([128, 384], BF16, "junk3"); b_junk = P.buf()
        ss2 = two([128, 4], F32, "ss3"); b_ss2 = P.bufs(2); ssv = A.sb([128, 4], F32, "ssv3")
        ssl = A.sb([128, 4], F32, "ssl3"); rstd = A.sb([128, 4], F32, "rstd3")
        b_ssv, b_ssl, b_rstd = P.buf(), P.buf(), P.buf()
        ymix = two([128, D_SSM], BF16, "ymix"); b_ymix = P.bufs(2)
        pc = A.ps([128, 512], F32, "pc"); b_pc = P.buf()
        pcb = A.ps([128, 4, 128], F32, "pcb"); b_pcb = P.buf()
        pseg = [A.ps([128, 512], F32, f"pseg{i}") for i in range(3)]; b_pseg = P.bufs(3)
        pyd = A.ps([128, 512], F32, "pyd"); b_pyd = P.buf()
        pyo = A.ps([128, 512], F32, "pyo"); b_pyo = P.buf()
        pst = A.ps([128, 512], F32, "pst"); b_pst = P.buf()
        cnt = {"seg": 0}

        def bc(ap, shape, axis):
            return ap.unsqueeze(axis).to_broadcast(shape)

        identf = A.sb([128, 128], F32, "identf")
        dfull = A.sb([128, 128], F32, "dfull")
        dhif = A.sb([128, 128], F32, "dhif")
        Dhi = A.sb([128, NH_SSM, 128], BF16, "Dhi")
        Dlo = A.sb([128, NH_SSM, 128], BF16, "Dlo")
        b_idf, b_dfull, b_dhif, b_Dhl = P.buf(), P.buf(), P.buf(), P.buf()
        P.dma("sp", identf[:], T["identf"], "c3i", writes=[b_idf])
        for h in range(NH_SSM):
            P.op("dve", lambda e, h=h: e.tensor_scalar(out=dfull[:], in0=identf[:], scalar1=dsk[:, h:h + 1], scalar2=None,
                                                       op0=ALU.mult), reads=[b_idf, b_c], writes=[b_dfull])
            P.op("dve", lambda e, h=h: e.tensor_copy(out=Dhi[:, h, :], in_=dfull[:]), reads=[b_dfull], writes=[b_Dhl])
            P.op("dve", lambda e, h=h: e.tensor_copy(out=dhif[:], in_=Dhi[:, h, :]), reads=[b_Dhl], writes=[b_dhif])
            P.op("dve", lambda e, h=h: e.tensor_tensor(out=Dlo[:, h, :], in0=dfull[:], in1=dhif[:], op=ALU.subtract),
                 reads=[b_dfull, b_dhif], writes=[b_Dhl])

        def load(s, c):
            k = (s * 16 + c) % 3
            t0 = c * 128
            P.dma("sp", xs[k][:], T["xs"][s, t0:t0 + 128, :], f"l_xs{k}", writes=[b_xs[k]])
            P.dma("sp", bt[k][:], T["btok"][s, t0:t0 + 128, :], f"l_bt{k}", writes=[b_bt[k]])
            P.dma("sp", bT[k][:], T["bT"][s, :, t0:t0 + 128].rearrange("(g n) l -> n g l", n=128), f"l_bT{k}",
                  writes=[b_bT[k]])
            P.dma("sp", cT[k][:], T["cT"][s, :, t0:t0 + 128].rearrange("(g n) l -> n g l", n=128), f"l_cT{k}",
                  writes=[b_cT[k]])
            P.dma("sp", sz[k][:], T["sz"][s, t0:t0 + 128, :], f"l_sz{k}", writes=[b_sz[k]])
            P.dma("sp", dt[k][:], T["dt"][s, :, c, :], f"l_dt{k}", writes=[b_dt[k]])

        def prefront(s, c):
            k = (s * 16 + c) % 2
            kl = (s * 16 + c) % 3
            P.op("dve", lambda e: e.tensor_tensor(out=adt[k][:], in0=dt[kl][:], in1=arep[:], op=ALU.mult),
                 reads=[b_dt[kl], b_a], writes=[b_adt[k]])
            for j, lm in enumerate((U, Ls, ones)):
                P.op("pe", lambda e, j=j, lm=lm: e.matmul(pc[:, j * 24:(j + 1) * 24], lhsT=lm[:], rhs=adt[k][:],
                                                          start=True, stop=True),
                     reads=[b_c, b_adt[k]], writes=[b_pc], sig=(j == 2))
            P.op("act", lambda e: e.activation(out=ecs3[k][:], in_=pc[:, 0:72], func=AF.Exp),
                 reads=[b_pc], writes=[b_ecs3[k]])
            P.op("act", lambda e: e.activation(out=lndt[k][:], in_=dt[kl][:], func=AF.Ln), reads=[b_dt[kl]], writes=[b_lndt[k]])
            P.op("pool", lambda e: e.tensor_tensor(out=Dm[k][:], in0=bc(U[:], [128, NH_SSM, 128], 1),
                                                   in1=bc(adt[k][:], [128, NH_SSM, 128], 2), op=ALU.mult),
                 reads=[b_c, b_adt[k]], writes=[b_Dm[k]])
            P.op("dve", lambda e: e.tensor_tensor(out=dtw[k][:], in0=dt[kl][:], in1=ecs3[k][:, 24:48], op=ALU.mult),
                 reads=[b_dt[kl], b_ecs3[k]], writes=[b_dtw[k]])
            P.op("pool", lambda e: e.tensor_tensor(out=xdtw[k][:].rearrange("p (h d) -> p h d", d=64),
                                                   in0=xs[kl][:].rearrange("p (h d) -> p h d", d=64),
                                                   in1=bc(dtw[k][:], [128, NH_SSM, 64], 2), op=ALU.mult),
                 reads=[b_xs[kl], b_dtw[k]], writes=[b_xdtw[k]])
            for g in range(4):
                P.op("pe", lambda e, g=g: e.matmul(pcb[:, g, :], lhsT=bT[kl][:, g, :], rhs=cT[kl][:, g, :],
                                                   start=True, stop=True),
                     reads=[b_bT[kl], b_cT[kl]], writes=[b_pcb], sig=(g == 3))
            P.op("dve", lambda e: e.tensor_tensor(out=cbm[k][:], in0=pcb[:], in1=bc(U[:], [128, 4, 128], 1), op=ALU.mult),
                 reads=[b_pcb, b_c], writes=[b_cbm[k]])

        def segEM(s, c, hg):
            k = (s * 16 + c) % 2
            if True:
                g = hg // 2
                si = cnt["seg"] % 3
                ei = cnt["seg"] % 2
                cnt["seg"] += 1
                P.op("pe", lambda e, hg=hg, si=si: e.matmul(
                    pseg[si][:, 0:384], lhsT=Ls[:], rhs=Dm[k][:, hg * 3:(hg + 1) * 3, :], start=True, stop=True),
                    reads=[b_c, b_Dm[k]], writes=[b_pseg[si]])
                for j in range(3):
                    h = hg * 3 + j
                    P.op("act", lambda e, si=si, ei=ei, j=j, h=h: e.activation(
                        out=E[ei][:, j, :], in_=pseg[si][:, j * 128:(j + 1) * 128], func=AF.Exp, bias=lndt[k][:, h:h + 1]),
                        reads=[b_pseg[si], b_lndt[k]], writes=[b_E[ei]])
                P.op("dve", lambda e, hg=hg, ei=ei, g=g: e.tensor_tensor(
                    out=M[k][:, hg * 3:(hg + 1) * 3, :], in0=E[ei][:], in1=bc(cbm[k][:, g, :], [128, 3, 128], 1),
                    op=ALU.mult), reads=[b_E[ei], b_cbm[k]], writes=[b_M[k][hg]])

        def back_g(s, c, g):
            k = (s * 16 + c) % 2
            kl = (s * 16 + c) % 3
            yall, b_y, ss, b_ss = yall2[k], b_y2[k], ss2[k], b_ss2[k]
            if True:
                gs = slice(g * 384, (g + 1) * 384)
                hs = slice(g * 6, (g + 1) * 6)
                for kk in range(6):
                    h = g * 6 + kk
                    for mi, lm in enumerate((M[k], Dhi, Dlo)):
                        P.op("pe", lambda e, kk=kk, h=h, mi=mi, lm=lm: e.matmul(
                            pyd[:, kk * 64:(kk + 1) * 64], lhsT=lm[:, h, :], rhs=xs[kl][:, h * 64:(h + 1) * 64],
                            start=(mi == 0), stop=(mi == 2)),
                            reads=[b_M[k][h // 3], b_xs[kl], b_Dhl], writes=[b_pyd], sig=(kk == 5 and mi == 2))
                if c > 0:
                    P.op("pe", lambda e: e.matmul(pyo[:, 0:384], lhsT=cT[kl][:, g, :], rhs=hbf[:, gs], start=True, stop=True),
                         reads=[b_cT[kl], b_hbf[g]], writes=[b_pyo])
                    P.op("dve", lambda e: e.tensor_tensor(
                        out=yo[:].rearrange("p (h d) -> p h d", d=64),
                        in0=pyo[:, 0:384].rearrange("p (h d) -> p h d", d=64),
                        in1=bc(ecs3[k][:, hs], [128, 6, 64], 2), op=ALU.mult),
                        reads=[b_pyo, b_ecs3[k]], writes=[b_yo])
                    P.op("dve", lambda e: e.tensor_tensor(out=yall[:, gs], in0=pyd[:, 0:384], in1=yo[:], op=ALU.add),
                         reads=[b_pyd, b_yo], writes=[b_y[g]])
                    P.op("dve", lambda e: e.tensor_tensor(out=yall[:, gs], in0=yall[:, gs], in1=sz[kl][:, gs], op=ALU.mult),
                         reads=[b_y[g], b_sz[kl]], writes=[b_y[g]])
                else:
                    P.op("dve", lambda e: e.tensor_tensor(out=yall[:, gs], in0=pyd[:, 0:384], in1=sz[kl][:, gs], op=ALU.mult),
                         reads=[b_pyd, b_sz[kl]], writes=[b_y[g]])
                P.op("act", lambda e, g=g: e.activation(out=junk[:], in_=yall[:, gs], func=AF.Square,
                                                       accum_out=ss[:, g:g + 1]), reads=[b_y[g]], writes=[b_junk, b_ss])
                if c < 15:
                    P.op("pe", lambda e: e.matmul(pst[:, 0:384], lhsT=bt[kl][:, g * 128:(g + 1) * 128], rhs=xdtw[k][:, gs],
                                                  start=True, stop=True), reads=[b_bt[kl], b_xdtw[k]], writes=[b_pst])
                    if c > 0:
                        P.op("pool", lambda e: e.tensor_tensor(
                            out=hst[:, gs].rearrange("p (h d) -> p h d", d=64),
                            in0=hst[:, gs].rearrange("p (h d) -> p h d", d=64),
                            in1=bc(ecs3[k][:, 48 + g * 6:48 + (g + 1) * 6], [128, 6, 64], 2), op=ALU.mult),
                            reads=[b_h[g], b_ecs3[k]], writes=[b_h[g]])
                        P.op("dve", lambda e: e.tensor_tensor(out=hst[:, gs], in0=pst[:, 0:384], in1=hst[:, gs], op=ALU.add),
                             reads=[b_pst, b_h[g]], writes=[b_h[g]])
                    else:
                        P.op("dve", lambda e: e.tensor_copy(out=hst[:, gs], in_=pst[:, 0:384]),
                             reads=[b_pst], writes=[b_h[g]])
                    P.op("act", lambda e: e.copy(out=hbf[:, gs], in_=hst[:, gs]), reads=[b_h[g]], writes=[b_hbf[g]])

        def tail(s, c):
            k = (s * 16 + c) % 2
            t0 = c * 128
            yall, b_y, ss, b_ss = yall2[k], b_y2[k], ss2[k], b_ss2[k]
            P.op("dve", lambda e: e.tensor_scalar(out=ssv[:], in0=ss[:], scalar1=1.0 / 384, scalar2=EPS,
                                                  op0=ALU.mult, op1=ALU.add), reads=[b_ss], writes=[b_ssv])
            P.op("act", lambda e: e.activation(out=ssl[:], in_=ssv[:], func=AF.Ln), reads=[b_ssv], writes=[b_ssl])
            P.op("act", lambda e: e.activation(out=rstd[:], in_=ssl[:], func=AF.Exp, scale=-0.5),
                 reads=[b_ssl], writes=[b_rstd])
            for g in range(4):
                gs = slice(g * 384, (g + 1) * 384)
                P.op("act", lambda e, g=g: e.activation(out=ymix[k][:, gs], in_=yall[:, gs], func=AF.Copy,
                                                       scale=rstd[:, g:g + 1]),
                     reads=[b_y[g], b_rstd], writes=[b_ymix[k]])
            P.dma("sp", T["mixed"][s, t0:t0 + 128, D_ATT:2048], ymix[k][:], f"ymo{k}", reads=[b_ymix[k]])

        chunks = [(s, c) for s in range(nseq) for c in range(16)]
        N = len(chunks)
        load(*chunks[0])
        if N > 1:
            load(*chunks[1])
        prefront(*chunks[0])
        for hg in range(8):
            segEM(*chunks[0], hg)
        for n, (s, c) in enumerate(chunks):
            if n + 2 < N:
                load(*chunks[n + 2])
            if n + 1 < N:
                prefront(*chunks[n + 1])
            for g in range(4):
                back_g(s, c, g)
                if g == 0 and n > 0:
                    tail(*chunks[n - 1])
                if n + 1 < N:
                    segEM(*chunks[n + 1], 2 * g)
                    segEM(*chunks[n + 1], 2 * g + 1)
        tail(*chunks[N - 1])
        P.barrier()


def load_weight(P, T, name, w_bf, nk, buf, key):
    for kc in range(nk):
        P.dma("pool", w_bf[:, kc, :], T[name][kc * 128:(kc + 1) * 128, :], key, writes=[buf])


def phase3a(P, T, nseq, w_out_bf, b_wout):
    nc = P.nc
    with contextlib.ExitStack() as es:
        A = Alloc(nc, es)
        ident = A.sb([128, 128], BF16, "ident3")
        g2 = A.sb([128, D], F32, "g2rep")
        b_c = P.buf("consts3a")
        P.dma("sp", ident[:], T["ident"], "c4", writes=[b_c])
        P.dma("sp", g2[:], T["ln2_g"], "c4", writes=[b_c])
        mx = [A.sb([128, 2048], BF16, f"mx{i}") for i in range(2)]; b_mx = P.bufs(2)
        xin = [A.sb([128, D], F32, f"x3in{i}") for i in range(2)]; b_xin = P.bufs(2)
        mxT = [A.sb([128, 16, 128], BF16, f"mxT{i}") for i in range(2)]; b_mxT = P.bufs(2)
        x1 = [A.sb([128, D], F32, f"x1_{i}") for i in range(2)]; b_x1 = P.bufs(2)
        junk = A.sb([128, D], BF16, "junk4"); b_junk = P.buf()
        ss = [A.sb([128, 1], F32, f"ss4_{i}") for i in range(2)]
        ssv = [A.sb([128, 1], F32, f"ssv4_{i}") for i in range(2)]
        ssl = [A.sb([128, 1], F32, f"ssl4_{i}") for i in range(2)]
        rstd = [A.sb([128, 1], F32, f"rstd4_{i}") for i in range(2)]
        b_ss, b_ssv, b_ssl, b_rstd = P.bufs(2), P.bufs(2), P.bufs(2), P.bufs(2)
        h2 = [A.sb([128, D], BF16, f"h2_{i}") for i in range(2)]; b_h2 = P.bufs(2)
        h2T = [A.sb([128, 8, 512], BF16, f"h2T{i}") for i in range(2)]; b_h2T = P.bufs(2)
        pmt = [A.ps([128, 8, 128], BF16, f"pmt{i}") for i in range(2)]; b_pmt = P.bufs(2)
        po = [A.ps([128, 512], F32, f"po{i}") for i in range(4)]; b_po = P.bufs(4)
        ph = A.ps([128, 8, 128], BF16, "ph"); b_ph = P.buf()
        cnt = {"pmt": 0}
        ntiles = nseq * 16

        def load(n):
            s, t0, k = n // 16, (n % 16) * 128, n % 2
            P.dma("sp", mx[k][:], T["mixed"][s, t0:t0 + 128, :], f"l_mx{k}", writes=[b_mx[k]])
            P.dma("pool", xin[k][:], T["x"][s, t0:t0 + 128, :], f"l_x3{k}", writes=[b_xin[k]])

        def transposes(n):
            k = n % 2
            for half in range(2):
                pi = cnt["pmt"] % 2
                cnt["pmt"] += 1
                for j in range(8):
                    kc = half * 8 + j
                    P.op("pe", lambda e, j=j, kc=kc, pi=pi: e.transpose(
                        out=pmt[pi][:, j, :], in_=mx[k][:, kc * 128:(kc + 1) * 128], identity=ident[:]),
                        reads=[b_mx[k], b_c], writes=[b_pmt[pi]], sig=(j == 7))
                P.op("act", lambda e, half=half, pi=pi: e.copy(out=mxT[k][:, half * 8:(half + 1) * 8, :], in_=pmt[pi][:]),
                     reads=[b_pmt[pi]], writes=[b_mxT[k]])

        load(0)
        if ntiles > 1:
            load(1)
        transposes(0)
        tails = []
        for n in range(ntiles):
            s, t0, k = n // 16, (n % 16) * 128, n % 2
            if n + 1 < ntiles:
                transposes(n + 1)
            for nh in range(2):
                pi = k * 2 + nh
                for kc in range(16):
                    P.op("pe", lambda e, kc=kc, nh=nh, pi=pi: e.matmul(
                        po[pi][:], lhsT=mxT[k][:, kc, :], rhs=w_out_bf[:, kc, nh * 512:(nh + 1) * 512],
                        start=(kc == 0), stop=(kc == 15)), reads=[b_mxT[k], b_wout], writes=[b_po[pi]], sig=(kc == 15))
                P.op("dve", lambda e, nh=nh, pi=pi: e.tensor_tensor(
                    out=x1[k][:, nh * 512:(nh + 1) * 512], in0=po[pi][:], in1=xin[k][:, nh * 512:(nh + 1) * 512], op=ALU.add),
                    reads=[b_po[pi], b_xin[k]], writes=[b_x1[k]])
            P.dma("sp", T["out"][s, t0:t0 + 128, :], x1[k][:], f"x1o{k}", reads=[b_x1[k]])
            if n + 2 < ntiles:
                load(n + 2)
            while tails:
                tails.pop(0)()
            P.op("act", lambda e: e.activation(out=junk[:], in_=x1[k][:], func=AF.Square, accum_out=ss[k][:]),
                 reads=[b_x1[k]], writes=[b_junk, b_ss[k]])
            P.op("dve", lambda e: e.tensor_scalar(out=ssv[k][:], in0=ss[k][:], scalar1=1.0 / D, scalar2=EPS,
                                                  op0=ALU.mult, op1=ALU.add), reads=[b_ss[k]], writes=[b_ssv[k]])
            P.op("act", lambda e: e.activation(out=ssl[k][:], in_=ssv[k][:], func=AF.Ln), reads=[b_ssv[k]], writes=[b_ssl[k]])
            P.op("act", lambda e: e.activation(out=rstd[k][:], in_=ssl[k][:], func=AF.Exp, scale=-0.5),
                 reads=[b_ssl[k]], writes=[b_rstd[k]])
            P.op("dve", lambda e: e.scalar_tensor_tensor(out=h2[k][:], in0=x1[k][:], scalar=rstd[k][:, 0:1], in1=g2[:],
                                                         op0=ALU.mult, op1=ALU.mult),
                 reads=[b_x1[k], b_rstd[k], b_c], writes=[b_h2[k]])
            def tail(n=n, s=s, t0=t0, k=k):
                for kc in range(8):
                    P.op("pe", lambda e, kc=kc: e.transpose(out=ph[:, kc, :], in_=h2[k][:, kc * 128:(kc + 1) * 128],
                                                            identity=ident[:]),
                         reads=[b_h2[k], b_c], writes=[b_ph], sig=(kc == 7))
                gslot = (n // 4) % 2
                tt = n % 4
                P.op("act", lambda e: e.copy(out=h2T[gslot][:, :, tt * 128:(tt + 1) * 128], in_=ph[:]),
                     reads=[b_ph], writes=[b_h2T[gslot]])
                if tt == 3:
                    g0 = t0 - 384
                    P.dma("sp", T["h2T"][s, :, g0:g0 + 512].rearrange("(kc p) t -> p kc t", p=128), h2T[gslot][:],
                          f"h2To{gslot}", reads=[b_h2T[gslot]])
            tails.append(tail)
        while tails:
            tails.pop(0)()
        P.barrier()


def phase3b(P, T, nseq, w_dn_bf, b_wdn):
    nc = P.nc
    NP = D_FF // 128
    with contextlib.ExitStack() as es:
        A = Alloc(nc, es)
        w_up_bf = A.sb([128, 8, 2 * D_FF], BF16, "w_up_bf")
        b_wup = P.buf("w_up")
        load_weight(P, T, "w_up", w_up_bf, 8, b_wup, "w3")
        gf = A.sb([128, D], F32, "gfrep")
        cw = A.sb([128, 2, NP, 3], F32, "cw3")
        cb = A.sb([128, 2, NP], F32, "cb3")
        b_c = P.buf("consts3b")
        P.dma("sp", gf[:], T["lnf_g"], "c5", writes=[b_c])
        P.dma("sp", cw[:], T["ffn_cw"], "c5", writes=[b_c])
        P.dma("sp", cb[:], T["ffn_cb"], "c5", writes=[b_c])
        h2T = [A.sb([128, 8, 256], BF16, f"h2Tb{i}") for i in range(2)]; b_h2T = P.bufs(2)
        x1 = [A.sb([128, D], F32, f"x1b{i}") for i in range(2)]; b_x1 = P.bufs(2)
        gT = [A.sb([128, NP, 256], BF16, f"gT{i}") for i in range(2)]; b_gT = P.bufs(2)
        pre = [A.sb([128, 2, 258], F32, f"preb{i}") for i in range(2)]; b_pre = P.bufs(2)
        accg = [A.sb([128, 256], F32, f"accg{i}") for i in range(2)]; b_accg = P.bufs(2)
        accv = [A.sb([128, 256], F32, f"accv{i}") for i in range(2)]; b_accv = P.bufs(2)
        sgt = [A.sb([128, 256], F32, f"sgt{i}") for i in range(2)]; b_sgt = P.bufs(2)
        hist = A.sb([128, 2, NP, 2], F32, "histb"); b_hist = P.bufs(NP)
        x2 = [A.sb([128, D], F32, f"x2_{i}") for i in range(2)]; b_x2 = P.bufs(2)
        junk = A.sb([128, D], BF16, "junk5"); b_junk = P.buf()
        ss = [A.sb([128, 1], F32, f"ss5_{i}") for i in range(2)]
        ssv = [A.sb([128, 1], F32, f"ssv5_{i}") for i in range(2)]
        ssl = [A.sb([128, 1], F32, f"ssl5_{i}") for i in range(2)]
        rstd = [A.sb([128, 1], F32, f"rstd5_{i}") for i in range(2)]
        b_ss, b_ssv, b_ssl, b_rstd = P.bufs(2), P.bufs(2), P.bufs(2), P.bufs(2)
        pu = [A.ps([128, 2, 256], F32, f"pu{i}") for i in range(3)]; b_pu = P.bufs(3)
        pdn = [A.ps([128, 512], F32, f"pdn{i}") for i in range(4)]; b_pdn = P.bufs(4)
        cnt = {"pu": 0, "pre": 0}
        posts = []
        ngroups = nseq * 8

        def load(n):
            s, t0, k = n // 8, (n % 8) * 256, n % 2
            P.dma("sp", h2T[k][:], T["h2T"][s, :, t0:t0 + 256].rearrange("(kc p) t -> p kc t", p=128), f"l_h2T{k}",
                  writes=[b_h2T[k]])

        def up_pair(n, p):
            s, t0, k = n // 8, (n % 8) * 256, n % 2
            pi = cnt["pu"] % 3
            cnt["pu"] += 1
            for half in range(2):
                col0 = half * D_FF + p * 128
                for kc in range(8):
                    P.op("pe", lambda e, kc=kc, half=half, col0=col0: e.matmul(
                        pu[pi][:, half, :], lhsT=w_up_bf[:, kc, col0:col0 + 128], rhs=h2T[k][:, kc, :],
                        start=(kc == 0), stop=(kc == 7)),
                        reads=[b_wup, b_h2T[k]], writes=[b_pu[pi]], sig=(kc == 7 and half == 1))
            pr = cnt["pre"] % 2
            cnt["pre"] += 1
            P.op("act", lambda e: e.copy(out=pre[pr][:, :, 2:258], in_=pu[pi][:]), reads=[b_pu[pi]], writes=[b_pre[pr]])
            P.op("act", lambda e: e.activation(out=accg[pr][:], in_=pu[pi][:, 0, :], func=AF.Identity,
                                               scale=cw[:, 0, p, 2:3], bias=cb[:, 0, p:p + 1]),
                 reads=[b_pu[pi], b_c], writes=[b_accg[pr]])
            P.op("act", lambda e: e.activation(out=accv[pr][:], in_=pu[pi][:, 1, :], func=AF.Identity,
                                               scale=cw[:, 1, p, 2:3], bias=cb[:, 1, p:p + 1]),
                 reads=[b_pu[pi], b_c], writes=[b_accv[pr]])
            while posts:
                posts.pop(0)()
            if t0 == 0:
                P.op("dve", lambda e: e.memset(pre[pr][:, :, 0:2], 0.0), writes=[b_pre[pr]])
            else:
                P.op("dve", lambda e: e.tensor_copy(out=pre[pr][:, :, 0:2], in_=hist[:, :, p, :]),
                     reads=[b_hist[p]], writes=[b_pre[pr]])
            for kk in (1, 0):
                P.op("dve", lambda e, kk=kk: e.scalar_tensor_tensor(
                    out=accg[pr][:], in0=pre[pr][:, 0, kk:kk + 256], scalar=cw[:, 0, p, kk:kk + 1],
                    in1=accg[pr][:], op0=ALU.mult, op1=ALU.add),
                    reads=[b_pre[pr], b_c, b_accg[pr]], writes=[b_accg[pr]])
                P.op("dve", lambda e, kk=kk: e.scalar_tensor_tensor(
                    out=accv[pr][:], in0=pre[pr][:, 1, kk:kk + 256], scalar=cw[:, 1, p, kk:kk + 1],
                    in1=accv[pr][:], op0=ALU.mult, op1=ALU.add),
                    reads=[b_pre[pr], b_c, b_accv[pr]], writes=[b_accv[pr]])
            P.op("dve", lambda e: e.tensor_copy(out=hist[:, :, p, :], in_=pre[pr][:, :, 256:258]),
                 reads=[b_pre[pr]], writes=[b_hist[p]])
            def post():
                P.op("act", lambda e: e.activation(out=sgt[pr][:], in_=accg[pr][:], func=AF.Silu),
                     reads=[b_accg[pr]], writes=[b_sgt[pr]])
                P.op("pool", lambda e: e.tensor_tensor(out=gT[k][:, p, :], in0=sgt[pr][:], in1=accv[pr][:], op=ALU.mult),
                     reads=[b_sgt[pr], b_accv[pr]], writes=[b_gT[k]])
            posts.append(post)
            if p == NP - 1:
                while posts:
                    posts.pop(0)()

        def down_mm(n, m):
            s, t0, k = n // 8, (n % 8) * 256, n % 2
            tt, nh, fc = m // 44, (m // 22) % 2, m % 22
            pi = tt * 2 + nh
            P.op("pe", lambda e: e.matmul(
                pdn[pi][:], lhsT=gT[k][:, fc, tt * 128:(tt + 1) * 128], rhs=w_dn_bf[:, fc, nh * 512:(nh + 1) * 512],
                start=(fc == 0), stop=(fc == NP - 1)),
                reads=[b_gT[k], b_wdn], writes=[b_pdn[pi]], sig=(fc == NP - 1))
            if fc == NP - 1:
                ti = tt
                P.op("dve", lambda e: e.tensor_tensor(
                    out=x2[ti][:, nh * 512:(nh + 1) * 512], in0=pdn[pi][:], in1=x1[tt][:, nh * 512:(nh + 1) * 512],
                    op=ALU.add), reads=[b_pdn[pi], b_x1[tt]], writes=[b_x2[ti]])
                if nh == 1:
                    P.op("act", lambda e: e.activation(out=junk[:], in_=x2[ti][:], func=AF.Square, accum_out=ss[ti][:]),
                         reads=[b_x2[ti]], writes=[b_junk, b_ss[ti]])
                    P.op("dve", lambda e: e.tensor_scalar(out=ssv[ti][:], in0=ss[ti][:], scalar1=1.0 / D, scalar2=EPS,
                                                          op0=ALU.mult, op1=ALU.add), reads=[b_ss[ti]], writes=[b_ssv[ti]])
                    P.op("act", lambda e: e.activation(out=ssl[ti][:], in_=ssv[ti][:], func=AF.Ln),
                         reads=[b_ssv[ti]], writes=[b_ssl[ti]])
                    P.op("act", lambda e: e.activation(out=rstd[ti][:], in_=ssl[ti][:], func=AF.Exp, scale=-0.5),
                         reads=[b_ssl[ti]], writes=[b_rstd[ti]])
                    P.op("dve", lambda e: e.scalar_tensor_tensor(
                        out=x2[ti][:], in0=x2[ti][:], scalar=rstd[ti][:, 0:1], in1=gf[:], op0=ALU.mult, op1=ALU.mult),
                        reads=[b_x2[ti], b_rstd[ti], b_c], writes=[b_x2[ti]])
                    P.dma("sp", T["out"][s, t0 + tt * 128:t0 + (tt + 1) * 128, :], x2[ti][:], f"outo{ti}",
                          reads=[b_x2[ti]])

        def load_x1(n):
            s, t0 = n // 8, (n % 8) * 256
            for tt in range(2):
                P.dma("sp", x1[tt][:], T["out"][s, t0 + tt * 128:t0 + (tt + 1) * 128, :], f"l_x1b{tt}", writes=[b_x1[tt]])

        load(0)
        for n in range(ngroups + 1):
            if n + 1 < ngroups:
                load(n + 1)
            if n >= 1:
                load_x1(n - 1)
            for p in range(NP):
                if n < ngroups:
                    up_pair(n, p)
                if n >= 1:
                    for m in range(4 * p, 4 * p + 4):
                        down_mm(n - 1, m)
        P.barrier()


SCRATCH = {
    "qk": ([16, 64, S], BF16), "v": ([S, 512], BF16), "xs": ([S, D_SSM], BF16), "btok": ([S, 512], BF16),
    "bT": ([512, S], BF16), "cT": ([512, S], BF16), "sz": ([S, D_SSM], BF16), "dt": ([128, 16, NH_SSM], F32),
    "mixed": ([S, 2048], BF16), "h2T": ([D, S], BF16),
}
CONSTS = {
    "ident": ([128, 128], BF16), "cmask": ([128, 2, 256], BF16), "ownmask": ([128, 16, 8], F32), "kbias": ([128, 8, 2], F32),
    "pastmask": ([128, 16, 8], F32), "dmbase": ([128, 16, 8], F32), "karows": ([9, S], BF16), "qrow": ([8, S], BF16),
    "U": ([128, 128], F32), "Ls": ([128, 128], F32), "ones": ([128, 128], F32), "identf": ([128, 128], F32),
}
PARAMS = {
    "ln1_g": [128, D], "ssm_cw": [128, 20, 4], "ssm_cb": [128, 20], "dt_bias": [128, NH_SSM],
    "attn_g": [128, D_ATT], "a_log": [128, NH_SSM], "d_skip": [128, NH_SSM], "ssm_g": [128, D_SSM],
    "ln2_g": [128, D], "lnf_g": [128, D], "ffn_cw": [128, 2, 22, 3], "ffn_cb": [128, 2, 22], "mix_g": [128, 16],
}


def build_program(nseq=4, phases=(1,), debug=False):
    nc = bass.Bass("TRN2", target_bir_lowering=False)
    T = {}
    T["x"] = nc.dram_tensor("x", [nseq, S, D], F32, kind="ExternalInput").ap()
    T["w_in"] = nc.dram_tensor("w_in", [D, D_IN], F32, kind="ExternalInput").ap()
    T["w_out"] = nc.dram_tensor("w_out", [2048, D], F32, kind="ExternalInput").ap()
    T["w_up"] = nc.dram_tensor("w_up", [D, 2 * D_FF], F32, kind="ExternalInput").ap()
    T["w_down"] = nc.dram_tensor("w_down", [D_FF, D], F32, kind="ExternalInput").ap()
    for k, shp in PARAMS.items():
        T[k] = nc.dram_tensor(k, shp, F32, kind="ExternalInput").ap()
    for k, (shp, dt) in CONSTS.items():
        T[k] = nc.dram_tensor(k, shp, dt, kind="ExternalInput").ap()
    for k, (shp, dt) in SCRATCH.items():
        T[k] = nc.dram_tensor(k, [nseq] + shp, dt, kind="ExternalOutput" if debug else "Internal").ap()
    T["out"] = nc.dram_tensor("out", [nseq, S, D], F32, kind="ExternalOutput").ap()
    with contextlib.ExitStack() as es:
        P = Prog(nc, es)
        if 1 in phases:
            phase1(P, T, nseq)
        with contextlib.ExitStack() as es_dn:
            w_dn_bf = Alloc(nc, es_dn).sb([128, 22, D], BF16, "w_dn_bf")
            b_wdn = P.buf("w_dn")
            with contextlib.ExitStack() as es_out:
                w_out_bf = Alloc(nc, es_out).sb([128, 16, D], BF16, "w_out_bf")
                b_wout = P.buf("w_out")
                if 5 in phases:
                    load_weight(P, T, "w_down", w_dn_bf, 22, b_wdn, "w2d")
                if 2 in phases:
                    phase2a(P, T, nseq, w_out_bf if 4 in phases else None, b_wout)
                if 3 in phases:
                    phase2b(P, T, nseq)
                if 4 in phases:
                    phase3a(P, T, nseq, w_out_bf, b_wout)
            if 5 in phases:
                phase3b(P, T, nseq, w_dn_bf, b_wdn)
        P.finish()
    return nc


def host_consts():
    bf = ml_dtypes.bfloat16
    c = {"ident": np.eye(128, dtype=np.float32).astype(bf)}
    sl = np.array(SLOPES, dtype=np.float64)
    p = np.arange(128)
    krel = (np.arange(2)[None, :] * 128 + p[:, None]).astype(np.float64)
    q = np.arange(256, dtype=np.float64)
    d = q[None, None, :] - krel[:, :, None]
    c["cmask"] = np.where(d >= 0, 0.0, -BIG).astype(np.float32).astype(bf)
    c["kbias"] = (sl[None, :, None] * krel[:, None, :]).astype(np.float32)
    t = np.arange(16)
    j = np.arange(8)
    past = (j[None, :] < (t[:, None] // 2))
    c["pastmask"] = np.ascontiguousarray(np.broadcast_to(np.where(past, 0.0, -1e30)[None], (128, 16, 8))).astype(np.float32)
    own = (j[None, :] == (t[:, None] // 2)).astype(np.float32)
    c["ownmask"] = np.ascontiguousarray(np.broadcast_to(own[None], (128, 16, 8))).astype(np.float32)
    dmb = np.where(past, (t[:, None] // 2 - j[None, :]), 0).astype(np.float32)
    c["dmbase"] = np.ascontiguousarray(np.broadcast_to(dmb[None], (128, 16, 8))).astype(np.float32)
    kr = np.zeros((9, S), dtype=np.float32)
    for jj in range(8):
        kr[jj, jj * 256:(jj + 1) * 256] = 1.0
    kr[8, :] = 1.0
    c["karows"] = kr.astype(bf)
    qrel = (np.arange(S) % 256).astype(np.float64)
    c["qrow"] = (-8.0 * sl[:, None] * qrel[None, :]).astype(np.float32).astype(bf)
    li = np.arange(128)
    c["U"] = (li[:, None] <= li[None, :]).astype(np.float32)
    c["Ls"] = (li[:, None] > li[None, :]).astype(np.float32)
    c["ones"] = np.ones((128, 128), dtype=np.float32)
    c["identf"] = np.eye(128, dtype=np.float32)
    return c


def host_params(inp):
    f = np.float32
    p = {}
    p["ln1_g"] = np.ascontiguousarray(np.broadcast_to(inp["ln1_g"][0][None, :], (128, D))).astype(f)
    cw = inp["ssm_conv_w"][0]
    p["ssm_cw"] = np.ascontiguousarray(cw.reshape(4, 20, 128).transpose(2, 1, 0)).astype(f)
    p["ssm_cb"] = np.ascontiguousarray(inp["ssm_conv_b"][0].reshape(20, 128).T).astype(f)
    p["dt_bias"] = np.ascontiguousarray(np.broadcast_to(inp["dt_bias"][0][None, :], (128, NH_SSM))).astype(f)
    p["attn_g"] = np.ascontiguousarray(np.broadcast_to(inp["attn_norm_g"][0][None, :], (128, D_ATT))).astype(f)
    p["a_log"] = np.ascontiguousarray(np.broadcast_to(inp["a_log"][0][None, :], (128, NH_SSM))).astype(f)
    p["d_skip"] = np.ascontiguousarray(np.broadcast_to(inp["d_skip"][0][None, :], (128, NH_SSM))).astype(f)
    p["ssm_g"] = np.ascontiguousarray(np.broadcast_to(inp["ssm_norm_g"][0][None, :], (128, D_SSM))).astype(f)
    p["ln2_g"] = np.ascontiguousarray(np.broadcast_to(inp["ln2_g"][0][None, :], (128, D))).astype(f)
    p["lnf_g"] = np.ascontiguousarray(np.broadcast_to(inp["lnf_g"][None, :], (128, D))).astype(f)
    fw = inp["ffn_conv_w"][0]
    p["ffn_cw"] = np.ascontiguousarray(fw.reshape(3, 2, 22, 128).transpose(3, 1, 2, 0)).astype(f)
    p["ffn_cb"] = np.ascontiguousarray(inp["ffn_conv_b"][0].reshape(2, 22, 128).transpose(2, 0, 1)).astype(f)
    mg = np.concatenate([inp["attn_norm_g"][0], inp["ssm_norm_g"][0]])
    p["mix_g"] = np.ascontiguousarray(mg.reshape(16, 128).T).astype(f)
    return p


def kernel(**inp):
    inp = {k: np.asarray(v) for k, v in inp.items()}
    nseq = 32 // NCORES
    nc = build_program(nseq=nseq, phases=(1, 2, 3, 4, 5))
    base = {"w_in": np.ascontiguousarray(inp["w_in"][0]), "w_out": np.ascontiguousarray(inp["w_out"][0]),
            "w_up": np.ascontiguousarray(inp["w_up"][0]), "w_down": np.ascontiguousarray(inp["w_down"][0])}
    base.update(host_consts())
    base.update(host_params(inp))
    in_maps = []
    for c in range(NCORES):
        m = dict(base)
        m["x"] = np.ascontiguousarray(inp["x"][c * nseq:(c + 1) * nseq])
        in_maps.append(m)
    res = run_bass_kernel_spmd(nc, in_maps, core_ids=list(range(NCORES)))
    return np.concatenate([r["out"] for r in res.results], axis=0)
```

```python
import contextlib
import numpy as np
import ml_dtypes
import concourse.bass as bass
import concourse.mybir as mybir
from concourse.bass_utils import run_bass_kernel_spmd

F32 = mybir.dt.float32
BF16 = mybir.dt.bfloat16
AF = mybir.ActivationFunctionType
ALU = mybir.AluOpType
AX = mybir.AxisListType

NCORES = 8
S = 2048
D = 1024
D_ATT = 512
D_SSM = 1536
NH_SSM = 24
D_XBC = 2560
D_IN = 5656
D_FF = 2816
EPS = 1e-6
C_Q, C_K, C_V, C_Z, C_XBC, C_DT = 0, 512, 1024, 1536, 3072, 5632
BIG = 30000.0


class Tok:
    __slots__ = ("key", "val")

    def __init__(self, key, val):
        self.key, self.val = key, val


class Buf:
    __slots__ = ("name", "w", "r")

    def __init__(self, name):
        self.name, self.w, self.r = name, {}, {}


class Prog:
    def __init__(self, nc, es):
        self.nc, self.es = nc, es
        self.eng = {"pe": nc.tensor, "act": nc.scalar, "dve": nc.vector, "pool": nc.gpsimd, "sp": nc.sync}
        self.sems, self.cnt, self.seen = {}, {}, {}
        self.pending = {e: [] for e in self.eng}
        self.nbuf = 0
        for e in self.eng:
            self._sem(e)

    def _sem(self, key):
        if key not in self.sems:
            self.sems[key] = self.es.enter_context(self.nc.semaphore("s_" + key))
            self.cnt[key] = 0
        return self.sems[key]

    def buf(self, name=None):
        self.nbuf += 1
        return Buf(name or f"b{self.nbuf}")

    def bufs(self, n, name=None):
        return [self.buf(None if name is None else f"{name}{i}") for i in range(n)]

    def wait(self, eng, tok):
        if tok is None:
            return
        if self.seen.get((eng, tok.key), 0) >= tok.val:
            return
        self.eng[eng].wait_ge(self.sems[tok.key], tok.val)
        self.seen[(eng, tok.key)] = tok.val

    def _deps(self, eng, reads, writes):
        for b in reads:
            for t in b.w.values():
                self.wait(eng, t)
        for b in writes:
            for t in b.w.values():
                self.wait(eng, t)
            for t in b.r.values():
                self.wait(eng, t)

    def _mark(self, tok, reads, writes):
        for b in reads:
            b.r[tok.key] = tok
        for b in writes:
            if b.r:
                b.r = {}
                b.w = {}
            b.w[tok.key] = tok

    def op(self, eng, fn, reads=(), writes=(), sig=True):
        self._deps(eng, reads, writes)
        ins = fn(self.eng[eng])
        if not sig:
            self.pending[eng].append((tuple(reads), tuple(writes)))
            return None
        ins.then_inc(self.sems[eng], 1)
        self.cnt[eng] += 1
        tok = Tok(eng, self.cnt[eng])
        for (r, w) in self.pending[eng]:
            self._mark(tok, r, w)
        self.pending[eng] = []
        self._mark(tok, reads, writes)
        return tok

    def dma(self, q, out, in_, key, reads=(), writes=()):
        self._sem(key)
        self._deps(q, reads, writes)
        self.eng[q].dma_start(out=out, in_=in_).then_inc(self.sems[key], 16)
        self.cnt[key] += 16
        tok = Tok(key, self.cnt[key])
        self._mark(tok, reads, writes)
        return tok

    def barrier(self):
        for e in self.eng:
            assert not self.pending[e], e
        for key in self.sems:
            if self.cnt[key] > 0:
                self.wait("sp", Tok(key, self.cnt[key]))
        self.eng["sp"].sem_inc(self.sems["sp"], 1)
        self.cnt["sp"] += 1
        tok = Tok("sp", self.cnt["sp"])
        for e in self.eng:
            self.wait(e, tok)

    def finish(self):
        for key in self.sems:
            if self.cnt[key] > 0:
                self.wait("sp", Tok(key, self.cnt[key]))


class Alloc:
    def __init__(self, nc, es):
        self.nc, self.es, self.n = nc, es, 0

    def sb(self, shape, dt, name=None):
        self.n += 1
        return self.es.enter_context(self.nc.sbuf_tensor(f"t{self.n}_" + (name or "sb"), list(shape), dt))

    def ps(self, shape, dt, name=None):
        self.n += 1
        return self.es.enter_context(self.nc.psum_tensor(f"p{self.n}_" + (name or "ps"), list(shape), dt))


def phase1(P, T, nseq):
    nc = P.nc
    with contextlib.ExitStack() as es:
        A = Alloc(nc, es)
        ngroups = nseq * 4
        w_bf = A.sb([128, 8, D_IN], BF16, "w_in_bf")
        b_w = P.buf("w_in")
        for kc in range(8):
            P.dma("pool", w_bf[:, kc, :], T["w_in"][kc * 128:(kc + 1) * 128, :], "w1", writes=[b_w])
        ident = A.sb([128, 128], BF16, "ident1")
        g1 = A.sb([128, D], F32, "g1rep")
        cw = A.sb([128, 20, 4], F32, "cw1")
        cb = A.sb([128, 20], F32, "cb1")
        dtb = A.sb([128, NH_SSM], F32, "dtb")
        b_c = P.buf("consts1")
        P.dma("sp", ident[:], T["ident"], "c1", writes=[b_c])
        P.dma("sp", g1[:], T["ln1_g"], "c1", writes=[b_c])
        P.dma("sp", cw[:], T["ssm_cw"], "c1", writes=[b_c])
        P.dma("sp", cb[:], T["ssm_cb"], "c1", writes=[b_c])
        P.dma("sp", dtb[:], T["dt_bias"], "c1", writes=[b_c])

        xin = [A.sb([128, D], F32, f"xin{i}") for i in range(4)]
        b_xin = P.bufs(4, "xin")
        junk = A.sb([128, D], BF16, "junk1")
        b_junk = P.buf("junk")
        ss = A.sb([128, 4], F32, "ss")
        ssv = A.sb([128, 4], F32, "ssv")
        ssl = A.sb([128, 4], F32, "ssl")
        rstd = A.sb([128, 4], F32, "rstd")
        b_ss, b_ssv, b_ssl, b_rstd = P.buf(), P.buf(), P.buf(), P.buf()
        hn = [A.sb([128, D], BF16, f"hn{i}") for i in range(4)]
        b_hn = P.bufs(4, "hn")
        hnT = [A.sb([128, 8, 512], BF16, f"hnT{i}") for i in range(2)]
        b_hnT = P.bufs(2, "hnT")
        pt = [A.ps([128, 8, 128], BF16, f"pt{i}") for i in range(2)]
        b_pt = P.bufs(2, "pt")
        pm = [A.ps([128, 512], F32, f"pm{i}") for i in range(3)]
        b_pm = P.bufs(3, "pm")
        px = [A.ps([128, 8, 128], BF16, f"px{i}") for i in range(2)]
        b_px = P.bufs(2, "px")
        pd = A.ps([128, 512], F32, "pd")
        b_pd = P.buf("pd")
        pre = [A.sb([128, 515], F32, f"pre{i}") for i in range(2)]
        b_pre = P.bufs(2, "pre")
        acc = [A.sb([128, 512], F32, f"acc{i}") for i in range(2)]
        b_acc = P.bufs(2, "acc")
        hist = A.sb([128, 20, 3], F32, "hist")
        b_hist = P.bufs(20, "hist")
        cvo = [A.sb([128, 512], BF16, f"cvo{i}") for i in range(4)]
        b_cvo = P.bufs(4, "cvo")
        xs_tok = A.sb([128, 4, D_SSM], BF16, "xs_tok")
        b_xs_tok = P.buf("xs_tok")
        b_tok = A.sb([128, 4, 512], BF16, "b_tok")
        b_b_tok = P.buf("b_tok")
        qk_ev = [A.sb([128, 512], BF16, f"qkev{i}") for i in range(3)]
        b_qk_ev = P.bufs(3, "qkev")
        v_ev = [A.sb([128, 512], BF16, f"vev{i}") for i in range(2)]
        b_v_ev = P.bufs(2, "vev")
        sz_ev = [A.sb([128, D_SSM], BF16, f"szev{i}") for i in range(2)]
        b_sz_ev = P.bufs(2, "szev")
        ntiles = nseq * 16
        dt_all = A.sb([128, ntiles, NH_SSM], F32, "dt_all")
        dt_e = A.sb([128, ntiles, NH_SSM], F32, "dt_e")
        b_dt_all, b_dt_e = P.buf("dt_all"), P.buf("dt_e")

        cnt = {"pm": 0, "pt": 0, "px": 0, "cvo": 0, "pre": 0, "qk": 0, "v": 0, "sz": 0}

        def norm(gi):
            s, t0 = gi // 4, (gi % 4) * 512
            g = gi % 2
            for tt in range(4):
                P.dma("pool", xin[tt][:], T["x"][s, t0 + tt * 128:t0 + (tt + 1) * 128, :], f"xin{tt}",
                      writes=[b_xin[tt]])
                P.op("act", lambda e, tt=tt: e.activation(out=junk[:], in_=xin[tt][:], func=AF.Square,
                                                         accum_out=ss[:, tt:tt + 1]),
                     reads=[b_xin[tt]], writes=[b_junk, b_ss])
            P.op("dve", lambda e: e.tensor_scalar(out=ssv[:], in0=ss[:], scalar1=1.0 / D, scalar2=EPS,
                                                  op0=ALU.mult, op1=ALU.add), reads=[b_ss], writes=[b_ssv])
            P.op("act", lambda e: e.activation(out=ssl[:], in_=ssv[:], func=AF.Ln), reads=[b_ssv], writes=[b_ssl])
            P.op("act", lambda e: e.activation(out=rstd[:], in_=ssl[:], func=AF.Exp, scale=-0.5),
                 reads=[b_ssl], writes=[b_rstd])
            for tt in range(4):
                h = tt
                P.op("dve", lambda e, tt=tt, h=h: e.scalar_tensor_tensor(
                    out=hn[h][:], in0=xin[tt][:], scalar=rstd[:, tt:tt + 1], in1=g1[:], op0=ALU.mult, op1=ALU.mult),
                    reads=[b_xin[tt], b_rstd, b_c], writes=[b_hn[h]])

        def norm_pe(gi):
            g = gi % 2
            for tt in range(4):
                h = tt
                pi = cnt["pt"] % 2
                cnt["pt"] += 1
                for kc in range(8):
                    P.op("pe", lambda e, pi=pi, kc=kc, h=h: e.transpose(
                        out=pt[pi][:, kc, :], in_=hn[h][:, kc * 128:(kc + 1) * 128], identity=ident[:]),
                        reads=[b_hn[h], b_c], writes=[b_pt[pi]], sig=(kc == 7))
                P.op("act", lambda e, pi=pi, tt=tt, g=g: e.copy(
                    out=hnT[g][:, :, tt * 128:(tt + 1) * 128], in_=pt[pi][:]),
                    reads=[b_pt[pi]], writes=[b_hnT[g]])

        def mm(gi, nxt):
            s, t0 = gi // 4, (gi % 4) * 512
            g = gi % 2
            deferred = []
            units = tok_units(gi)
            post = mm_post_factory(gi, deferred)

            def flush(upto):
                while deferred and deferred[0][0] <= upto:
                    deferred.pop(0)[1]()

            for c in range(28):
                col0 = c * 128 if c < 8 else C_XBC + (c - 8) * 128
                pi = cnt["pm"] % 3
                cnt["pm"] += 1
                for kc in range(8):
                    P.op("pe", lambda e, pi=pi, kc=kc, col0=col0: e.matmul(
                        pm[pi][:], lhsT=w_bf[:, kc, col0:col0 + 128], rhs=hnT[g][:, kc, :],
                        start=(kc == 0), stop=(kc == 7)),
                        reads=[b_w, b_hnT[g]], writes=[b_pm[pi]], sig=(kc == 7))
                if c < 8:
                    qi = cnt["qk"] % 3
                    cnt["qk"] += 1
                    P.op("dve", lambda e, qi=qi, pi=pi: e.tensor_copy(out=qk_ev[qi][:], in_=pm[pi][:]),
                         reads=[b_pm[pi]], writes=[b_qk_ev[qi]])
                    P.dma("sp", T["qk"][s, 2 * c:2 * c + 2, :, t0:t0 + 512].rearrange("h d t -> (h d) t"),
                          qk_ev[qi][:], f"qko{qi}", reads=[b_qk_ev[qi]])
                else:
                    cc = c - 8
                    pr = cnt["pre"] % 2
                    cnt["pre"] += 1
                    P.op("act", lambda e, pr=pr, pi=pi: e.copy(out=pre[pr][:, 3:515], in_=pm[pi][:]),
                         reads=[b_pm[pi]], writes=[b_pre[pr]])
                    if t0 == 0:
                        P.op("dve", lambda e, pr=pr: e.memset(pre[pr][:, 0:3], 0.0), writes=[b_pre[pr]])
                    else:
                        P.op("dve", lambda e, pr=pr, cc=cc: e.tensor_copy(out=pre[pr][:, 0:3], in_=hist[:, cc, :]),
                             reads=[b_hist[cc]], writes=[b_pre[pr]])
                    P.op("act", lambda e, pr=pr, cc=cc, pi=pi: e.activation(
                        out=acc[pr][:], in_=pm[pi][:], func=AF.Identity, scale=cw[:, cc, 3:4], bias=cb[:, cc:cc + 1]),
                        reads=[b_pm[pi], b_c], writes=[b_acc[pr]])
                    for k in (2, 1, 0):
                        P.op("dve", lambda e, pr=pr, cc=cc, k=k: e.scalar_tensor_tensor(
                            out=acc[pr][:], in0=pre[pr][:, k:k + 512], scalar=cw[:, cc, k:k + 1], in1=acc[pr][:],
                            op0=ALU.mult, op1=ALU.add), reads=[b_pre[pr], b_c, b_acc[pr]], writes=[b_acc[pr]])
                    P.op("dve", lambda e, pr=pr, cc=cc: e.tensor_copy(out=hist[:, cc, :], in_=pre[pr][:, 512:515]),
                         reads=[b_pre[pr]], writes=[b_hist[cc]])
                    deferred.append((c + 1, lambda c=c, cc=cc, pr=pr: post(c, cc, pr)))
                if c >= 8:
                    units.pop(0)()
                flush(c)
            flush(10 ** 9)
            assert not units
            if nxt is not None:
                norm_pe(nxt)

        def mm_post_factory(gi, deferred):
            s, t0 = gi // 4, (gi % 4) * 512

            def post(c, cc, pr):
                    ci = cnt["cvo"] % 4
                    cnt["cvo"] += 1
                    P.op("act", lambda e, ci=ci, pr=pr: e.activation(out=cvo[ci][:], in_=acc[pr][:], func=AF.Silu),
                         reads=[b_acc[pr]], writes=[b_cvo[ci]])
                    if cc >= 12:
                        gg = (cc - 12) % 4
                        dst = T["bT"] if cc < 16 else T["cT"]
                        P.dma("sp", dst[s, gg * 128:(gg + 1) * 128, t0:t0 + 512], cvo[ci][:], f"cvo{ci}",
                              reads=[b_cvo[ci]])
                    if cc < 16:
                        def tr(ci=ci, cc=cc):
                            xi = cnt["px"] % 2
                            cnt["px"] += 1
                            for tt in range(4):
                                P.op("pe", lambda e, xi=xi, tt=tt, ci=ci: e.transpose(
                                    out=px[xi][:, tt, :], in_=cvo[ci][:, tt * 128:(tt + 1) * 128], identity=ident[:]),
                                    reads=[b_cvo[ci], b_c], writes=[b_px[xi]], sig=(tt == 3))
                            deferred.append((c + 4, lambda: tr_ev(xi, cc)))

                        def tr_ev(xi, cc):
                            if cc < 12:
                                P.op("act", lambda e, xi=xi, cc=cc: e.copy(
                                    out=xs_tok[:, :, cc * 128:(cc + 1) * 128], in_=px[xi][:, 0:4, :]),
                                    reads=[b_px[xi]], writes=[b_xs_tok])
                                if cc == 11:
                                    P.dma("sp", T["xs"][s, t0:t0 + 512, :].rearrange("(tt p) c -> p tt c", p=128),
                                          xs_tok[:], "xso", reads=[b_xs_tok])
                            else:
                                gg = cc - 12
                                P.op("act", lambda e, xi=xi, gg=gg: e.copy(
                                    out=b_tok[:, :, gg * 128:(gg + 1) * 128], in_=px[xi][:, 0:4, :]),
                                    reads=[b_px[xi]], writes=[b_b_tok])
                                if gg == 3:
                                    P.dma("sp", T["btok"][s, t0:t0 + 512, :].rearrange("(tt p) c -> p tt c", p=128),
                                          b_tok[:], "bto", reads=[b_b_tok])
                        deferred.append((c + 3, tr))
            return post

        def tok_units(gi):
            s, t0 = gi // 4, (gi % 4) * 512
            g = gi % 2
            units = []
            for tt in range(4):
                units += tok_tile(gi, s, t0, g, tt)
            return units

        def tok_tile(gi, s, t0, g, tt):
            tsl = slice(tt * 128, (tt + 1) * 128)
            tile_idx = gi * 4 + tt
            st = {}

            def u_v():
                pi = cnt["pm"] % 3
                cnt["pm"] += 1
                for kc in range(8):
                    P.op("pe", lambda e, pi=pi, kc=kc: e.matmul(
                        pm[pi][:], lhsT=hnT[g][:, kc, tsl], rhs=w_bf[:, kc, C_V:C_V + 512],
                        start=(kc == 0), stop=(kc == 7)), reads=[b_w, b_hnT[g]], writes=[b_pm[pi]], sig=(kc == 7))
                vi = cnt["v"] % 2
                cnt["v"] += 1
                P.op("dve", lambda e, vi=vi, pi=pi: e.tensor_copy(out=v_ev[vi][:], in_=pm[pi][:]),
                     reads=[b_pm[pi]], writes=[b_v_ev[vi]])
                P.dma("sp", T["v"][s, t0 + tt * 128:t0 + (tt + 1) * 128, :], v_ev[vi][:], f"vo{vi}",
                      reads=[b_v_ev[vi]])
                st["zi"] = cnt["sz"] % 2
                cnt["sz"] += 1

            def u_z(j):
                zi = st["zi"]
                pi = cnt["pm"] % 3
                cnt["pm"] += 1
                for kc in range(8):
                    P.op("pe", lambda e, pi=pi, kc=kc, j=j: e.matmul(
                        pm[pi][:], lhsT=hnT[g][:, kc, tsl], rhs=w_bf[:, kc, C_Z + j * 512:C_Z + (j + 1) * 512],
                        start=(kc == 0), stop=(kc == 7)),
                        reads=[b_w, b_hnT[g]], writes=[b_pm[pi]], sig=(kc == 7))
                P.op("act", lambda e, zi=zi, pi=pi, j=j: e.activation(
                    out=sz_ev[zi][:, j * 512:(j + 1) * 512], in_=pm[pi][:], func=AF.Silu),
                    reads=[b_pm[pi]], writes=[b_sz_ev[zi]])
                if j == 2:
                    P.dma("sp", T["sz"][s, t0 + tt * 128:t0 + (tt + 1) * 128, :], sz_ev[zi][:], f"szo{zi}",
                          reads=[b_sz_ev[zi]])

            def u_dt():
                for kc in range(8):
                    P.op("pe", lambda e, kc=kc: e.matmul(
                        pd[:, 0:NH_SSM], lhsT=hnT[g][:, kc, tsl], rhs=w_bf[:, kc, C_DT:C_DT + NH_SSM],
                        start=(kc == 0), stop=(kc == 7)), reads=[b_w, b_hnT[g]], writes=[b_pd], sig=(kc == 7))
                P.op("dve", lambda e, tile_idx=tile_idx: e.tensor_tensor(
                    out=dt_all[:, tile_idx, :], in0=pd[:, 0:NH_SSM], in1=dtb[:], op=ALU.add),
                    reads=[b_pd, b_c], writes=[b_dt_all])

            return [u_v, lambda: u_z(0), lambda: u_z(1), lambda: u_z(2), u_dt]

        norm(0)
        norm_pe(0)
        for gi in range(ngroups):
            if gi + 1 < ngroups:
                norm(gi + 1)
            mm(gi, gi + 1 if gi + 1 < ngroups else None)
        P.op("dve", lambda e: e.tensor_scalar_min(out=dt_all[:], in0=dt_all[:], scalar1=60.0),
             reads=[b_dt_all], writes=[b_dt_all])
        P.op("act", lambda e: e.activation(out=dt_e[:], in_=dt_all[:], func=AF.Exp), reads=[b_dt_all], writes=[b_dt_e])
        P.op("act", lambda e: e.activation(out=dt_all[:], in_=dt_e[:], func=AF.Ln, bias=1.0, scale=1.0),
             reads=[b_dt_e], writes=[b_dt_all])
        for s in range(nseq):
            P.dma("sp", T["dt"][s], dt_all[:, s * 16:(s + 1) * 16, :], "dto",
                  reads=[b_dt_all])
        P.barrier()


SLOPES = [2.0 ** (-(h + 1)) for h in range(8)]


def phase2a(P, T, nseq, w_out_bf=None, b_wout=None):
    nc = P.nc
    with contextlib.ExitStack() as es:
        A = Alloc(nc, es)
        ident = A.sb([128, 128], BF16, "ident2")
        cmask = A.sb([128, 2, 256], BF16, "cmask")
        ownmask = A.sb([128, 16, 8], F32, "ownmask")
        kbias = A.sb([128, 8, 2], F32, "kbias")
        pastmask = A.sb([128, 16, 8], F32, "pastmask")
        dmbase = A.sb([128, 16, 8], F32, "dmbase")
        ag = A.sb([128, D_ATT], F32, "ag")
        b_c = P.buf("consts2")
        P.dma("sp", ident[:], T["ident"], "c2", writes=[b_c])
        P.dma("sp", cmask[:], T["cmask"], "c2", writes=[b_c])
        P.dma("sp", ownmask[:], T["ownmask"], "c2", writes=[b_c])
        P.dma("sp", kbias[:], T["kbias"], "c2", writes=[b_c])
        P.dma("sp", pastmask[:], T["pastmask"], "c2", writes=[b_c])
        P.dma("sp", dmbase[:], T["dmbase"], "c2", writes=[b_c])
        P.dma("sp", ag[:], T["attn_g"], "c2", writes=[b_c])
        qa = [A.sb([128, S], BF16, f"qa{i}") for i in range(2)]
        ka = [A.sb([128, S], BF16, f"ka{i}") for i in range(2)]
        b_qa, b_ka = P.bufs(2, "qa"), P.bufs(2, "ka")
        for i in range(2):
            P.op("pool", lambda e, i=i: e.memset(qa[i][64:128, :], 0.0), writes=[b_qa[i]])
            P.op("pool", lambda e, i=i: e.memset(ka[i][64:128, :], 0.0), writes=[b_ka[i]])
        for i in range(2):
            P.dma("sp", ka[i][64:73, :], T["karows"], f"kld{i}", writes=[b_ka[i]])
        vh = [A.sb([128, 16, 65], BF16, f"vh{i}") for i in range(2)]
        b_vh = P.bufs(2, "vh")
        for i in range(2):
            P.op("pool", lambda e, i=i: e.memset(vh[i][:, :, 64:65], 1.0), writes=[b_vh[i]])
        ksum = A.sb([64, 8], F32, "ksum")
        kmT = A.sb([64, 8], BF16, "kmT")
        b_ksum, b_kmT = P.buf(), P.buf()
        Gm = A.sb([128, 16, 8], F32, "Gm")
        top8 = A.sb([128, 16, 8], F32, "top8")
        selm = A.sb([128, 16, 8], F32, "selm")
        selt = A.sb([128, 16, 8], F32, "selt")
        pen = A.sb([128, 16, 8], BF16, "pen")
        b_Gm, b_top8, b_selm, b_selt, b_pen = P.buf(), P.buf(), P.buf(), P.buf(), P.buf()
        PT = [A.sb([128, 256], BF16, f"PT{i}") for i in range(4)]
        b_PT = P.bufs(4, "PT")
        att_tok2 = [A.sb([128, 16, D_ATT], F32, f"att_tok{i}") for i in range(2)]
        b_att2 = P.bufs(2, "att_tok")
        rden = A.sb([128, 2, 2], F32, "rden")
        b_rden = P.bufs(2, "rden")
        junk = A.sb([128, D_ATT], BF16, "junk2")
        b_junk = P.buf()
        ss = A.sb([128, 16], F32, "ss2")
        ssv = A.sb([128, 16], F32, "ssv2")
        ssl = A.sb([128, 16], F32, "ssl2")
        rstd = A.sb([128, 16], F32, "rstd2")
        b_ss, b_ssv, b_ssl, b_rstd = P.buf(), P.buf(), P.buf(), P.buf()
        atn = [A.sb([128, D_ATT], BF16, f"atn{i}") for i in range(2)]
        b_atn = P.bufs(2, "atn")
        pG_ = A.ps([128, 64, 8], F32, "pG")
        pG = pG_[:, 0:16, :]
        b_pG = P.buf("pG")
        ppen = A.ps([8, 1024], BF16, "ppen")
        b_ppen = P.buf("ppen")
        pS = [A.ps([128, 512], F32, f"pS{i}") for i in range(4)]
        b_pS = P.bufs(4, "pS")
        pacc = [A.ps([128, 4, 128], F32, f"pacc{i}") for i in range(2)]
        b_pacc = P.bufs(2, "pacc")
        cnt = {"S": 0, "pen": 0, "atn": 0}

        def load_v(s, h):
            b = h % 2
            P.dma("sp", vh[b][:, :, 0:64], T["v"][s, :, h * 64:(h + 1) * 64].rearrange("(kt p) d -> p kt d", p=128),
                  f"vld{b}", writes=[b_vh[b]])

        def prep1(s, h):
            b = h % 2
            load_v(s, h)
            P.dma("sp", qa[b][0:64, :], T["qk"][s, h], f"qld{b}", writes=[b_qa[b]])
            P.dma("sp", qa[b][72:73, :], T["qrow"][h:h + 1, :], f"qld{b}", writes=[b_qa[b]])
            P.dma("sp", ka[b][0:64, :], T["qk"][s, 8 + h], f"kld{b}", writes=[b_ka[b]])
            P.op("dve", lambda e: e.tensor_reduce(out=ksum[:], in_=ka[b][0:64, :].rearrange("p (j k) -> p j k", k=256),
                                                  axis=AX.X, op=ALU.add), reads=[b_ka[b]], writes=[b_ksum])
            P.op("dve", lambda e: e.tensor_scalar(out=kmT[:], in0=ksum[:], scalar1=1.0 / 256, scalar2=None, op0=ALU.mult),
                 reads=[b_ksum], writes=[b_kmT])
            for t in range(16):
                P.op("pe", lambda e, t=t: e.matmul(pG_[:, t, :], lhsT=qa[b][0:64, t * 128:(t + 1) * 128], rhs=kmT[:],
                                                   start=True, stop=True),
                     reads=[b_qa[b], b_kmT], writes=[b_pG], sig=(t == 15))
            P.op("dve", lambda e: e.tensor_tensor(out=Gm[:], in0=pG, in1=pastmask[:], op=ALU.add),
                 reads=[b_pG, b_c], writes=[b_Gm])
            for t in range(16):
                P.op("dve", lambda e, t=t: e.max(out=top8[:, t, :], in_=Gm[:, t, :]), reads=[b_Gm], writes=[b_top8])
            P.op("dve", lambda e: e.tensor_tensor(out=selm[:], in0=Gm[:], in1=top8[:, :, 2:3].to_broadcast([128, 16, 8]),
                                                  op=ALU.is_ge), reads=[b_Gm, b_top8], writes=[b_selm])
            P.op("dve", lambda e: e.tensor_tensor(out=selm[:], in0=selm[:], in1=ownmask[:], op=ALU.max),
                 reads=[b_selm, b_c], writes=[b_selm])
            P.op("dve", lambda e: e.tensor_scalar(out=selt[:], in0=selm[:], scalar1=-1.0, scalar2=BIG,
                                                  op0=ALU.add, op1=ALU.mult), reads=[b_selm], writes=[b_selt])
            P.op("dve", lambda e: e.scalar_tensor_tensor(out=pen[:], in0=dmbase[:], scalar=-2048.0 * SLOPES[h],
                                                         in1=selt[:], op0=ALU.mult, op1=ALU.add),
                 reads=[b_selt, b_c], writes=[b_pen])

        def prep2(s, h):
            b = h % 2
            for grp in range(4):
                for j in range(4):
                    t = grp * 4 + j
                    P.op("pe", lambda e, t=t, j=j: e.transpose(
                        out=ppen[:, j * 128:(j + 1) * 128], in_=pen[:, t, :], identity=ident[:]),
                        reads=[b_pen, b_c], writes=[b_ppen], sig=(j == 3))
                P.op("dve", lambda e, grp=grp: e.tensor_copy(out=qa[b][64:72, grp * 512:(grp + 1) * 512],
                                                            in_=ppen[:, 0:512]),
                     reads=[b_ppen], writes=[b_qa[b]])

        def qk(s, h, i, kt):
            b = h % 2
            si = cnt["S"] % 4
            cnt["S"] += 1
            Sp = pS[si][:, 0:256]
            qs = slice(i * 256, (i + 1) * 256)
            ks = slice(kt * 128, (kt + 1) * 128)
            diag = (kt // 2 == i)
            P.op("pe", lambda e: e.matmul(Sp, lhsT=ka[b][:, ks], rhs=qa[b][:, qs], start=True, stop=not diag),
                 reads=[b_ka[b], b_qa[b]], writes=[b_pS[si]], sig=not diag)
            if diag:
                P.op("pe", lambda e: e.matmul(Sp, lhsT=ident[:], rhs=cmask[:, kt % 2, :], start=False, stop=True),
                     reads=[b_c], writes=[b_pS[si]])
            P.op("act", lambda e: e.activation(out=PT[si][:], in_=Sp, func=AF.Exp, scale=0.125,
                                               bias=kbias[:, h, kt % 2:kt % 2 + 1]),
                 reads=[b_pS[si], b_c], writes=[b_PT[si]])
            return si

        def pv(s, h, i, kt, si):
            last = 2 * i + 1
            a = i % 2
            for u in range(2):
                P.op("pe", lambda e, u=u: e.matmul(pacc[a][:, u, 0:65], lhsT=PT[si][:, u * 128:(u + 1) * 128],
                                                   rhs=vh[h % 2][:, kt, :], start=(kt == 0 and u == 0), stop=(kt == last),
                                                   skip_group_check=True),
                     reads=[b_PT[si], b_vh[h % 2]], writes=[b_pacc[a]], sig=(u == 1))
            if kt == last:
                P.op("dve", lambda e: e.reciprocal(out=rden[:, a, :], in_=pacc[a][:, 0:2, 64]),
                     reads=[b_pacc[a]], writes=[b_rden[a]])
                for u in range(2):
                    P.op("dve", lambda e, u=u: e.tensor_scalar(
                        out=att_tok2[s % 2][:, 2 * i + u, h * 64:(h + 1) * 64], in0=pacc[a][:, u, 0:64],
                        scalar1=rden[:, a, u:u + 1], scalar2=None, op0=ALU.mult),
                        reads=[b_pacc[a], b_rden[a]], writes=[b_att2[s % 2]])

        def main(s, h, hooks):
            steps = [(i, kt) for i in range(8) for kt in range(2 * i + 2)]
            look = 3
            sis = {}
            for n in range(min(look, len(steps))):
                sis[n] = qk(s, h, *steps[n])
            for n, (i, kt) in enumerate(steps):
                if n + look < len(steps):
                    sis[n + look] = qk(s, h, *steps[n + look])
                pv(s, h, i, kt, sis.pop(n))
                if kt == 2 * i + 1 and i in hooks:
                    hooks[i]()

        def finish_seq(s):
            att_tok, b_att = att_tok2[s % 2], b_att2[s % 2]
            for t in range(16):
                P.op("act", lambda e, t=t: e.activation(out=junk[:], in_=att_tok[:, t, :], func=AF.Square,
                                                       accum_out=ss[:, t:t + 1]), reads=[b_att], writes=[b_junk, b_ss])
            P.op("dve", lambda e: e.tensor_scalar(out=ssv[:], in0=ss[:], scalar1=1.0 / D_ATT, scalar2=EPS,
                                                  op0=ALU.mult, op1=ALU.add), reads=[b_ss], writes=[b_ssv])
            P.op("act", lambda e: e.activation(out=ssl[:], in_=ssv[:], func=AF.Ln), reads=[b_ssv], writes=[b_ssl])
            P.op("act", lambda e: e.activation(out=rstd[:], in_=ssl[:], func=AF.Exp, scale=-0.5),
                 reads=[b_ssl], writes=[b_rstd])
            for t in range(16):
                ai = cnt["atn"] % 2
                cnt["atn"] += 1
                P.op("dve", lambda e, t=t, ai=ai: e.tensor_scalar(
                    out=atn[ai][:], in0=att_tok[:, t, :], scalar1=rstd[:, t:t + 1], scalar2=None, op0=ALU.mult),
                    reads=[b_att, b_rstd], writes=[b_atn[ai]])
                P.dma("sp", T["mixed"][s, t * 128:(t + 1) * 128, 0:D_ATT], atn[ai][:], f"atno{ai}", reads=[b_atn[ai]])

        wq = []
        if w_out_bf is not None:
            gcol = A.sb([128, 16], F32, "gcol")
            b_gcol = P.buf("gcol")
            P.dma("sp", gcol[:], T["mix_g"], "gcol", writes=[b_gcol])
            stage = [A.sb([128, D], F32, f"wstage{i}") for i in range(2)]
            b_stage = P.bufs(2)

            def wload(kc):
                si = kc % 2
                P.dma("sp", stage[si][:], T["w_out"][kc * 128:(kc + 1) * 128, :], f"wst{si}", writes=[b_stage[si]])
                P.op("act", lambda e: e.activation(out=w_out_bf[:, kc, :], in_=stage[si][:], func=AF.Copy,
                                                   scale=gcol[:, kc:kc + 1]),
                     reads=[b_stage[si], b_gcol], writes=[b_wout])
            wq = [lambda kc=kc: wload(kc) for kc in range(16)]

        heads = [(s, h) for s in range(nseq) for h in range(8)]
        prep1(*heads[0])
        prep2(*heads[0])
        pending_fin = []
        for n, (s, h) in enumerate(heads):
            hooks = {}
            extra = {}
            if wq:
                for i in (0, 2, 4, 6):
                    if wq:
                        extra[i] = wq.pop(0)
            if pending_fin:
                extra[1] = pending_fin.pop(0)
            nxt = heads[n + 1] if n + 1 < len(heads) else None

            def mk(i, nxt=nxt, extra=extra):
                def f():
                    if i in extra:
                        extra[i]()
                    if nxt is not None and i == 3:
                        prep1(*nxt)
                    if nxt is not None and i == 5:
                        prep2(*nxt)
                return f
            for i in range(8):
                hooks[i] = mk(i)
            main(s, h, hooks)
            if h == 7:
                pending_fin.append(lambda s=s: finish_seq(s))
        while wq:
            wq.pop(0)()
        while pending_fin:
            pending_fin.pop(0)()
        P.barrier()


def phase2b(P, T, nseq):
    nc = P.nc
    with contextlib.ExitStack() as es:
        A = Alloc(nc, es)
        U = A.sb([128, 128], F32, "U")
        Ls = A.sb([128, 128], F32, "Ls")
        ones = A.sb([128, 128], F32, "ones")
        alog = A.sb([128, NH_SSM], F32, "alog")
        arep = A.sb([128, NH_SSM], F32, "arep")
        dsk = A.sb([128, NH_SSM], F32, "dsk")
        sg = A.sb([128, D_SSM], F32, "sg")
        b_c = P.buf("consts2b")
        P.dma("sp", U[:], T["U"], "c3", writes=[b_c])
        P.dma("sp", Ls[:], T["Ls"], "c3", writes=[b_c])
        P.dma("sp", ones[:], T["ones"], "c3", writes=[b_c])
        P.dma("sp", alog[:], T["a_log"], "c3", writes=[b_c])
        P.dma("sp", dsk[:], T["d_skip"], "c3", writes=[b_c])
        P.dma("sp", sg[:], T["ssm_g"], "c3", writes=[b_c])
        b_a = P.buf("arep")
        P.op("act", lambda e: e.activation(out=arep[:], in_=alog[:], func=AF.Exp), reads=[b_c], writes=[b_a])
        P.op("dve", lambda e: e.tensor_scalar(out=arep[:], in0=arep[:], scalar1=-1.0, scalar2=None, op0=ALU.mult),
             reads=[b_a], writes=[b_a])

        def two(shape, dt, name, n=2):
            return [A.sb(shape, dt, f"{name}{i}") for i in range(n)]
        xs = two([128, D_SSM], BF16, "xs", 3); b_xs = P.bufs(3)
        bt = two([128, 512], BF16, "bt", 3); b_bt = P.bufs(3)
        bT = two([128, 4, 128], BF16, "bT", 3); b_bT = P.bufs(3)
        cT = two([128, 4, 128], BF16, "cT", 3); b_cT = P.bufs(3)
        sz = two([128, D_SSM], BF16, "sz", 3); b_sz = P.bufs(3)
        dt = two([128, NH_SSM], F32, "dt", 3); b_dt = P.bufs(3)
        adt = two([128, NH_SSM], F32, "adt"); b_adt = P.bufs(2)
        ecs3 = two([128, 72], F32, "ecs3"); b_ecs3 = P.bufs(2)
        Dm = two([128, NH_SSM, 128], F32, "Dm"); b_Dm = P.bufs(2)
        xdtw = two([128, D_SSM], BF16, "xdtw"); b_xdtw = P.bufs(2)
        cbm = two([128, 4, 128], BF16, "cbm"); b_cbm = P.bufs(2)
        lndt = two([128, NH_SSM], F32, "lndt"); b_lndt = P.bufs(2)
        dtw = two([128, NH_SSM], F32, "dtw"); b_dtw = P.bufs(2)
        M = two([128, NH_SSM, 128], BF16, "M")
        b_M = [P.bufs(8), P.bufs(8)]
        E = two([128, 3, 128], BF16, "E"); b_E = P.bufs(2)
        hst = A.sb([128, D_SSM], F32, "hst"); b_h = P.bufs(4, "h")
        hbf = A.sb([128, D_SSM], BF16, "hbf"); b_hbf = P.bufs(4, "hbf")
        yo = A.sb([128, 384], F32, "yo"); b_yo = P.buf()
        yall2 = two([128, D_SSM], F32, "yall"); b_y2 = [P.bufs(4), P.bufs(4)]
        junk = A.sb([128, 384], BF16, "junk3"); b_junk = P.buf()
        ss2 = two([128, 4], F32, "ss3"); b_ss2 = P.bufs(2); ssv = A.sb([128, 4], F32, "ssv3")
        ssl = A.sb([128, 4], F32, "ssl3"); rstd = A.sb([128, 4], F32, "rstd3")
        b_ssv, b_ssl, b_rstd = P.buf(), P.buf(), P.buf()
        ymix = two([128, D_SSM], BF16, "ymix"); b_ymix = P.bufs(2)
        pcb = A.ps([128, 4, 128], F32, "pcb"); b_pcb = P.buf()
        pseg = [A.ps([128, 512], F32, f"pseg{i}") for i in range(2)]; b_pseg = P.bufs(2)
        pyd2 = [A.ps([128, 512], F32, f"pyd{i}") for i in range(2)]; b_pyd2 = P.bufs(2)
        pyo2 = [A.ps([128, 512], F32, f"pyo{i}") for i in range(2)]; b_pyo2 = P.bufs(2)
        pst = A.ps([128, 512], F32, "pst"); b_pst = P.buf()
        pc = pst[:, 384:512]; b_pc = b_pst
        cnt = {"seg": 0}

        def bc(ap, shape, axis):
            return ap.unsqueeze(axis).to_broadcast(shape)

        identf = A.sb([128, 128], F32, "identf")
        dfull = A.sb([128, 128], F32, "dfull")
        dhif = A.sb([128, 128], F32, "dhif")
        Dhi = A.sb([128, NH_SSM, 128], BF16, "Dhi")
        Dlo = A.sb([128, NH_SSM, 128], BF16, "Dlo")
        b_idf, b_dfull, b_dhif, b_Dhl = P.buf(), P.buf(), P.buf(), P.buf()
        P.dma("sp", identf[:], T["identf"], "c3i", writes=[b_idf])
        for h in range(NH_SSM):
            P.op("dve", lambda e, h=h: e.tensor_scalar(out=dfull[:], in0=identf[:], scalar1=dsk[:, h:h + 1], scalar2=None,
                                                       op0=ALU.mult), reads=[b_idf, b_c], writes=[b_dfull])
            P.op("dve", lambda e, h=h: e.tensor_copy(out=Dhi[:, h, :], in_=dfull[:]), reads=[b_dfull], writes=[b_Dhl])
            P.op("dve", lambda e, h=h: e.tensor_copy(out=dhif[:], in_=Dhi[:, h, :]), reads=[b_Dhl], writes=[b_dhif])
            P.op("dve", lambda e, h=h: e.tensor_tensor(out=Dlo[:, h, :], in0=dfull[:], in1=dhif[:], op=ALU.subtract),
                 reads=[b_dfull, b_dhif], writes=[b_Dhl])

        def load(s, c):
            k = (s * 16 + c) % 3
            t0 = c * 128
            P.dma("sp", xs[k][:], T["xs"][s, t0:t0 + 128, :], f"l_xs{k}", writes=[b_xs[k]])
            P.dma("sp", bt[k][:], T["btok"][s, t0:t0 + 128, :], f"l_bt{k}", writes=[b_bt[k]])
            P.dma("sp", bT[k][:], T["bT"][s, :, t0:t0 + 128].rearrange("(g n) l -> n g l", n=128), f"l_bT{k}",
                  writes=[b_bT[k]])
            P.dma("sp", cT[k][:], T["cT"][s, :, t0:t0 + 128].rearrange("(g n) l -> n g l", n=128), f"l_cT{k}",
                  writes=[b_cT[k]])
            P.dma("sp", sz[k][:], T["sz"][s, t0:t0 + 128, :], f"l_sz{k}", writes=[b_sz[k]])
            P.dma("sp", dt[k][:], T["dt"][s, :, c, :], f"l_dt{k}", writes=[b_dt[k]])

        def prefront(s, c):
            k = (s * 16 + c) % 2
            kl = (s * 16 + c) % 3
            P.op("dve", lambda e: e.tensor_tensor(out=adt[k][:], in0=dt[kl][:], in1=arep[:], op=ALU.mult),
                 reads=[b_dt[kl], b_a], writes=[b_adt[k]])
            for j, lm in enumerate((U, Ls, ones)):
                P.op("pe", lambda e, j=j, lm=lm: e.matmul(pc[:, j * 24:(j + 1) * 24], lhsT=lm[:], rhs=adt[k][:],
                                                          start=True, stop=True),
                     reads=[b_c, b_adt[k]], writes=[b_pc], sig=(j == 2))
            P.op("act", lambda e: e.activation(out=ecs3[k][:], in_=pc[:, 0:72], func=AF.Exp),
                 reads=[b_pc], writes=[b_ecs3[k]])
            P.op("act", lambda e: e.activation(out=lndt[k][:], in_=dt[kl][:], func=AF.Ln), reads=[b_dt[kl]], writes=[b_lndt[k]])
            P.op("pool", lambda e: e.tensor_tensor(out=Dm[k][:], in0=bc(U[:], [128, NH_SSM, 128], 1),
                                                   in1=bc(adt[k][:], [128, NH_SSM, 128], 2), op=ALU.mult),
                 reads=[b_c, b_adt[k]], writes=[b_Dm[k]])
            P.op("dve", lambda e: e.tensor_tensor(out=dtw[k][:], in0=dt[kl][:], in1=ecs3[k][:, 24:48], op=ALU.mult),
                 reads=[b_dt[kl], b_ecs3[k]], writes=[b_dtw[k]])
            P.op("pool", lambda e: e.tensor_tensor(out=xdtw[k][:].rearrange("p (h d) -> p h d", d=64),
                                                   in0=xs[kl][:].rearrange("p (h d) -> p h d", d=64),
                                                   in1=bc(dtw[k][:], [128, NH_SSM, 64], 2), op=ALU.mult),
                 reads=[b_xs[kl], b_dtw[k]], writes=[b_xdtw[k]])
            for g in range(4):
                P.op("pe", lambda e, g=g: e.matmul(pcb[:, g, :], lhsT=bT[kl][:, g, :], rhs=cT[kl][:, g, :],
                                                   start=True, stop=True),
                     reads=[b_bT[kl], b_cT[kl]], writes=[b_pcb], sig=(g == 3))
            P.op("dve", lambda e: e.tensor_tensor(out=cbm[k][:], in0=pcb[:], in1=bc(U[:], [128, 4, 128], 1), op=ALU.mult),
                 reads=[b_pcb, b_c], writes=[b_cbm[k]])

        def segEM(s, c, hg):
            k = (s * 16 + c) % 2
            if True:
                g = hg // 2
                si = cnt["seg"] % 2
                ei = cnt["seg"] % 2
                cnt["seg"] += 1
                P.op("pe", lambda e, hg=hg, si=si: e.matmul(
                    pseg[si][:, 0:384], lhsT=Ls[:], rhs=Dm[k][:, hg * 3:(hg + 1) * 3, :], start=True, stop=True),
                    reads=[b_c, b_Dm[k]], writes=[b_pseg[si]])
                for j in range(3):
                    h = hg * 3 + j
                    P.op("act", lambda e, si=si, ei=ei, j=j, h=h: e.activation(
                        out=E[ei][:, j, :], in_=pseg[si][:, j * 128:(j + 1) * 128], func=AF.Exp, bias=lndt[k][:, h:h + 1]),
                        reads=[b_pseg[si], b_lndt[k]], writes=[b_E[ei]])
                P.op("dve", lambda e, hg=hg, ei=ei, g=g: e.tensor_tensor(
                    out=M[k][:, hg * 3:(hg + 1) * 3, :], in0=E[ei][:], in1=bc(cbm[k][:, g, :], [128, 3, 128], 1),
                    op=ALU.mult), reads=[b_E[ei], b_cbm[k]], writes=[b_M[k][hg]])

        def back_g(s, c, g):
            k = (s * 16 + c) % 2
            kl = (s * 16 + c) % 3
            yall, b_y, ss, b_ss = yall2[k], b_y2[k], ss2[k], b_ss2[k]
            pyd, b_pyd, pyo, b_pyo = pyd2[g % 2], b_pyd2[g % 2], pyo2[g % 2], b_pyo2[g % 2]
            if True:
                gs = slice(g * 384, (g + 1) * 384)
                hs = slice(g * 6, (g + 1) * 6)
                for kk in range(6):
                    h = g * 6 + kk
                    for mi, lm in enumerate((M[k], Dhi, Dlo)):
                        P.op("pe", lambda e, kk=kk, h=h, mi=mi, lm=lm: e.matmul(
                            pyd[:, kk * 64:(kk + 1) * 64], lhsT=lm[:, h, :], rhs=xs[kl][:, h * 64:(h + 1) * 64],
                            start=(mi == 0), stop=(mi == 2)),
                            reads=[b_M[k][h // 3], b_xs[kl], b_Dhl], writes=[b_pyd], sig=(kk == 5 and mi == 2))
                if c > 0:
                    P.op("pe", lambda e: e.matmul(pyo[:, 0:384], lhsT=cT[kl][:, g, :], rhs=hbf[:, gs], start=True, stop=True),
                         reads=[b_cT[kl], b_hbf[g]], writes=[b_pyo])
                    P.op("dve", lambda e: e.tensor_tensor(
                        out=yo[:].rearrange("p (h d) -> p h d", d=64),
                        in0=pyo[:, 0:384].rearrange("p (h d) -> p h d", d=64),
                        in1=bc(ecs3[k][:, hs], [128, 6, 64], 2), op=ALU.mult),
                        reads=[b_pyo, b_ecs3[k]], writes=[b_yo])
                    P.op("dve", lambda e: e.tensor_tensor(out=yall[:, gs], in0=pyd[:, 0:384], in1=yo[:], op=ALU.add),
                         reads=[b_pyd, b_yo], writes=[b_y[g]])
                    P.op("dve", lambda e: e.tensor_tensor(out=yall[:, gs], in0=yall[:, gs], in1=sz[kl][:, gs], op=ALU.mult),
                         reads=[b_y[g], b_sz[kl]], writes=[b_y[g]])
                else:
                    P.op("dve", lambda e: e.tensor_tensor(out=yall[:, gs], in0=pyd[:, 0:384], in1=sz[kl][:, gs], op=ALU.mult),
                         reads=[b_pyd, b_sz[kl]], writes=[b_y[g]])
                P.op("act", lambda e, g=g: e.activation(out=junk[:], in_=yall[:, gs], func=AF.Square,
                                                       accum_out=ss[:, g:g + 1]), reads=[b_y[g]], writes=[b_junk, b_ss])
                if c < 15:
                    P.op("pe", lambda e: e.matmul(pst[:, 0:384], lhsT=bt[kl][:, g * 128:(g + 1) * 128], rhs=xdtw[k][:, gs],
                                                  start=True, stop=True), reads=[b_bt[kl], b_xdtw[k]], writes=[b_pst])
                    if c > 0:
                        P.op("pool", lambda e: e.tensor_tensor(
                            out=hst[:, gs].rearrange("p (h d) -> p h d", d=64),
                            in0=hst[:, gs].rearrange("p (h d) -> p h d", d=64),
                            in1=bc(ecs3[k][:, 48 + g * 6:48 + (g + 1) * 6], [128, 6, 64], 2), op=ALU.mult),
                            reads=[b_h[g], b_ecs3[k]], writes=[b_h[g]])
                        P.op("dve", lambda e: e.tensor_tensor(out=hst[:, gs], in0=pst[:, 0:384], in1=hst[:, gs], op=ALU.add),
                             reads=[b_pst, b_h[g]], writes=[b_h[g]])
                    else:
                        P.op("dve", lambda e: e.tensor_copy(out=hst[:, gs], in_=pst[:, 0:384]),
                             reads=[b_pst], writes=[b_h[g]])
                    P.op("act", lambda e: e.copy(out=hbf[:, gs], in_=hst[:, gs]), reads=[b_h[g]], writes=[b_hbf[g]])

        def tail(s, c):
            k = (s * 16 + c) % 2
            t0 = c * 128
            yall, b_y, ss, b_ss = yall2[k], b_y2[k], ss2[k], b_ss2[k]
            P.op("dve", lambda e: e.tensor_scalar(out=ssv[:], in0=ss[:], scalar1=1.0 / 384, scalar2=EPS,
                                                  op0=ALU.mult, op1=ALU.add), reads=[b_ss], writes=[b_ssv])
            P.op("act", lambda e: e.activation(out=ssl[:], in_=ssv[:], func=AF.Ln), reads=[b_ssv], writes=[b_ssl])
            P.op("act", lambda e: e.activation(out=rstd[:], in_=ssl[:], func=AF.Exp, scale=-0.5),
                 reads=[b_ssl], writes=[b_rstd])
            for g in range(4):
                gs = slice(g * 384, (g + 1) * 384)
                P.op("act", lambda e, g=g: e.activation(out=ymix[k][:, gs], in_=yall[:, gs], func=AF.Copy,
                                                       scale=rstd[:, g:g + 1]),
                     reads=[b_y[g], b_rstd], writes=[b_ymix[k]])
            P.dma("sp", T["mixed"][s, t0:t0 + 128, D_ATT:2048], ymix[k][:], f"ymo{k}", reads=[b_ymix[k]])

        chunks = [(s, c) for s in range(nseq) for c in range(16)]
        N = len(chunks)
        load(*chunks[0])
        if N > 1:
            load(*chunks[1])
        prefront(*chunks[0])
        for hg in range(8):
            segEM(*chunks[0], hg)
        for n, (s, c) in enumerate(chunks):
            if n + 2 < N:
                load(*chunks[n + 2])
            if n + 1 < N:
                prefront(*chunks[n + 1])
            for g in range(4):
                back_g(s, c, g)
                if g == 0 and n > 0:
                    tail(*chunks[n - 1])
                if n + 1 < N:
                    segEM(*chunks[n + 1], 2 * g)
                    segEM(*chunks[n + 1], 2 * g + 1)
        tail(*chunks[N - 1])
        P.barrier()


def load_weight(P, T, name, w_bf, nk, buf, key):
    for kc in range(nk):
        P.dma("pool", w_bf[:, kc, :], T[name][kc * 128:(kc + 1) * 128, :], key, writes=[buf])


def phase3a(P, T, nseq, w_out_bf, b_wout):
    nc = P.nc
    with contextlib.ExitStack() as es:
        A = Alloc(nc, es)
        ident = A.sb([128, 128], BF16, "ident3")
        g2 = A.sb([128, D], F32, "g2rep")
        b_c = P.buf("consts3a")
        P.dma("sp", ident[:], T["ident"], "c4", writes=[b_c])
        P.dma("sp", g2[:], T["ln2_g"], "c4", writes=[b_c])
        mx = [A.sb([128, 2048], BF16, f"mx{i}") for i in range(3)]; b_mx = P.bufs(3)
        xin = [A.sb([128, D], F32, f"x3in{i}") for i in range(3)]; b_xin = P.bufs(3)
        mxT = [A.sb([128, 16, 128], BF16, f"mxT{i}") for i in range(2)]; b_mxT = P.bufs(2)
        x1 = [A.sb([128, D], F32, f"x1_{i}") for i in range(2)]; b_x1 = P.bufs(2)
        junk = A.sb([128, D], BF16, "junk4"); b_junk = P.buf()
        ss = [A.sb([128, 1], F32, f"ss4_{i}") for i in range(2)]
        ssv = [A.sb([128, 1], F32, f"ssv4_{i}") for i in range(2)]
        ssl = [A.sb([128, 1], F32, f"ssl4_{i}") for i in range(2)]
        rstd = [A.sb([128, 1], F32, f"rstd4_{i}") for i in range(2)]
        b_ss, b_ssv, b_ssl, b_rstd = P.bufs(2), P.bufs(2), P.bufs(2), P.bufs(2)
        h2 = [A.sb([128, D], BF16, f"h2_{i}") for i in range(2)]; b_h2 = P.bufs(2)
        h2T = [A.sb([128, 8, 512], BF16, f"h2T{i}") for i in range(2)]; b_h2T = P.bufs(2)
        pmt = [A.ps([128, 8, 128], BF16, f"pmt{i}") for i in range(2)]; b_pmt = P.bufs(2)
        po = [A.ps([128, 512], F32, f"po{i}") for i in range(4)]; b_po = P.bufs(4)
        ph = A.ps([128, 8, 128], BF16, "ph"); b_ph = P.buf()
        cnt = {"pmt": 0}
        ntiles = nseq * 16

        def load(n):
            s, t0, k = n // 16, (n % 16) * 128, n % 3
            P.dma("sp", mx[k][:], T["mixed"][s, t0:t0 + 128, :], f"l_mx{k}", writes=[b_mx[k]])
            P.dma("pool", xin[k][:], T["x"][s, t0:t0 + 128, :], f"l_x3{k}", writes=[b_xin[k]])

        def transposes(n):
            k = n % 2
            k3 = n % 3
            for half in range(2):
                pi = cnt["pmt"] % 2
                cnt["pmt"] += 1
                for j in range(8):
                    kc = half * 8 + j
                    P.op("pe", lambda e, j=j, kc=kc, pi=pi: e.transpose(
                        out=pmt[pi][:, j, :], in_=mx[k3][:, kc * 128:(kc + 1) * 128], identity=ident[:]),
                        reads=[b_mx[k3], b_c], writes=[b_pmt[pi]], sig=(j == 7))
                P.op("act", lambda e, half=half, pi=pi: e.copy(out=mxT[k][:, half * 8:(half + 1) * 8, :], in_=pmt[pi][:]),
                     reads=[b_pmt[pi]], writes=[b_mxT[k]])

        load(0)
        if ntiles > 1:
            load(1)
        transposes(0)
        tails = []
        for n in range(ntiles):
            s, t0, k = n // 16, (n % 16) * 128, n % 2
            if n + 2 < ntiles:
                load(n + 2)
            if n + 1 < ntiles:
                transposes(n + 1)
            for nh in range(2):
                pi = k * 2 + nh
                for kc in range(16):
                    P.op("pe", lambda e, kc=kc, nh=nh, pi=pi: e.matmul(
                        po[pi][:], lhsT=mxT[k][:, kc, :], rhs=w_out_bf[:, kc, nh * 512:(nh + 1) * 512],
                        start=(kc == 0), stop=(kc == 15)), reads=[b_mxT[k], b_wout], writes=[b_po[pi]], sig=(kc == 15))
                P.op("dve", lambda e, nh=nh, pi=pi: e.tensor_tensor(
                    out=x1[k][:, nh * 512:(nh + 1) * 512], in0=po[pi][:], in1=xin[n % 3][:, nh * 512:(nh + 1) * 512], op=ALU.add),
                    reads=[b_po[pi], b_xin[n % 3]], writes=[b_x1[k]])
            P.dma("sp", T["out"][s, t0:t0 + 128, :], x1[k][:], f"x1o{k}", reads=[b_x1[k]])
            while tails:
                tails.pop(0)()
            P.op("act", lambda e: e.activation(out=junk[:], in_=x1[k][:], func=AF.Square, accum_out=ss[k][:]),
                 reads=[b_x1[k]], writes=[b_junk, b_ss[k]])
            P.op("dve", lambda e: e.tensor_scalar(out=ssv[k][:], in0=ss[k][:], scalar1=1.0 / D, scalar2=EPS,
                                                  op0=ALU.mult, op1=ALU.add), reads=[b_ss[k]], writes=[b_ssv[k]])
            P.op("act", lambda e: e.activation(out=ssl[k][:], in_=ssv[k][:], func=AF.Ln), reads=[b_ssv[k]], writes=[b_ssl[k]])
            P.op("act", lambda e: e.activation(out=rstd[k][:], in_=ssl[k][:], func=AF.Exp, scale=-0.5),
                 reads=[b_ssl[k]], writes=[b_rstd[k]])
            P.op("dve", lambda e: e.scalar_tensor_tensor(out=h2[k][:], in0=x1[k][:], scalar=rstd[k][:, 0:1], in1=g2[:],
                                                         op0=ALU.mult, op1=ALU.mult),
                 reads=[b_x1[k], b_rstd[k], b_c], writes=[b_h2[k]])
            def tail(n=n, s=s, t0=t0, k=k):
                for kc in range(8):
                    P.op("pe", lambda e, kc=kc: e.transpose(out=ph[:, kc, :], in_=h2[k][:, kc * 128:(kc + 1) * 128],
                                                            identity=ident[:]),
                         reads=[b_h2[k], b_c], writes=[b_ph], sig=(kc == 7))
                gslot = (n // 4) % 2
                tt = n % 4
                P.op("act", lambda e: e.copy(out=h2T[gslot][:, :, tt * 128:(tt + 1) * 128], in_=ph[:]),
                     reads=[b_ph], writes=[b_h2T[gslot]])
                if tt == 3:
                    g0 = t0 - 384
                    P.dma("sp", T["h2T"][s, :, g0:g0 + 512].rearrange("(kc p) t -> p kc t", p=128), h2T[gslot][:],
                          f"h2To{gslot}", reads=[b_h2T[gslot]])
            tails.append(tail)
        while tails:
            tails.pop(0)()
        P.barrier()


def phase3b(P, T, nseq, w_dn_bf, b_wdn):
    nc = P.nc
    NP = D_FF // 128
    with contextlib.ExitStack() as es:
        A = Alloc(nc, es)
        w_up_bf = A.sb([128, 8, 2 * D_FF], BF16, "w_up_bf")
        b_wup = P.buf("w_up")
        load_weight(P, T, "w_up", w_up_bf, 8, b_wup, "w3")
        gf = A.sb([128, D], F32, "gfrep")
        cw = A.sb([128, 2, NP, 3], F32, "cw3")
        cb = A.sb([128, 2, NP], F32, "cb3")
        b_c = P.buf("consts3b")
        P.dma("sp", gf[:], T["lnf_g"], "c5", writes=[b_c])
        P.dma("sp", cw[:], T["ffn_cw"], "c5", writes=[b_c])
        P.dma("sp", cb[:], T["ffn_cb"], "c5", writes=[b_c])
        h2T = [A.sb([128, 8, 256], BF16, f"h2Tb{i}") for i in range(2)]; b_h2T = P.bufs(2)
        x1 = [A.sb([128, D], F32, f"x1b{i}") for i in range(2)]; b_x1 = P.bufs(2)
        gT = [A.sb([128, NP, 256], BF16, f"gT{i}") for i in range(2)]; b_gT = P.bufs(2)
        pre = [A.sb([128, 2, 258], F32, f"preb{i}") for i in range(2)]; b_pre = P.bufs(2)
        accg = [A.sb([128, 256], F32, f"accg{i}") for i in range(2)]; b_accg = P.bufs(2)
        accv = [A.sb([128, 256], F32, f"accv{i}") for i in range(2)]; b_accv = P.bufs(2)
        sgt = [A.sb([128, 256], F32, f"sgt{i}") for i in range(2)]; b_sgt = P.bufs(2)
        hist = A.sb([128, 2, NP, 2], F32, "histb"); b_hist = P.bufs(NP)
        x2 = [A.sb([128, D], F32, f"x2_{i}") for i in range(2)]; b_x2 = P.bufs(2)
        junk = A.sb([128, D], BF16, "junk5"); b_junk = P.buf()
        ss = [A.sb([128, 1], F32, f"ss5_{i}") for i in range(2)]
        ssv = [A.sb([128, 1], F32, f"ssv5_{i}") for i in range(2)]
        ssl = [A.sb([128, 1], F32, f"ssl5_{i}") for i in range(2)]
        rstd = [A.sb([128, 1], F32, f"rstd5_{i}") for i in range(2)]
        b_ss, b_ssv, b_ssl, b_rstd = P.bufs(2), P.bufs(2), P.bufs(2), P.bufs(2)
        pu = [A.ps([128, 2, 256], F32, f"pu{i}") for i in range(3)]; b_pu = P.bufs(3)
        pdn = [A.ps([128, 512], F32, f"pdn{i}") for i in range(4)]; b_pdn = P.bufs(4)
        cnt = {"pu": 0, "pre": 0}
        posts = []
        ngroups = nseq * 8

        def load(n):
            s, t0, k = n // 8, (n % 8) * 256, n % 2
            P.dma("sp", h2T[k][:], T["h2T"][s, :, t0:t0 + 256].rearrange("(kc p) t -> p kc t", p=128), f"l_h2T{k}",
                  writes=[b_h2T[k]])

        def up_pair(n, p):
            s, t0, k = n // 8, (n % 8) * 256, n % 2
            pi = cnt["pu"] % 3
            cnt["pu"] += 1
            for half in range(2):
                col0 = half * D_FF + p * 128
                for kc in range(8):
                    P.op("pe", lambda e, kc=kc, half=half, col0=col0: e.matmul(
                        pu[pi][:, half, :], lhsT=w_up_bf[:, kc, col0:col0 + 128], rhs=h2T[k][:, kc, :],
                        start=(kc == 0), stop=(kc == 7)),
                        reads=[b_wup, b_h2T[k]], writes=[b_pu[pi]], sig=(kc == 7 and half == 1))
            pr = cnt["pre"] % 2
            cnt["pre"] += 1
            P.op("act", lambda e: e.copy(out=pre[pr][:, :, 2:258], in_=pu[pi][:]), reads=[b_pu[pi]], writes=[b_pre[pr]])
            P.op("act", lambda e: e.activation(out=accg[pr][:], in_=pu[pi][:, 0, :], func=AF.Identity,
                                               scale=cw[:, 0, p, 2:3], bias=cb[:, 0, p:p + 1]),
                 reads=[b_pu[pi], b_c], writes=[b_accg[pr]])
            P.op("act", lambda e: e.activation(out=accv[pr][:], in_=pu[pi][:, 1, :], func=AF.Identity,
                                               scale=cw[:, 1, p, 2:3], bias=cb[:, 1, p:p + 1]),
                 reads=[b_pu[pi], b_c], writes=[b_accv[pr]])
            while posts:
                posts.pop(0)()
            if t0 == 0:
                P.op("dve", lambda e: e.memset(pre[pr][:, :, 0:2], 0.0), writes=[b_pre[pr]])
            else:
                P.op("dve", lambda e: e.tensor_copy(out=pre[pr][:, :, 0:2], in_=hist[:, :, p, :]),
                     reads=[b_hist[p]], writes=[b_pre[pr]])
            for kk in (1, 0):
                P.op("dve", lambda e, kk=kk: e.scalar_tensor_tensor(
                    out=accg[pr][:], in0=pre[pr][:, 0, kk:kk + 256], scalar=cw[:, 0, p, kk:kk + 1],
                    in1=accg[pr][:], op0=ALU.mult, op1=ALU.add),
                    reads=[b_pre[pr], b_c, b_accg[pr]], writes=[b_accg[pr]])
                P.op("dve", lambda e, kk=kk: e.scalar_tensor_tensor(
                    out=accv[pr][:], in0=pre[pr][:, 1, kk:kk + 256], scalar=cw[:, 1, p, kk:kk + 1],
                    in1=accv[pr][:], op0=ALU.mult, op1=ALU.add),
                    reads=[b_pre[pr], b_c, b_accv[pr]], writes=[b_accv[pr]])
            P.op("dve", lambda e: e.tensor_copy(out=hist[:, :, p, :], in_=pre[pr][:, :, 256:258]),
                 reads=[b_pre[pr]], writes=[b_hist[p]])
            def post():
                P.op("act", lambda e: e.activation(out=sgt[pr][:], in_=accg[pr][:], func=AF.Silu),
                     reads=[b_accg[pr]], writes=[b_sgt[pr]])
                P.op("pool", lambda e: e.tensor_tensor(out=gT[k][:, p, :], in0=sgt[pr][:], in1=accv[pr][:], op=ALU.mult),
                     reads=[b_sgt[pr], b_accv[pr]], writes=[b_gT[k]])
            posts.append(post)
            if p == NP - 1:
                while posts:
                    posts.pop(0)()

        def down_mm(n, m):
            s, t0, k = n // 8, (n % 8) * 256, n % 2
            tt, nh, fc = m // 44, (m // 22) % 2, m % 22
            pi = tt * 2 + nh
            P.op("pe", lambda e: e.matmul(
                pdn[pi][:], lhsT=gT[k][:, fc, tt * 128:(tt + 1) * 128], rhs=w_dn_bf[:, fc, nh * 512:(nh + 1) * 512],
                start=(fc == 0), stop=(fc == NP - 1)),
                reads=[b_gT[k], b_wdn], writes=[b_pdn[pi]], sig=(fc == NP - 1))
            if fc == NP - 1:
                ti = tt
                P.op("dve", lambda e: e.tensor_tensor(
                    out=x2[ti][:, nh * 512:(nh + 1) * 512], in0=pdn[pi][:], in1=x1[tt][:, nh * 512:(nh + 1) * 512],
                    op=ALU.add), reads=[b_pdn[pi], b_x1[tt]], writes=[b_x2[ti]])
                if nh == 1:
                    P.op("act", lambda e: e.activation(out=junk[:], in_=x2[ti][:], func=AF.Square, accum_out=ss[ti][:]),
                         reads=[b_x2[ti]], writes=[b_junk, b_ss[ti]])
                    P.op("dve", lambda e: e.tensor_scalar(out=ssv[ti][:], in0=ss[ti][:], scalar1=1.0 / D, scalar2=EPS,
                                                          op0=ALU.mult, op1=ALU.add), reads=[b_ss[ti]], writes=[b_ssv[ti]])
                    P.op("act", lambda e: e.activation(out=ssl[ti][:], in_=ssv[ti][:], func=AF.Ln),
                         reads=[b_ssv[ti]], writes=[b_ssl[ti]])
                    P.op("act", lambda e: e.activation(out=rstd[ti][:], in_=ssl[ti][:], func=AF.Exp, scale=-0.5),
                         reads=[b_ssl[ti]], writes=[b_rstd[ti]])
                    P.op("dve", lambda e: e.scalar_tensor_tensor(
                        out=x2[ti][:], in0=x2[ti][:], scalar=rstd[ti][:, 0:1], in1=gf[:], op0=ALU.mult, op1=ALU.mult),
                        reads=[b_x2[ti], b_rstd[ti], b_c], writes=[b_x2[ti]])
                    P.dma("sp", T["out"][s, t0 + tt * 128:t0 + (tt + 1) * 128, :], x2[ti][:], f"outo{ti}",
                          reads=[b_x2[ti]])

        def load_x1(n):
            s, t0 = n // 8, (n % 8) * 256
            for tt in range(2):
                P.dma("sp", x1[tt][:], T["out"][s, t0 + tt * 128:t0 + (tt + 1) * 128, :], f"l_x1b{tt}", writes=[b_x1[tt]])

        load(0)
        for n in range(ngroups + 1):
            if n + 1 < ngroups:
                load(n + 1)
            if n >= 1:
                load_x1(n - 1)
            for p in range(NP):
                if n < ngroups:
                    up_pair(n, p)
                if n >= 1:
                    for m in range(4 * p, 4 * p + 4):
                        down_mm(n - 1, m)
        P.barrier()


SCRATCH = {
    "qk": ([16, 64, S], BF16), "v": ([S, 512], BF16), "xs": ([S, D_SSM], BF16), "btok": ([S, 512], BF16),
    "bT": ([512, S], BF16), "cT": ([512, S], BF16), "sz": ([S, D_SSM], BF16), "dt": ([128, 16, NH_SSM], F32),
    "mixed": ([S, 2048], BF16), "h2T": ([D, S], BF16),
}
CONSTS = {
    "ident": ([128, 128], BF16), "cmask": ([128, 2, 256], BF16), "ownmask": ([128, 16, 8], F32), "kbias": ([128, 8, 2], F32),
    "pastmask": ([128, 16, 8], F32), "dmbase": ([128, 16, 8], F32), "karows": ([9, S], BF16), "qrow": ([8, S], BF16),
    "U": ([128, 128], F32), "Ls": ([128, 128], F32), "ones": ([128, 128], F32), "identf": ([128, 128], F32),
}
PARAMS = {
    "ln1_g": [128, D], "ssm_cw": [128, 20, 4], "ssm_cb": [128, 20], "dt_bias": [128, NH_SSM],
    "attn_g": [128, D_ATT], "a_log": [128, NH_SSM], "d_skip": [128, NH_SSM], "ssm_g": [128, D_SSM],
    "ln2_g": [128, D], "lnf_g": [128, D], "ffn_cw": [128, 2, 22, 3], "ffn_cb": [128, 2, 22], "mix_g": [128, 16],
}


def build_program(nseq=4, phases=(1,), debug=False):
    nc = bass.Bass("TRN2", target_bir_lowering=False)
    T = {}
    T["x"] = nc.dram_tensor("x", [nseq, S, D], F32, kind="ExternalInput").ap()
    T["w_in"] = nc.dram_tensor("w_in", [D, D_IN], F32, kind="ExternalInput").ap()
    T["w_out"] = nc.dram_tensor("w_out", [2048, D], F32, kind="ExternalInput").ap()
    T["w_up"] = nc.dram_tensor("w_up", [D, 2 * D_FF], F32, kind="ExternalInput").ap()
    T["w_down"] = nc.dram_tensor("w_down", [D_FF, D], F32, kind="ExternalInput").ap()
    for k, shp in PARAMS.items():
        T[k] = nc.dram_tensor(k, shp, F32, kind="ExternalInput").ap()
    for k, (shp, dt) in CONSTS.items():
        T[k] = nc.dram_tensor(k, shp, dt, kind="ExternalInput").ap()
    for k, (shp, dt) in SCRATCH.items():
        T[k] = nc.dram_tensor(k, [nseq] + shp, dt, kind="ExternalOutput" if debug else "Internal").ap()
    T["out"] = nc.dram_tensor("out", [nseq, S, D], F32, kind="ExternalOutput").ap()
    with contextlib.ExitStack() as es:
        P = Prog(nc, es)
        if 1 in phases:
            phase1(P, T, nseq)
        with contextlib.ExitStack() as es_dn:
            w_dn_bf = Alloc(nc, es_dn).sb([128, 22, D], BF16, "w_dn_bf")
            b_wdn = P.buf("w_dn")
            with contextlib.ExitStack() as es_out:
                w_out_bf = Alloc(nc, es_out).sb([128, 16, D], BF16, "w_out_bf")
                b_wout = P.buf("w_out")
                if 5 in phases:
                    load_weight(P, T, "w_down", w_dn_bf, 22, b_wdn, "w2d")
                if 2 in phases:
                    phase2a(P, T, nseq, w_out_bf if 4 in phases else None, b_wout)
                if 3 in phases:
                    phase2b(P, T, nseq)
                if 4 in phases:
                    phase3a(P, T, nseq, w_out_bf, b_wout)
            if 5 in phases:
                phase3b(P, T, nseq, w_dn_bf, b_wdn)
        P.finish()
    return nc


def host_consts():
    bf = ml_dtypes.bfloat16
    c = {"ident": np.eye(128, dtype=np.float32).astype(bf)}
    sl = np.array(SLOPES, dtype=np.float64)
    p = np.arange(128)
    krel = (np.arange(2)[None, :] * 128 + p[:, None]).astype(np.float64)
    q = np.arange(256, dtype=np.float64)
    d = q[None, None, :] - krel[:, :, None]
    c["cmask"] = np.where(d >= 0, 0.0, -BIG).astype(np.float32).astype(bf)
    c["kbias"] = (sl[None, :, None] * krel[:, None, :]).astype(np.float32)
    t = np.arange(16)
    j = np.arange(8)
    past = (j[None, :] < (t[:, None] // 2))
    c["pastmask"] = np.ascontiguousarray(np.broadcast_to(np.where(past, 0.0, -1e30)[None], (128, 16, 8))).astype(np.float32)
    own = (j[None, :] == (t[:, None] // 2)).astype(np.float32)
    c["ownmask"] = np.ascontiguousarray(np.broadcast_to(own[None], (128, 16, 8))).astype(np.float32)
    dmb = np.where(past, (t[:, None] // 2 - j[None, :]), 0).astype(np.float32)
    c["dmbase"] = np.ascontiguousarray(np.broadcast_to(dmb[None], (128, 16, 8))).astype(np.float32)
    kr = np.zeros((9, S), dtype=np.float32)
    for jj in range(8):
        kr[jj, jj * 256:(jj + 1) * 256] = 1.0
    kr[8, :] = 1.0
    c["karows"] = kr.astype(bf)
    qrel = (np.arange(S) % 256).astype(np.float64)
    c["qrow"] = (-8.0 * sl[:, None] * qrel[None, :]).astype(np.float32).astype(bf)
    li = np.arange(128)
    c["U"] = (li[:, None] <= li[None, :]).astype(np.float32)
    c["Ls"] = (li[:, None] > li[None, :]).astype(np.float32)
    c["ones"] = np.ones((128, 128), dtype=np.float32)
    c["identf"] = np.eye(128, dtype=np.float32)
    return c


def host_params(inp):
    f = np.float32
    p = {}
    p["ln1_g"] = np.ascontiguousarray(np.broadcast_to(inp["ln1_g"][0][None, :], (128, D))).astype(f)
    cw = inp["ssm_conv_w"][0]
    p["ssm_cw"] = np.ascontiguousarray(cw.reshape(4, 20, 128).transpose(2, 1, 0)).astype(f)
    p["ssm_cb"] = np.ascontiguousarray(inp["ssm_conv_b"][0].reshape(20, 128).T).astype(f)
    p["dt_bias"] = np.ascontiguousarray(np.broadcast_to(inp["dt_bias"][0][None, :], (128, NH_SSM))).astype(f)
    p["attn_g"] = np.ascontiguousarray(np.broadcast_to(inp["attn_norm_g"][0][None, :], (128, D_ATT))).astype(f)
    p["a_log"] = np.ascontiguousarray(np.broadcast_to(inp["a_log"][0][None, :], (128, NH_SSM))).astype(f)
    p["d_skip"] = np.ascontiguousarray(np.broadcast_to(inp["d_skip"][0][None, :], (128, NH_SSM))).astype(f)
    p["ssm_g"] = np.ascontiguousarray(np.broadcast_to(inp["ssm_norm_g"][0][None, :], (128, D_SSM))).astype(f)
    p["ln2_g"] = np.ascontiguousarray(np.broadcast_to(inp["ln2_g"][0][None, :], (128, D))).astype(f)
    p["lnf_g"] = np.ascontiguousarray(np.broadcast_to(inp["lnf_g"][None, :], (128, D))).astype(f)
    fw = inp["ffn_conv_w"][0]
    p["ffn_cw"] = np.ascontiguousarray(fw.reshape(3, 2, 22, 128).transpose(3, 1, 2, 0)).astype(f)
    p["ffn_cb"] = np.ascontiguousarray(inp["ffn_conv_b"][0].reshape(2, 22, 128).transpose(2, 0, 1)).astype(f)
    mg = np.concatenate([inp["attn_norm_g"][0], inp["ssm_norm_g"][0]])
    p["mix_g"] = np.ascontiguousarray(mg.reshape(16, 128).T).astype(f)
    return p


def kernel(**inp):
    inp = {k: np.asarray(v) for k, v in inp.items()}
    nseq = 32 // NCORES
    nc = build_program(nseq=nseq, phases=(1, 2, 3, 4, 5))
    base = {"w_in": np.ascontiguousarray(inp["w_in"][0]), "w_out": np.ascontiguousarray(inp["w_out"][0]),
            "w_up": np.ascontiguousarray(inp["w_up"][0]), "w_down": np.ascontiguousarray(inp["w_down"][0])}
    base.update(host_consts())
    base.update(host_params(inp))
    in_maps = []
    for c in range(NCORES):
        m = dict(base)
        m["x"] = np.ascontiguousarray(inp["x"][c * nseq:(c + 1) * nseq])
        in_maps.append(m)
    res = run_bass_kernel_spmd(nc, in_maps, core_ids=list(range(NCORES)))
    return np.concatenate([r["out"] for r in res.results], axis=0)
```

```python
import contextlib
import numpy as np
import ml_dtypes
import concourse.bass as bass
import concourse.mybir as mybir
from concourse.bass_utils import run_bass_kernel_spmd

F32 = mybir.dt.float32
BF16 = mybir.dt.bfloat16
AF = mybir.ActivationFunctionType
ALU = mybir.AluOpType
AX = mybir.AxisListType

NCORES = 8
S = 2048
D = 1024
D_ATT = 512
D_SSM = 1536
NH_SSM = 24
D_XBC = 2560
D_IN = 5656
D_FF = 2816
EPS = 1e-6
C_Q, C_K, C_V, C_Z, C_XBC, C_DT = 0, 512, 1024, 1536, 3072, 5632
BIG = 30000.0


class Tok:
    __slots__ = ("key", "val")

    def __init__(self, key, val):
        self.key, self.val = key, val


class Buf:
    __slots__ = ("name", "w", "r")

    def __init__(self, name):
        self.name, self.w, self.r = name, {}, {}


class Prog:
    def __init__(self, nc, es):
        self.nc, self.es = nc, es
        self.eng = {"pe": nc.tensor, "act": nc.scalar, "dve": nc.vector, "pool": nc.gpsimd, "sp": nc.sync}
        self.sems, self.cnt, self.seen = {}, {}, {}
        self.pending = {e: [] for e in self.eng}
        self.nbuf = 0
        for e in self.eng:
            self._sem(e)

    def _sem(self, key):
        if key not in self.sems:
            self.sems[key] = self.es.enter_context(self.nc.semaphore("s_" + key))
            self.cnt[key] = 0
        return self.sems[key]

    def buf(self, name=None):
        self.nbuf += 1
        return Buf(name or f"b{self.nbuf}")

    def bufs(self, n, name=None):
        return [self.buf(None if name is None else f"{name}{i}") for i in range(n)]

    def wait(self, eng, tok):
        if tok is None:
            return
        if self.seen.get((eng, tok.key), 0) >= tok.val:
            return
        self.eng[eng].wait_ge(self.sems[tok.key], tok.val)
        self.seen[(eng, tok.key)] = tok.val

    def _deps(self, eng, reads, writes):
        for b in reads:
            for t in b.w.values():
                self.wait(eng, t)
        for b in writes:
            for t in b.w.values():
                self.wait(eng, t)
            for t in b.r.values():
                self.wait(eng, t)

    def _mark(self, tok, reads, writes):
        for b in reads:
            b.r[tok.key] = tok
        for b in writes:
            if b.r:
                b.r = {}
                b.w = {}
            b.w[tok.key] = tok

    def op(self, eng, fn, reads=(), writes=(), sig=True):
        self._deps(eng, reads, writes)
        ins = fn(self.eng[eng])
        if not sig:
            self.pending[eng].append((tuple(reads), tuple(writes)))
            return None
        ins.then_inc(self.sems[eng], 1)
        self.cnt[eng] += 1
        tok = Tok(eng, self.cnt[eng])
        for (r, w) in self.pending[eng]:
            self._mark(tok, r, w)
        self.pending[eng] = []
        self._mark(tok, reads, writes)
        return tok

    def dma(self, q, out, in_, key, reads=(), writes=()):
        self._sem(key)
        self._deps(q, reads, writes)
        self.eng[q].dma_start(out=out, in_=in_).then_inc(self.sems[key], 16)
        self.cnt[key] += 16
        tok = Tok(key, self.cnt[key])
        self._mark(tok, reads, writes)
        return tok

    def barrier(self):
        for e in self.eng:
            assert not self.pending[e], e
        for key in self.sems:
            if self.cnt[key] > 0:
                self.wait("sp", Tok(key, self.cnt[key]))
        self.eng["sp"].sem_inc(self.sems["sp"], 1)
        self.cnt["sp"] += 1
        tok = Tok("sp", self.cnt["sp"])
        for e in self.eng:
            self.wait(e, tok)

    def finish(self):
        for key in self.sems:
            if self.cnt[key] > 0:
                self.wait("sp", Tok(key, self.cnt[key]))


class Alloc:
    def __init__(self, nc, es):
        self.nc, self.es, self.n = nc, es, 0

    def sb(self, shape, dt, name=None):
        self.n += 1
        return self.es.enter_context(self.nc.sbuf_tensor(f"t{self.n}_" + (name or "sb"), list(shape), dt))

    def ps(self, shape, dt, name=None):
        self.n += 1
        return self.es.enter_context(self.nc.psum_tensor(f"p{self.n}_" + (name or "ps"), list(shape), dt))


def phase1(P, T, nseq):
    nc = P.nc
    with contextlib.ExitStack() as es:
        A = Alloc(nc, es)
        ngroups = nseq * 4
        w_bf = A.sb([128, 8, D_IN], BF16, "w_in_bf")
        b_w = P.buf("w_in")
        for kc in range(8):
            P.dma("pool", w_bf[:, kc, :], T["w_in"][kc * 128:(kc + 1) * 128, :], "w1", writes=[b_w])
        ident = A.sb([128, 128], BF16, "ident1")
        g1 = A.sb([128, D], F32, "g1rep")
        cw = A.sb([128, 20, 4], F32, "cw1")
        cb = A.sb([128, 20], F32, "cb1")
        dtb = A.sb([128, NH_SSM], F32, "dtb")
        b_c = P.buf("consts1")
        P.dma("sp", ident[:], T["ident"], "c1", writes=[b_c])
        P.dma("sp", g1[:], T["ln1_g"], "c1", writes=[b_c])
        P.dma("sp", cw[:], T["ssm_cw"], "c1", writes=[b_c])
        P.dma("sp", cb[:], T["ssm_cb"], "c1", writes=[b_c])
        P.dma("sp", dtb[:], T["dt_bias"], "c1", writes=[b_c])

        xin = [A.sb([128, D], F32, f"xin{i}") for i in range(4)]
        b_xin = P.bufs(4, "xin")
        junk = A.sb([128, D], BF16, "junk1")
        b_junk = P.buf("junk")
        ss = A.sb([128, 4], F32, "ss")
        ssv = A.sb([128, 4], F32, "ssv")
        ssl = A.sb([128, 4], F32, "ssl")
        rstd = A.sb([128, 4], F32, "rstd")
        b_ss, b_ssv, b_ssl, b_rstd = P.buf(), P.buf(), P.buf(), P.buf()
        hn = [A.sb([128, D], BF16, f"hn{i}") for i in range(4)]
        b_hn = P.bufs(4, "hn")
        hnT = [A.sb([128, 8, 512], BF16, f"hnT{i}") for i in range(2)]
        b_hnT = P.bufs(2, "hnT")
        pt = [A.ps([128, 8, 128], BF16, f"pt{i}") for i in range(2)]
        b_pt = P.bufs(2, "pt")
        pm = [A.ps([128, 512], F32, f"pm{i}") for i in range(3)]
        b_pm = P.bufs(3, "pm")
        px = [A.ps([128, 8, 128], BF16, f"px{i}") for i in range(2)]
        b_px = P.bufs(2, "px")
        pd = A.ps([128, 512], F32, "pd")
        b_pd = P.buf("pd")
        pre = [A.sb([128, 515], F32, f"pre{i}") for i in range(2)]
        b_pre = P.bufs(2, "pre")
        acc = [A.sb([128, 512], F32, f"acc{i}") for i in range(2)]
        b_acc = P.bufs(2, "acc")
        hist = A.sb([128, 20, 3], F32, "hist")
        b_hist = P.bufs(20, "hist")
        cvo = [A.sb([128, 512], BF16, f"cvo{i}") for i in range(4)]
        b_cvo = P.bufs(4, "cvo")
        xs_tok = A.sb([128, 4, D_SSM], BF16, "xs_tok")
        b_xs_tok = P.buf("xs_tok")
        b_tok = A.sb([128, 4, 512], BF16, "b_tok")
        b_b_tok = P.buf("b_tok")
        qk_ev = [A.sb([128, 512], BF16, f"qkev{i}") for i in range(3)]
        b_qk_ev = P.bufs(3, "qkev")
        v_ev = [A.sb([128, 512], BF16, f"vev{i}") for i in range(2)]
        b_v_ev = P.bufs(2, "vev")
        sz_ev = [A.sb([128, D_SSM], BF16, f"szev{i}") for i in range(2)]
        b_sz_ev = P.bufs(2, "szev")
        ntiles = nseq * 16
        dt_all = A.sb([128, ntiles, NH_SSM], F32, "dt_all")
        dt_e = A.sb([128, ntiles, NH_SSM], F32, "dt_e")
        b_dt_all, b_dt_e = P.buf("dt_all"), P.buf("dt_e")

        cnt = {"pm": 0, "pt": 0, "px": 0, "cvo": 0, "pre": 0, "qk": 0, "v": 0, "sz": 0}

        def norm(gi):
            s, t0 = gi // 4, (gi % 4) * 512
            g = gi % 2
            for tt in range(4):
                P.dma("pool", xin[tt][:], T["x"][s, t0 + tt * 128:t0 + (tt + 1) * 128, :], f"xin{tt}",
                      writes=[b_xin[tt]])
                P.op("act", lambda e, tt=tt: e.activation(out=junk[:], in_=xin[tt][:], func=AF.Square,
                                                         accum_out=ss[:, tt:tt + 1]),
                     reads=[b_xin[tt]], writes=[b_junk, b_ss])
            P.op("dve", lambda e: e.tensor_scalar(out=ssv[:], in0=ss[:], scalar1=1.0 / D, scalar2=EPS,
                                                  op0=ALU.mult, op1=ALU.add), reads=[b_ss], writes=[b_ssv])
            P.op("act", lambda e: e.activation(out=ssl[:], in_=ssv[:], func=AF.Ln), reads=[b_ssv], writes=[b_ssl])
            P.op("act", lambda e: e.activation(out=rstd[:], in_=ssl[:], func=AF.Exp, scale=-0.5),
                 reads=[b_ssl], writes=[b_rstd])
            for tt in range(4):
                h = tt
                P.op("dve", lambda e, tt=tt, h=h: e.scalar_tensor_tensor(
                    out=hn[h][:], in0=xin[tt][:], scalar=rstd[:, tt:tt + 1], in1=g1[:], op0=ALU.mult, op1=ALU.mult),
                    reads=[b_xin[tt], b_rstd, b_c], writes=[b_hn[h]])

        def norm_pe(gi):
            g = gi % 2
            for tt in range(4):
                h = tt
                pi = cnt["pt"] % 2
                cnt["pt"] += 1
                for kc in range(8):
                    P.op("pe", lambda e, pi=pi, kc=kc, h=h: e.transpose(
                        out=pt[pi][:, kc, :], in_=hn[h][:, kc * 128:(kc + 1) * 128], identity=ident[:]),
                        reads=[b_hn[h], b_c], writes=[b_pt[pi]], sig=(kc == 7))
                P.op("act", lambda e, pi=pi, tt=tt, g=g: e.copy(
                    out=hnT[g][:, :, tt * 128:(tt + 1) * 128], in_=pt[pi][:]),
                    reads=[b_pt[pi]], writes=[b_hnT[g]])

        def mm(gi, nxt):
            s, t0 = gi // 4, (gi % 4) * 512
            g = gi % 2
            deferred = []
            units = tok_units(gi)
            post = mm_post_factory(gi, deferred)

            def flush(upto):
                while deferred and deferred[0][0] <= upto:
                    deferred.pop(0)[1]()

            for c in range(28):
                col0 = c * 128 if c < 8 else C_XBC + (c - 8) * 128
                pi = cnt["pm"] % 3
                cnt["pm"] += 1
                for kc in range(8):
                    P.op("pe", lambda e, pi=pi, kc=kc, col0=col0: e.matmul(
                        pm[pi][:], lhsT=w_bf[:, kc, col0:col0 + 128], rhs=hnT[g][:, kc, :],
                        start=(kc == 0), stop=(kc == 7)),
                        reads=[b_w, b_hnT[g]], writes=[b_pm[pi]], sig=(kc == 7))
                if c < 8:
                    qi = cnt["qk"] % 3
                    cnt["qk"] += 1
                    P.op("dve", lambda e, qi=qi, pi=pi: e.tensor_copy(out=qk_ev[qi][:], in_=pm[pi][:]),
                         reads=[b_pm[pi]], writes=[b_qk_ev[qi]])
                    P.dma("sp", T["qk"][s, 2 * c:2 * c + 2, :, t0:t0 + 512].rearrange("h d t -> (h d) t"),
                          qk_ev[qi][:], f"qko{qi}", reads=[b_qk_ev[qi]])
                else:
                    cc = c - 8
                    pr = cnt["pre"] % 2
                    cnt["pre"] += 1
                    P.op("act", lambda e, pr=pr, pi=pi: e.copy(out=pre[pr][:, 3:515], in_=pm[pi][:]),
                         reads=[b_pm[pi]], writes=[b_pre[pr]])
                    if t0 == 0:
                        P.op("dve", lambda e, pr=pr: e.memset(pre[pr][:, 0:3], 0.0), writes=[b_pre[pr]])
                    else:
                        P.op("dve", lambda e, pr=pr, cc=cc: e.tensor_copy(out=pre[pr][:, 0:3], in_=hist[:, cc, :]),
                             reads=[b_hist[cc]], writes=[b_pre[pr]])
                    P.op("act", lambda e, pr=pr, cc=cc, pi=pi: e.activation(
                        out=acc[pr][:], in_=pm[pi][:], func=AF.Identity, scale=cw[:, cc, 3:4], bias=cb[:, cc:cc + 1]),
                        reads=[b_pm[pi], b_c], writes=[b_acc[pr]])
                    for k in (2, 1, 0):
                        P.op("dve", lambda e, pr=pr, cc=cc, k=k: e.scalar_tensor_tensor(
                            out=acc[pr][:], in0=pre[pr][:, k:k + 512], scalar=cw[:, cc, k:k + 1], in1=acc[pr][:],
                            op0=ALU.mult, op1=ALU.add), reads=[b_pre[pr], b_c, b_acc[pr]], writes=[b_acc[pr]])
                    P.op("dve", lambda e, pr=pr, cc=cc: e.tensor_copy(out=hist[:, cc, :], in_=pre[pr][:, 512:515]),
                         reads=[b_pre[pr]], writes=[b_hist[cc]])
                    deferred.append((c + 1, lambda c=c, cc=cc, pr=pr: post(c, cc, pr)))
                if c >= 8:
                    units.pop(0)()
                flush(c)
            flush(10 ** 9)
            assert not units
            if nxt is not None:
                norm_pe(nxt)

        def mm_post_factory(gi, deferred):
            s, t0 = gi // 4, (gi % 4) * 512

            def post(c, cc, pr):
                    ci = cnt["cvo"] % 4
                    cnt["cvo"] += 1
                    P.op("act", lambda e, ci=ci, pr=pr: e.activation(out=cvo[ci][:], in_=acc[pr][:], func=AF.Silu),
                         reads=[b_acc[pr]], writes=[b_cvo[ci]])
                    if cc >= 12:
                        gg = (cc - 12) % 4
                        dst = T["bT"] if cc < 16 else T["cT"]
                        P.dma("sp", dst[s, gg * 128:(gg + 1) * 128, t0:t0 + 512], cvo[ci][:], f"cvo{ci}",
                              reads=[b_cvo[ci]])
                    if cc < 16:
                        def tr(ci=ci, cc=cc):
                            xi = cnt["px"] % 2
                            cnt["px"] += 1
                            for tt in range(4):
                                P.op("pe", lambda e, xi=xi, tt=tt, ci=ci: e.transpose(
                                    out=px[xi][:, tt, :], in_=cvo[ci][:, tt * 128:(tt + 1) * 128], identity=ident[:]),
                                    reads=[b_cvo[ci], b_c], writes=[b_px[xi]], sig=(tt == 3))
                            deferred.append((c + 4, lambda: tr_ev(xi, cc)))

                        def tr_ev(xi, cc):
                            if cc < 12:
                                P.op("act", lambda e, xi=xi, cc=cc: e.copy(
                                    out=xs_tok[:, :, cc * 128:(cc + 1) * 128], in_=px[xi][:, 0:4, :]),
                                    reads=[b_px[xi]], writes=[b_xs_tok])
                                if cc == 11:
                                    P.dma("sp", T["xs"][s, t0:t0 + 512, :].rearrange("(tt p) c -> p tt c", p=128),
                                          xs_tok[:], "xso", reads=[b_xs_tok])
                            else:
                                gg = cc - 12
                                P.op("act", lambda e, xi=xi, gg=gg: e.copy(
                                    out=b_tok[:, :, gg * 128:(gg + 1) * 128], in_=px[xi][:, 0:4, :]),
                                    reads=[b_px[xi]], writes=[b_b_tok])
                                if gg == 3:
                                    P.dma("sp", T["btok"][s, t0:t0 + 512, :].rearrange("(tt p) c -> p tt c", p=128),
                                          b_tok[:], "bto", reads=[b_b_tok])
                        deferred.append((c + 3, tr))
            return post

        def tok_units(gi):
            s, t0 = gi // 4, (gi % 4) * 512
            g = gi % 2
            units = []
            for tt in range(4):
                units += tok_tile(gi, s, t0, g, tt)
            return units

        def tok_tile(gi, s, t0, g, tt):
            tsl = slice(tt * 128, (tt + 1) * 128)
            tile_idx = gi * 4 + tt
            st = {}

            def u_v():
                pi = cnt["pm"] % 3
                cnt["pm"] += 1
                for kc in range(8):
                    P.op("pe", lambda e, pi=pi, kc=kc: e.matmul(
                        pm[pi][:], lhsT=hnT[g][:, kc, tsl], rhs=w_bf[:, kc, C_V:C_V + 512],
                        start=(kc == 0), stop=(kc == 7)), reads=[b_w, b_hnT[g]], writes=[b_pm[pi]], sig=(kc == 7))
                vi = cnt["v"] % 2
                cnt["v"] += 1
                P.op("dve", lambda e, vi=vi, pi=pi: e.tensor_copy(out=v_ev[vi][:], in_=pm[pi][:]),
                     reads=[b_pm[pi]], writes=[b_v_ev[vi]])
                P.dma("sp", T["v"][s, t0 + tt * 128:t0 + (tt + 1) * 128, :], v_ev[vi][:], f"vo{vi}",
                      reads=[b_v_ev[vi]])
                st["zi"] = cnt["sz"] % 2
                cnt["sz"] += 1

            def u_z(j):
                zi = st["zi"]
                pi = cnt["pm"] % 3
                cnt["pm"] += 1
                for kc in range(8):
                    P.op("pe", lambda e, pi=pi, kc=kc, j=j: e.matmul(
                        pm[pi][:], lhsT=hnT[g][:, kc, tsl], rhs=w_bf[:, kc, C_Z + j * 512:C_Z + (j + 1) * 512],
                        start=(kc == 0), stop=(kc == 7)),
                        reads=[b_w, b_hnT[g]], writes=[b_pm[pi]], sig=(kc == 7))
                P.op("act", lambda e, zi=zi, pi=pi, j=j: e.activation(
                    out=sz_ev[zi][:, j * 512:(j + 1) * 512], in_=pm[pi][:], func=AF.Silu),
                    reads=[b_pm[pi]], writes=[b_sz_ev[zi]])
                if j == 2:
                    P.dma("sp", T["sz"][s, t0 + tt * 128:t0 + (tt + 1) * 128, :], sz_ev[zi][:], f"szo{zi}",
                          reads=[b_sz_ev[zi]])

            def u_dt():
                for kc in range(8):
                    P.op("pe", lambda e, kc=kc: e.matmul(
                        pd[:, 0:NH_SSM], lhsT=hnT[g][:, kc, tsl], rhs=w_bf[:, kc, C_DT:C_DT + NH_SSM],
                        start=(kc == 0), stop=(kc == 7)), reads=[b_w, b_hnT[g]], writes=[b_pd], sig=(kc == 7))
                P.op("dve", lambda e, tile_idx=tile_idx: e.tensor_tensor(
                    out=dt_all[:, tile_idx, :], in0=pd[:, 0:NH_SSM], in1=dtb[:], op=ALU.add),
                    reads=[b_pd, b_c], writes=[b_dt_all])

            return [u_v, lambda: u_z(0), lambda: u_z(1), lambda: u_z(2), u_dt]

        norm(0)
        norm_pe(0)
        for gi in range(ngroups):
            if gi + 1 < ngroups:
                norm(gi + 1)
            mm(gi, gi + 1 if gi + 1 < ngroups else None)
        P.op("dve", lambda e: e.tensor_scalar_min(out=dt_all[:], in0=dt_all[:], scalar1=60.0),
             reads=[b_dt_all], writes=[b_dt_all])
        P.op("act", lambda e: e.activation(out=dt_e[:], in_=dt_all[:], func=AF.Exp), reads=[b_dt_all], writes=[b_dt_e])
        P.op("act", lambda e: e.activation(out=dt_all[:], in_=dt_e[:], func=AF.Ln, bias=1.0, scale=1.0),
             reads=[b_dt_e], writes=[b_dt_all])
        for s in range(nseq):
            P.dma("sp", T["dt"][s], dt_all[:, s * 16:(s + 1) * 16, :], "dto",
                  reads=[b_dt_all])
        P.barrier()


SLOPES = [2.0 ** (-(h + 1)) for h in range(8)]


def phase2a(P, T, nseq, w_out_bf=None, b_wout=None):
    nc = P.nc
    with contextlib.ExitStack() as es:
        A = Alloc(nc, es)
        ident = A.sb([128, 128], BF16, "ident2")
        cmask = A.sb([128, 2, 256], BF16, "cmask")
        ownmask = A.sb([128, 16, 8], F32, "ownmask")
        kbias = A.sb([128, 8, 2], F32, "kbias")
        pastmask = A.sb([128, 16, 8], F32, "pastmask")
        dmbase = A.sb([128, 16, 8], F32, "dmbase")
        ag = A.sb([128, D_ATT], F32, "ag")
        b_c = P.buf("consts2")
        P.dma("sp", ident[:], T["ident"], "c2", writes=[b_c])
        P.dma("sp", cmask[:], T["cmask"], "c2", writes=[b_c])
        P.dma("sp", ownmask[:], T["ownmask"], "c2", writes=[b_c])
        P.dma("sp", kbias[:], T["kbias"], "c2", writes=[b_c])
        P.dma("sp", pastmask[:], T["pastmask"], "c2", writes=[b_c])
        P.dma("sp", dmbase[:], T["dmbase"], "c2", writes=[b_c])
        P.dma("sp", ag[:], T["attn_g"], "c2", writes=[b_c])
        qa = [A.sb([128, S], BF16, f"qa{i}") for i in range(2)]
        ka = [A.sb([128, S], BF16, f"ka{i}") for i in range(2)]
        b_qa, b_ka = P.bufs(2, "qa"), P.bufs(2, "ka")
        for i in range(2):
            P.op("pool", lambda e, i=i: e.memset(qa[i][64:128, :], 0.0), writes=[b_qa[i]])
            P.op("pool", lambda e, i=i: e.memset(ka[i][64:128, :], 0.0), writes=[b_ka[i]])
        for i in range(2):
            P.dma("sp", ka[i][64:73, :], T["karows"], f"kld{i}", writes=[b_ka[i]])
        vh = [A.sb([128, 16, 65], BF16, f"vh{i}") for i in range(2)]
        b_vh = P.bufs(2, "vh")
        for i in range(2):
            P.op("pool", lambda e, i=i: e.memset(vh[i][:, :, 64:65], 1.0), writes=[b_vh[i]])
        ksum = A.sb([64, 8], F32, "ksum")
        kmT = A.sb([64, 8], BF16, "kmT")
        b_ksum, b_kmT = P.buf(), P.buf()
        Gm = A.sb([128, 16, 8], F32, "Gm")
        top8 = A.sb([128, 16, 8], F32, "top8")
        selm = A.sb([128, 16, 8], F32, "selm")
        selt = A.sb([128, 16, 8], F32, "selt")
        pen = A.sb([128, 16, 8], BF16, "pen")
        b_Gm, b_top8, b_selm, b_selt, b_pen = P.buf(), P.buf(), P.buf(), P.buf(), P.buf()
        PT = [A.sb([128, 256], BF16, f"PT{i}") for i in range(4)]
        b_PT = P.bufs(4, "PT")
        att_tok2 = [A.sb([128, 16, D_ATT], F32, f"att_tok{i}") for i in range(2)]
        b_att2 = P.bufs(2, "att_tok")
        rden = A.sb([128, 2, 2], F32, "rden")
        b_rden = P.bufs(2, "rden")
        junk = A.sb([128, D_ATT], BF16, "junk2")
        b_junk = P.buf()
        ss = A.sb([128, 16], F32, "ss2")
        ssv = A.sb([128, 16], F32, "ssv2")
        ssl = A.sb([128, 16], F32, "ssl2")
        rstd = A.sb([128, 16], F32, "rstd2")
        b_ss, b_ssv, b_ssl, b_rstd = P.buf(), P.buf(), P.buf(), P.buf()
        atn = [A.sb([128, D_ATT], BF16, f"atn{i}") for i in range(2)]
        b_atn = P.bufs(2, "atn")
        pG_ = A.ps([128, 64, 8], F32, "pG")
        pG = pG_[:, 0:16, :]
        b_pG = P.buf("pG")
        ppen = A.ps([8, 1024], BF16, "ppen")
        b_ppen = P.buf("ppen")
        pS = [A.ps([128, 512], F32, f"pS{i}") for i in range(4)]
        b_pS = P.bufs(4, "pS")
        pacc = [A.ps([128, 4, 128], F32, f"pacc{i}") for i in range(2)]
        b_pacc = P.bufs(2, "pacc")
        cnt = {"S": 0, "pen": 0, "atn": 0}

        def load_v(s, h):
            b = h % 2
            P.dma("sp", vh[b][:, :, 0:64], T["v"][s, :, h * 64:(h + 1) * 64].rearrange("(kt p) d -> p kt d", p=128),
                  f"vld{b}", writes=[b_vh[b]])

        def prep1(s, h):
            b = h % 2
            load_v(s, h)
            P.dma("sp", qa[b][0:64, :], T["qk"][s, h], f"qld{b}", writes=[b_qa[b]])
            P.dma("sp", qa[b][72:73, :], T["qrow"][h:h + 1, :], f"qld{b}", writes=[b_qa[b]])
            P.dma("sp", ka[b][0:64, :], T["qk"][s, 8 + h], f"kld{b}", writes=[b_ka[b]])
            P.op("dve", lambda e: e.tensor_reduce(out=ksum[:], in_=ka[b][0:64, :].rearrange("p (j k) -> p j k", k=256),
                                                  axis=AX.X, op=ALU.add), reads=[b_ka[b]], writes=[b_ksum])
            P.op("dve", lambda e: e.tensor_scalar(out=kmT[:], in0=ksum[:], scalar1=1.0 / 256, scalar2=None, op0=ALU.mult),
                 reads=[b_ksum], writes=[b_kmT])
            for t in range(16):
                P.op("pe", lambda e, t=t: e.matmul(pG_[:, t, :], lhsT=qa[b][0:64, t * 128:(t + 1) * 128], rhs=kmT[:],
                                                   start=True, stop=True),
                     reads=[b_qa[b], b_kmT], writes=[b_pG], sig=(t == 15))
            P.op("dve", lambda e: e.tensor_tensor(out=Gm[:], in0=pG, in1=pastmask[:], op=ALU.add),
                 reads=[b_pG, b_c], writes=[b_Gm])
            for t in range(16):
                P.op("dve", lambda e, t=t: e.max(out=top8[:, t, :], in_=Gm[:, t, :]), reads=[b_Gm], writes=[b_top8])
            P.op("dve", lambda e: e.tensor_tensor(out=selm[:], in0=Gm[:], in1=top8[:, :, 2:3].to_broadcast([128, 16, 8]),
                                                  op=ALU.is_ge), reads=[b_Gm, b_top8], writes=[b_selm])
            P.op("dve", lambda e: e.tensor_tensor(out=selm[:], in0=selm[:], in1=ownmask[:], op=ALU.max),
                 reads=[b_selm, b_c], writes=[b_selm])
            P.op("dve", lambda e: e.tensor_scalar(out=selt[:], in0=selm[:], scalar1=-1.0, scalar2=BIG,
                                                  op0=ALU.add, op1=ALU.mult), reads=[b_selm], writes=[b_selt])
            P.op("dve", lambda e: e.scalar_tensor_tensor(out=pen[:], in0=dmbase[:], scalar=-2048.0 * SLOPES[h],
                                                         in1=selt[:], op0=ALU.mult, op1=ALU.add),
                 reads=[b_selt, b_c], writes=[b_pen])

        def prep2(s, h):
            b = h % 2
            for grp in range(4):
                for j in range(4):
                    t = grp * 4 + j
                    P.op("pe", lambda e, t=t, j=j: e.transpose(
                        out=ppen[:, j * 128:(j + 1) * 128], in_=pen[:, t, :], identity=ident[:]),
                        reads=[b_pen, b_c], writes=[b_ppen], sig=(j == 3))
                P.op("dve", lambda e, grp=grp: e.tensor_copy(out=qa[b][64:72, grp * 512:(grp + 1) * 512],
                                                            in_=ppen[:, 0:512]),
                     reads=[b_ppen], writes=[b_qa[b]])

        def qk(s, h, i, kt):
            b = h % 2
            si = cnt["S"] % 4
            cnt["S"] += 1
            Sp = pS[si][:, 0:256]
            qs = slice(i * 256, (i + 1) * 256)
            ks = slice(kt * 128, (kt + 1) * 128)
            diag = (kt // 2 == i)
            P.op("pe", lambda e: e.matmul(Sp, lhsT=ka[b][:, ks], rhs=qa[b][:, qs], start=True, stop=not diag),
                 reads=[b_ka[b], b_qa[b]], writes=[b_pS[si]], sig=not diag)
            if diag:
                P.op("pe", lambda e: e.matmul(Sp, lhsT=ident[:], rhs=cmask[:, kt % 2, :], start=False, stop=True),
                     reads=[b_c], writes=[b_pS[si]])
            P.op("act", lambda e: e.activation(out=PT[si][:], in_=Sp, func=AF.Exp, scale=0.125,
                                               bias=kbias[:, h, kt % 2:kt % 2 + 1]),
                 reads=[b_pS[si], b_c], writes=[b_PT[si]])
            return si

        def pv(s, h, i, kt, si):
            last = 2 * i + 1
            a = i % 2
            for u in range(2):
                P.op("pe", lambda e, u=u: e.matmul(pacc[a][:, u, 0:65], lhsT=PT[si][:, u * 128:(u + 1) * 128],
                                                   rhs=vh[h % 2][:, kt, :], start=(kt == 0 and u == 0), stop=(kt == last),
                                                   skip_group_check=True),
                     reads=[b_PT[si], b_vh[h % 2]], writes=[b_pacc[a]], sig=(u == 1))
            if kt == last:
                P.op("dve", lambda e: e.reciprocal(out=rden[:, a, :], in_=pacc[a][:, 0:2, 64]),
                     reads=[b_pacc[a]], writes=[b_rden[a]])
                for u in range(2):
                    P.op("dve", lambda e, u=u: e.tensor_scalar(
                        out=att_tok2[s % 2][:, 2 * i + u, h * 64:(h + 1) * 64], in0=pacc[a][:, u, 0:64],
                        scalar1=rden[:, a, u:u + 1], scalar2=None, op0=ALU.mult),
                        reads=[b_pacc[a], b_rden[a]], writes=[b_att2[s % 2]])

        def main(s, h, hooks):
            steps = [(i, kt) for i in range(8) for kt in range(2 * i + 2)]
            look = 3
            sis = {}
            for n in range(min(look, len(steps))):
                sis[n] = qk(s, h, *steps[n])
            for n, (i, kt) in enumerate(steps):
                if n + look < len(steps):
                    sis[n + look] = qk(s, h, *steps[n + look])
                pv(s, h, i, kt, sis.pop(n))
                if kt == 2 * i + 1 and i in hooks:
                    hooks[i]()

        def finish_seq(s):
            att_tok, b_att = att_tok2[s % 2], b_att2[s % 2]
            for t in range(16):
                P.op("act", lambda e, t=t: e.activation(out=junk[:], in_=att_tok[:, t, :], func=AF.Square,
                                                       accum_out=ss[:, t:t + 1]), reads=[b_att], writes=[b_junk, b_ss])
            P.op("dve", lambda e: e.tensor_scalar(out=ssv[:], in0=ss[:], scalar1=1.0 / D_ATT, scalar2=EPS,
                                                  op0=ALU.mult, op1=ALU.add), reads=[b_ss], writes=[b_ssv])
            P.op("act", lambda e: e.activation(out=ssl[:], in_=ssv[:], func=AF.Ln), reads=[b_ssv], writes=[b_ssl])
            P.op("act", lambda e: e.activation(out=rstd[:], in_=ssl[:], func=AF.Exp, scale=-0.5),
                 reads=[b_ssl], writes=[b_rstd])
            for t in range(16):
                ai = cnt["atn"] % 2
                cnt["atn"] += 1
                P.op("dve", lambda e, t=t, ai=ai: e.tensor_scalar(
                    out=atn[ai][:], in0=att_tok[:, t, :], scalar1=rstd[:, t:t + 1], scalar2=None, op0=ALU.mult),
                    reads=[b_att, b_rstd], writes=[b_atn[ai]])
                P.dma("sp", T["mixed"][s, t * 128:(t + 1) * 128, 0:D_ATT], atn[ai][:], f"atno{ai}", reads=[b_atn[ai]])

        wq = []
        if w_out_bf is not None:
            gcol = A.sb([128, 16], F32, "gcol")
            b_gcol = P.buf("gcol")
            P.dma("sp", gcol[:], T["mix_g"], "gcol", writes=[b_gcol])
            stage = [A.sb([128, D], F32, f"wstage{i}") for i in range(2)]
            b_stage = P.bufs(2)

            def wload(kc):
                si = kc % 2
                P.dma("sp", stage[si][:], T["w_out"][kc * 128:(kc + 1) * 128, :], f"wst{si}", writes=[b_stage[si]])
                P.op("act", lambda e: e.activation(out=w_out_bf[:, kc, :], in_=stage[si][:], func=AF.Copy,
                                                   scale=gcol[:, kc:kc + 1]),
                     reads=[b_stage[si], b_gcol], writes=[b_wout])
            wq = [lambda kc=kc: wload(kc) for kc in range(16)]

        heads = [(s, h) for s in range(nseq) for h in range(8)]
        prep1(*heads[0])
        prep2(*heads[0])
        pending_fin = []
        for n, (s, h) in enumerate(heads):
            hooks = {}
            extra = {}
            if wq:
                for i in (0, 2, 4, 6):
                    if wq:
                        extra[i] = wq.pop(0)
            if pending_fin:
                extra[1] = pending_fin.pop(0)
            nxt = heads[n + 1] if n + 1 < len(heads) else None

            def mk(i, nxt=nxt, extra=extra):
                def f():
                    if i in extra:
                        extra[i]()
                    if nxt is not None and i == 3:
                        prep1(*nxt)
                    if nxt is not None and i == 5:
                        prep2(*nxt)
                return f
            for i in range(8):
                hooks[i] = mk(i)
            main(s, h, hooks)
            if h == 7:
                pending_fin.append(lambda s=s: finish_seq(s))
        while wq:
            wq.pop(0)()
        while pending_fin:
            pending_fin.pop(0)()
        P.barrier()


def phase2b(P, T, nseq):
    nc = P.nc
    with contextlib.ExitStack() as es:
        A = Alloc(nc, es)
        U = A.sb([128, 128], F32, "U")
        Ls = A.sb([128, 128], F32, "Ls")
        ones = A.sb([128, 128], F32, "ones")
        alog = A.sb([128, NH_SSM], F32, "alog")
        arep = A.sb([128, NH_SSM], F32, "arep")
        dsk = A.sb([128, NH_SSM], F32, "dsk")
        sg = A.sb([128, D_SSM], F32, "sg")
        b_c = P.buf("consts2b")
        P.dma("sp", U[:], T["U"], "c3", writes=[b_c])
        P.dma("sp", Ls[:], T["Ls"], "c3", writes=[b_c])
        P.dma("sp", ones[:], T["ones"], "c3", writes=[b_c])
        P.dma("sp", alog[:], T["a_log"], "c3", writes=[b_c])
        P.dma("sp", dsk[:], T["d_skip"], "c3", writes=[b_c])
        P.dma("sp", sg[:], T["ssm_g"], "c3", writes=[b_c])
        b_a = P.buf("arep")
        P.op("act", lambda e: e.activation(out=arep[:], in_=alog[:], func=AF.Exp), reads=[b_c], writes=[b_a])
        P.op("dve", lambda e: e.tensor_scalar(out=arep[:], in0=arep[:], scalar1=-1.0, scalar2=None, op0=ALU.mult),
             reads=[b_a], writes=[b_a])

        def two(shape, dt, name, n=2):
            return [A.sb(shape, dt, f"{name}{i}") for i in range(n)]
        xs = two([128, D_SSM], BF16, "xs", 3); b_xs = P.bufs(3)
        bt = two([128, 512], BF16, "bt", 3); b_bt = P.bufs(3)
        bT = two([128, 4, 128], BF16, "bT", 3); b_bT = P.bufs(3)
        cT = two([128, 4, 128], BF16, "cT", 3); b_cT = P.bufs(3)
        sz = two([128, D_SSM], BF16, "sz", 3); b_sz = P.bufs(3)
        dt = two([128, NH_SSM], F32, "dt", 3); b_dt = P.bufs(3)
        adt = two([128, NH_SSM], F32, "adt"); b_adt = P.bufs(2)
        ecs3 = two([128, 72], F32, "ecs3"); b_ecs3 = P.bufs(2)
        Dm = two([128, NH_SSM, 128], F32, "Dm"); b_Dm = P.bufs(2)
        xdtw = two([128, D_SSM], BF16, "xdtw"); b_xdtw = P.bufs(2)
        cbm = two([128, 4, 128], BF16, "cbm"); b_cbm = P.bufs(2)
        lndt = two([128, NH_SSM], F32, "lndt"); b_lndt = P.bufs(2)
        dtw = two([128, NH_SSM], F32, "dtw"); b_dtw = P.bufs(2)
        M = two([128, NH_SSM, 128], BF16, "M")
        b_M = [P.bufs(8), P.bufs(8)]
        E = two([128, 3, 128], BF16, "E", 4); b_E = P.bufs(4)
        hst = A.sb([128, D_SSM], F32, "hst"); b_h = P.bufs(4, "h")
        hbf = A.sb([128, D_SSM], BF16, "hbf"); b_hbf = P.bufs(4, "hbf")
        yo = A.sb([128, 384], F32, "yo"); b_yo = P.buf()
        yall2 = two([128, D_SSM], F32, "yall"); b_y2 = [P.bufs(4), P.bufs(4)]
        junk = A.sb([128, 384], BF16, "junk3"); b_junk = P.buf()
        ss2 = two([128, 4], F32, "ss3"); b_ss2 = P.bufs(2); ssv = A.sb([128, 4], F32, "ssv3")
        ssl = A.sb([128, 4], F32, "ssl3"); rstd = A.sb([128, 4], F32, "rstd3")
        b_ssv, b_ssl, b_rstd = P.buf(), P.buf(), P.buf()
        ymix = two([128, D_SSM], BF16, "ymix"); b_ymix = P.bufs(2)
        pcb = A.ps([128, 4, 128], F32, "pcb"); b_pcb = P.buf()
        pseg = [A.ps([128, 512], F32, f"pseg{i}") for i in range(2)]; b_pseg = P.bufs(2)
        pyd2 = [A.ps([128, 512], F32, f"pyd{i}") for i in range(2)]; b_pyd2 = P.bufs(2)
        pyo2 = [A.ps([128, 512], F32, f"pyo{i}") for i in range(2)]; b_pyo2 = P.bufs(2)
        pst = A.ps([128, 512], F32, "pst"); b_pst = P.buf()
        pc = pst[:, 384:512]; b_pc = b_pst
        cnt = {"seg": 0}
        pend_dve, pend_act = [], []

        def flush(lst):
            while lst:
                lst.pop(0)()

        def bc(ap, shape, axis):
            return ap.unsqueeze(axis).to_broadcast(shape)

        identf = A.sb([128, 128], F32, "identf")
        dfull = A.sb([128, 128], F32, "dfull")
        dhif = A.sb([128, 128], F32, "dhif")
        Dhi = A.sb([128, NH_SSM, 128], BF16, "Dhi")
        Dlo = A.sb([128, NH_SSM, 128], BF16, "Dlo")
        b_idf, b_dfull, b_dhif, b_Dhl = P.buf(), P.buf(), P.buf(), P.buf()
        P.dma("sp", identf[:], T["identf"], "c3i", writes=[b_idf])
        for h in range(NH_SSM):
            P.op("dve", lambda e, h=h: e.tensor_scalar(out=dfull[:], in0=identf[:], scalar1=dsk[:, h:h + 1], scalar2=None,
                                                       op0=ALU.mult), reads=[b_idf, b_c], writes=[b_dfull])
            P.op("dve", lambda e, h=h: e.tensor_copy(out=Dhi[:, h, :], in_=dfull[:]), reads=[b_dfull], writes=[b_Dhl])
            P.op("dve", lambda e, h=h: e.tensor_copy(out=dhif[:], in_=Dhi[:, h, :]), reads=[b_Dhl], writes=[b_dhif])
            P.op("dve", lambda e, h=h: e.tensor_tensor(out=Dlo[:, h, :], in0=dfull[:], in1=dhif[:], op=ALU.subtract),
                 reads=[b_dfull, b_dhif], writes=[b_Dhl])

        def load(s, c):
            k = (s * 16 + c) % 3
            t0 = c * 128
            P.dma("sp", xs[k][:], T["xs"][s, t0:t0 + 128, :], f"l_xs{k}", writes=[b_xs[k]])
            P.dma("sp", bt[k][:], T["btok"][s, t0:t0 + 128, :], f"l_bt{k}", writes=[b_bt[k]])
            P.dma("sp", bT[k][:], T["bT"][s, :, t0:t0 + 128].rearrange("(g n) l -> n g l", n=128), f"l_bT{k}",
                  writes=[b_bT[k]])
            P.dma("sp", cT[k][:], T["cT"][s, :, t0:t0 + 128].rearrange("(g n) l -> n g l", n=128), f"l_cT{k}",
                  writes=[b_cT[k]])
            P.dma("sp", sz[k][:], T["sz"][s, t0:t0 + 128, :], f"l_sz{k}", writes=[b_sz[k]])
            P.dma("sp", dt[k][:], T["dt"][s, :, c, :], f"l_dt{k}", writes=[b_dt[k]])

        def prefront(s, c):
            k = (s * 16 + c) % 2
            kl = (s * 16 + c) % 3
            P.op("dve", lambda e: e.tensor_tensor(out=adt[k][:], in0=dt[kl][:], in1=arep[:], op=ALU.mult),
                 reads=[b_dt[kl], b_a], writes=[b_adt[k]])
            for j, lm in enumerate((U, Ls, ones)):
                P.op("pe", lambda e, j=j, lm=lm: e.matmul(pc[:, j * 24:(j + 1) * 24], lhsT=lm[:], rhs=adt[k][:],
                                                          start=True, stop=True),
                     reads=[b_c, b_adt[k]], writes=[b_pc], sig=(j == 2))
            P.op("act", lambda e: e.activation(out=ecs3[k][:], in_=pc[:, 0:72], func=AF.Exp),
                 reads=[b_pc], writes=[b_ecs3[k]])
            P.op("act", lambda e: e.activation(out=lndt[k][:], in_=dt[kl][:], func=AF.Ln), reads=[b_dt[kl]], writes=[b_lndt[k]])
            P.op("pool", lambda e: e.tensor_tensor(out=Dm[k][:], in0=bc(U[:], [128, NH_SSM, 128], 1),
                                                   in1=bc(adt[k][:], [128, NH_SSM, 128], 2), op=ALU.mult),
                 reads=[b_c, b_adt[k]], writes=[b_Dm[k]])
            P.op("dve", lambda e: e.tensor_tensor(out=dtw[k][:], in0=dt[kl][:], in1=ecs3[k][:, 24:48], op=ALU.mult),
                 reads=[b_dt[kl], b_ecs3[k]], writes=[b_dtw[k]])
            P.op("pool", lambda e: e.tensor_tensor(out=xdtw[k][:].rearrange("p (h d) -> p h d", d=64),
                                                   in0=xs[kl][:].rearrange("p (h d) -> p h d", d=64),
                                                   in1=bc(dtw[k][:], [128, NH_SSM, 64], 2), op=ALU.mult),
                 reads=[b_xs[kl], b_dtw[k]], writes=[b_xdtw[k]])
            for g in range(4):
                P.op("pe", lambda e, g=g: e.matmul(pcb[:, g, :], lhsT=bT[kl][:, g, :], rhs=cT[kl][:, g, :],
                                                   start=True, stop=True),
                     reads=[b_bT[kl], b_cT[kl]], writes=[b_pcb], sig=(g == 3))
            P.op("dve", lambda e: e.tensor_tensor(out=cbm[k][:], in0=pcb[:], in1=bc(U[:], [128, 4, 128], 1), op=ALU.mult),
                 reads=[b_pcb, b_c], writes=[b_cbm[k]])

        def segEM(s, c, hg):
            k = (s * 16 + c) % 2
            if True:
                g = hg // 2
                si = cnt["seg"] % 2
                ei = cnt["seg"] % 4
                cnt["seg"] += 1
                P.op("pe", lambda e, hg=hg, si=si: e.matmul(
                    pseg[si][:, 0:384], lhsT=Ls[:], rhs=Dm[k][:, hg * 3:(hg + 1) * 3, :], start=True, stop=True),
                    reads=[b_c, b_Dm[k]], writes=[b_pseg[si]])
                for j in range(3):
                    h = hg * 3 + j
                    P.op("act", lambda e, si=si, ei=ei, j=j, h=h: e.activation(
                        out=E[ei][:, j, :], in_=pseg[si][:, j * 128:(j + 1) * 128], func=AF.Exp, bias=lndt[k][:, h:h + 1]),
                        reads=[b_pseg[si], b_lndt[k]], writes=[b_E[ei]])
                pend_dve.append(lambda hg=hg, ei=ei, g=g, k=k: P.op("dve", lambda e: e.tensor_tensor(
                    out=M[k][:, hg * 3:(hg + 1) * 3, :], in0=E[ei][:], in1=bc(cbm[k][:, g, :], [128, 3, 128], 1),
                    op=ALU.mult), reads=[b_E[ei], b_cbm[k]], writes=[b_M[k][hg]]))

        def back_g(s, c, g):
            k = (s * 16 + c) % 2
            kl = (s * 16 + c) % 3
            yall, b_y, ss, b_ss = yall2[k], b_y2[k], ss2[k], b_ss2[k]
            pyd, b_pyd, pyo, b_pyo = pyd2[g % 2], b_pyd2[g % 2], pyo2[g % 2], b_pyo2[g % 2]
            if True:
                gs = slice(g * 384, (g + 1) * 384)
                hs = slice(g * 6, (g + 1) * 6)
                for kk in range(6):
                    h = g * 6 + kk
                    for mi, lm in enumerate((M[k], Dhi, Dlo)):
                        P.op("pe", lambda e, kk=kk, h=h, mi=mi, lm=lm: e.matmul(
                            pyd[:, kk * 64:(kk + 1) * 64], lhsT=lm[:, h, :], rhs=xs[kl][:, h * 64:(h + 1) * 64],
                            start=(mi == 0), stop=(mi == 2)),
                            reads=[b_M[k][h // 3], b_xs[kl], b_Dhl], writes=[b_pyd], sig=(kk == 5 and mi == 2))
                if c > 0:
                    P.op("pe", lambda e: e.matmul(pyo[:, 0:384], lhsT=cT[kl][:, g, :], rhs=hbf[:, gs], start=True, stop=True),
                         reads=[b_cT[kl], b_hbf[g]], writes=[b_pyo])
                if c < 15:
                    P.op("pe", lambda e: e.matmul(pst[:, 0:384], lhsT=bt[kl][:, g * 128:(g + 1) * 128], rhs=xdtw[k][:, gs],
                                                  start=True, stop=True), reads=[b_bt[kl], b_xdtw[k]], writes=[b_pst])
                    if c > 0:
                        P.op("pool", lambda e: e.tensor_tensor(
                            out=hst[:, gs].rearrange("p (h d) -> p h d", d=64),
                            in0=hst[:, gs].rearrange("p (h d) -> p h d", d=64),
                            in1=bc(ecs3[k][:, 48 + g * 6:48 + (g + 1) * 6], [128, 6, 64], 2), op=ALU.mult),
                            reads=[b_h[g], b_ecs3[k]], writes=[b_h[g]])
                if c > 0:
                    P.op("dve", lambda e: e.tensor_tensor(
                        out=yo[:].rearrange("p (h d) -> p h d", d=64),
                        in0=pyo[:, 0:384].rearrange("p (h d) -> p h d", d=64),
                        in1=bc(ecs3[k][:, hs], [128, 6, 64], 2), op=ALU.mult),
                        reads=[b_pyo, b_ecs3[k]], writes=[b_yo])
                    P.op("dve", lambda e: e.tensor_tensor(out=yall[:, gs], in0=pyd[:, 0:384], in1=yo[:], op=ALU.add),
                         reads=[b_pyd, b_yo], writes=[b_y[g]])
                    P.op("dve", lambda e: e.tensor_tensor(out=yall[:, gs], in0=yall[:, gs], in1=sz[kl][:, gs], op=ALU.mult),
                         reads=[b_y[g], b_sz[kl]], writes=[b_y[g]])
                else:
                    P.op("dve", lambda e: e.tensor_tensor(out=yall[:, gs], in0=pyd[:, 0:384], in1=sz[kl][:, gs], op=ALU.mult),
                         reads=[b_pyd, b_sz[kl]], writes=[b_y[g]])
                pend_act.append(lambda: P.op("act", lambda e: e.activation(
                    out=junk[:], in_=yall[:, gs], func=AF.Square, accum_out=ss[:, g:g + 1]),
                    reads=[b_y[g]], writes=[b_junk, b_ss]))
                if c < 15:
                    if c > 0:
                        P.op("dve", lambda e: e.tensor_tensor(out=hst[:, gs], in0=pst[:, 0:384], in1=hst[:, gs], op=ALU.add),
                             reads=[b_pst, b_h[g]], writes=[b_h[g]])
                    else:
                        P.op("dve", lambda e: e.tensor_copy(out=hst[:, gs], in_=pst[:, 0:384]),
                             reads=[b_pst], writes=[b_h[g]])
                    pend_act.append(lambda: P.op("act", lambda e: e.copy(out=hbf[:, gs], in_=hst[:, gs]),
                                                 reads=[b_h[g]], writes=[b_hbf[g]]))
                flush(pend_dve)

        def tail(s, c):
            k = (s * 16 + c) % 2
            t0 = c * 128
            yall, b_y, ss, b_ss = yall2[k], b_y2[k], ss2[k], b_ss2[k]
            P.op("dve", lambda e: e.tensor_scalar(out=ssv[:], in0=ss[:], scalar1=1.0 / 384, scalar2=EPS,
                                                  op0=ALU.mult, op1=ALU.add), reads=[b_ss], writes=[b_ssv])
            P.op("act", lambda e: e.activation(out=ssl[:], in_=ssv[:], func=AF.Ln), reads=[b_ssv], writes=[b_ssl])
            P.op("act", lambda e: e.activation(out=rstd[:], in_=ssl[:], func=AF.Exp, scale=-0.5),
                 reads=[b_ssl], writes=[b_rstd])
            for g in range(4):
                gs = slice(g * 384, (g + 1) * 384)
                P.op("act", lambda e, g=g: e.activation(out=ymix[k][:, gs], in_=yall[:, gs], func=AF.Copy,
                                                       scale=rstd[:, g:g + 1]),
                     reads=[b_y[g], b_rstd], writes=[b_ymix[k]])
            P.dma("sp", T["mixed"][s, t0:t0 + 128, D_ATT:2048], ymix[k][:], f"ymo{k}", reads=[b_ymix[k]])

        chunks = [(s, c) for s in range(nseq) for c in range(16)]
        N = len(chunks)
        load(*chunks[0])
        if N > 1:
            load(*chunks[1])
        prefront(*chunks[0])
        for hg in range(8):
            segEM(*chunks[0], hg)
            flush(pend_dve)
        for n, (s, c) in enumerate(chunks):
            if n + 2 < N:
                load(*chunks[n + 2])
            if n + 1 < N:
                prefront(*chunks[n + 1])
            for g in range(4):
                back_g(s, c, g)
                if n + 1 < N:
                    segEM(*chunks[n + 1], 2 * g)
                    segEM(*chunks[n + 1], 2 * g + 1)
                flush(pend_act)
                if g == 0 and n > 0:
                    tail(*chunks[n - 1])
            flush(pend_dve)
        flush(pend_act)
        tail(*chunks[N - 1])
        P.barrier()


def load_weight(P, T, name, w_bf, nk, buf, key):
    for kc in range(nk):
        P.dma("pool", w_bf[:, kc, :], T[name][kc * 128:(kc + 1) * 128, :], key, writes=[buf])


def phase3a(P, T, nseq, w_out_bf, b_wout):
    nc = P.nc
    with contextlib.ExitStack() as es:
        A = Alloc(nc, es)
        ident = A.sb([128, 128], BF16, "ident3")
        g2 = A.sb([128, D], F32, "g2rep")
        b_c = P.buf("consts3a")
        P.dma("sp", ident[:], T["ident"], "c4", writes=[b_c])
        P.dma("sp", g2[:], T["ln2_g"], "c4", writes=[b_c])
        mx = [A.sb([128, 2048], BF16, f"mx{i}") for i in range(3)]; b_mx = P.bufs(3)
        xin = [A.sb([128, D], F32, f"x3in{i}") for i in range(3)]; b_xin = P.bufs(3)
        mxT = [A.sb([128, 16, 128], BF16, f"mxT{i}") for i in range(2)]; b_mxT = P.bufs(2)
        x1 = [A.sb([128, D], F32, f"x1_{i}") for i in range(2)]; b_x1 = P.bufs(2)
        junk = A.sb([128, D], BF16, "junk4"); b_junk = P.buf()
        ss = [A.sb([128, 1], F32, f"ss4_{i}") for i in range(2)]
        ssv = [A.sb([128, 1], F32, f"ssv4_{i}") for i in range(2)]
        ssl = [A.sb([128, 1], F32, f"ssl4_{i}") for i in range(2)]
        rstd = [A.sb([128, 1], F32, f"rstd4_{i}") for i in range(2)]
        b_ss, b_ssv, b_ssl, b_rstd = P.bufs(2), P.bufs(2), P.bufs(2), P.bufs(2)
        h2 = [A.sb([128, D], BF16, f"h2_{i}") for i in range(2)]; b_h2 = P.bufs(2)
        h2T = [A.sb([128, 8, 512], BF16, f"h2T{i}") for i in range(2)]; b_h2T = P.bufs(2)
        pmt = [A.ps([128, 8, 128], BF16, f"pmt{i}") for i in range(2)]; b_pmt = P.bufs(2)
        po = [A.ps([128, 512], F32, f"po{i}") for i in range(4)]; b_po = P.bufs(4)
        ph = A.ps([128, 8, 128], BF16, "ph"); b_ph = P.buf()
        cnt = {"pmt": 0}
        ntiles = nseq * 16

        def load(n):
            s, t0, k = n // 16, (n % 16) * 128, n % 3
            P.dma("sp", mx[k][:], T["mixed"][s, t0:t0 + 128, :], f"l_mx{k}", writes=[b_mx[k]])
            P.dma("pool", xin[k][:], T["x"][s, t0:t0 + 128, :], f"l_x3{k}", writes=[b_xin[k]])

        def transposes(n):
            k = n % 2
            k3 = n % 3
            for half in range(2):
                pi = cnt["pmt"] % 2
                cnt["pmt"] += 1
                for j in range(8):
                    kc = half * 8 + j
                    P.op("pe", lambda e, j=j, kc=kc, pi=pi: e.transpose(
                        out=pmt[pi][:, j, :], in_=mx[k3][:, kc * 128:(kc + 1) * 128], identity=ident[:]),
                        reads=[b_mx[k3], b_c], writes=[b_pmt[pi]], sig=(j == 7))
                P.op("act", lambda e, half=half, pi=pi: e.copy(out=mxT[k][:, half * 8:(half + 1) * 8, :], in_=pmt[pi][:]),
                     reads=[b_pmt[pi]], writes=[b_mxT[k]])

        load(0)
        if ntiles > 1:
            load(1)
        transposes(0)
        tails = []
        for n in range(ntiles):
            s, t0, k = n // 16, (n % 16) * 128, n % 2
            if n + 2 < ntiles:
                load(n + 2)
            if n + 1 < ntiles:
                transposes(n + 1)
            for nh in range(2):
                pi = k * 2 + nh
                for kc in range(16):
                    P.op("pe", lambda e, kc=kc, nh=nh, pi=pi: e.matmul(
                        po[pi][:], lhsT=mxT[k][:, kc, :], rhs=w_out_bf[:, kc, nh * 512:(nh + 1) * 512],
                        start=(kc == 0), stop=(kc == 15)), reads=[b_mxT[k], b_wout], writes=[b_po[pi]], sig=(kc == 15))
                P.op("dve", lambda e, nh=nh, pi=pi: e.tensor_tensor(
                    out=x1[k][:, nh * 512:(nh + 1) * 512], in0=po[pi][:], in1=xin[n % 3][:, nh * 512:(nh + 1) * 512], op=ALU.add),
                    reads=[b_po[pi], b_xin[n % 3]], writes=[b_x1[k]])
            P.dma("sp", T["out"][s, t0:t0 + 128, :], x1[k][:], f"x1o{k}", reads=[b_x1[k]])
            while tails:
                tails.pop(0)()
            P.op("act", lambda e: e.activation(out=junk[:], in_=x1[k][:], func=AF.Square, accum_out=ss[k][:]),
                 reads=[b_x1[k]], writes=[b_junk, b_ss[k]])
            P.op("dve", lambda e: e.tensor_scalar(out=ssv[k][:], in0=ss[k][:], scalar1=1.0 / D, scalar2=EPS,
                                                  op0=ALU.mult, op1=ALU.add), reads=[b_ss[k]], writes=[b_ssv[k]])
            P.op("act", lambda e: e.activation(out=ssl[k][:], in_=ssv[k][:], func=AF.Ln), reads=[b_ssv[k]], writes=[b_ssl[k]])
            P.op("act", lambda e: e.activation(out=rstd[k][:], in_=ssl[k][:], func=AF.Exp, scale=-0.5),
                 reads=[b_ssl[k]], writes=[b_rstd[k]])
            P.op("dve", lambda e: e.scalar_tensor_tensor(out=h2[k][:], in0=x1[k][:], scalar=rstd[k][:, 0:1], in1=g2[:],
                                                         op0=ALU.mult, op1=ALU.mult),
                 reads=[b_x1[k], b_rstd[k], b_c], writes=[b_h2[k]])
            def tail(n=n, s=s, t0=t0, k=k):
                for kc in range(8):
                    P.op("pe", lambda e, kc=kc: e.transpose(out=ph[:, kc, :], in_=h2[k][:, kc * 128:(kc + 1) * 128],
                                                            identity=ident[:]),
                         reads=[b_h2[k], b_c], writes=[b_ph], sig=(kc == 7))
                gslot = (n // 4) % 2
                tt = n % 4
                P.op("act", lambda e: e.copy(out=h2T[gslot][:, :, tt * 128:(tt + 1) * 128], in_=ph[:]),
                     reads=[b_ph], writes=[b_h2T[gslot]])
                if tt == 3:
                    g0 = t0 - 384
                    P.dma("sp", T["h2T"][s, :, g0:g0 + 512].rearrange("(kc p) t -> p kc t", p=128), h2T[gslot][:],
                          f"h2To{gslot}", reads=[b_h2T[gslot]])
            tails.append(tail)
        while tails:
            tails.pop(0)()
        P.barrier()


def phase3b(P, T, nseq, w_dn_bf, b_wdn):
    nc = P.nc
    NP = D_FF // 128
    with contextlib.ExitStack() as es:
        A = Alloc(nc, es)
        w_up_bf = A.sb([128, 8, 2 * D_FF], BF16, "w_up_bf")
        b_wup = P.buf("w_up")
        load_weight(P, T, "w_up", w_up_bf, 8, b_wup, "w3")
        gf = A.sb([128, D], F32, "gfrep")
        cw = A.sb([128, 2, NP, 3], F32, "cw3")
        cb = A.sb([128, 2, NP], F32, "cb3")
        b_c = P.buf("consts3b")
        P.dma("sp", gf[:], T["lnf_g"], "c5", writes=[b_c])
        P.dma("sp", cw[:], T["ffn_cw"], "c5", writes=[b_c])
        P.dma("sp", cb[:], T["ffn_cb"], "c5", writes=[b_c])
        h2T = [A.sb([128, 8, 256], BF16, f"h2Tb{i}") for i in range(2)]; b_h2T = P.bufs(2)
        x1 = [A.sb([128, D], F32, f"x1b{i}") for i in range(2)]; b_x1 = P.bufs(2)
        gT = [A.sb([128, NP, 256], BF16, f"gT{i}") for i in range(2)]; b_gT = P.bufs(2)
        pre = [A.sb([128, 2, 258], F32, f"preb{i}") for i in range(2)]; b_pre = P.bufs(2)
        accg = [A.sb([128, 256], F32, f"accg{i}") for i in range(2)]; b_accg = P.bufs(2)
        accv = [A.sb([128, 256], F32, f"accv{i}") for i in range(2)]; b_accv = P.bufs(2)
        sgt = [A.sb([128, 256], F32, f"sgt{i}") for i in range(2)]; b_sgt = P.bufs(2)
        hist = A.sb([128, 2, NP, 2], F32, "histb"); b_hist = P.bufs(NP)
        x2 = [A.sb([128, D], F32, f"x2_{i}") for i in range(2)]; b_x2 = P.bufs(2)
        junk = A.sb([128, D], BF16, "junk5"); b_junk = P.buf()
        ss = [A.sb([128, 1], F32, f"ss5_{i}") for i in range(2)]
        ssv = [A.sb([128, 1], F32, f"ssv5_{i}") for i in range(2)]
        ssl = [A.sb([128, 1], F32, f"ssl5_{i}") for i in range(2)]
        rstd = [A.sb([128, 1], F32, f"rstd5_{i}") for i in range(2)]
        b_ss, b_ssv, b_ssl, b_rstd = P.bufs(2), P.bufs(2), P.bufs(2), P.bufs(2)
        pu = [A.ps([128, 2, 256], F32, f"pu{i}") for i in range(3)]; b_pu = P.bufs(3)
        pdn = [A.ps([128, 512], F32, f"pdn{i}") for i in range(4)]; b_pdn = P.bufs(4)
        cnt = {"pu": 0, "pre": 0}
        posts = []
        ngroups = nseq * 8

        def load(n):
            s, t0, k = n // 8, (n % 8) * 256, n % 2
            P.dma("sp", h2T[k][:], T["h2T"][s, :, t0:t0 + 256].rearrange("(kc p) t -> p kc t", p=128), f"l_h2T{k}",
                  writes=[b_h2T[k]])

        def up_pair(n, p):
            s, t0, k = n // 8, (n % 8) * 256, n % 2
            pi = cnt["pu"] % 3
            cnt["pu"] += 1
            for half in range(2):
                col0 = half * D_FF + p * 128
                for kc in range(8):
                    P.op("pe", lambda e, kc=kc, half=half, col0=col0: e.matmul(
                        pu[pi][:, half, :], lhsT=w_up_bf[:, kc, col0:col0 + 128], rhs=h2T[k][:, kc, :],
                        start=(kc == 0), stop=(kc == 7)),
                        reads=[b_wup, b_h2T[k]], writes=[b_pu[pi]], sig=(kc == 7 and half == 1))
            pr = cnt["pre"] % 2
            cnt["pre"] += 1
            P.op("act", lambda e: e.copy(out=pre[pr][:, :, 2:258], in_=pu[pi][:]), reads=[b_pu[pi]], writes=[b_pre[pr]])
            P.op("act", lambda e: e.activation(out=accg[pr][:], in_=pu[pi][:, 0, :], func=AF.Identity,
                                               scale=cw[:, 0, p, 2:3], bias=cb[:, 0, p:p + 1]),
                 reads=[b_pu[pi], b_c], writes=[b_accg[pr]])
            P.op("act", lambda e: e.activation(out=accv[pr][:], in_=pu[pi][:, 1, :], func=AF.Identity,
                                               scale=cw[:, 1, p, 2:3], bias=cb[:, 1, p:p + 1]),
                 reads=[b_pu[pi], b_c], writes=[b_accv[pr]])
            while posts:
                posts.pop(0)()
            if t0 == 0:
                P.op("dve", lambda e: e.memset(pre[pr][:, :, 0:2], 0.0), writes=[b_pre[pr]])
            else:
                P.op("dve", lambda e: e.tensor_copy(out=pre[pr][:, :, 0:2], in_=hist[:, :, p, :]),
                     reads=[b_hist[p]], writes=[b_pre[pr]])
            for kk in (1, 0):
                P.op("dve", lambda e, kk=kk: e.scalar_tensor_tensor(
                    out=accg[pr][:], in0=pre[pr][:, 0, kk:kk + 256], scalar=cw[:, 0, p, kk:kk + 1],
                    in1=accg[pr][:], op0=ALU.mult, op1=ALU.add),
                    reads=[b_pre[pr], b_c, b_accg[pr]], writes=[b_accg[pr]])
                P.op("dve", lambda e, kk=kk: e.scalar_tensor_tensor(
                    out=accv[pr][:], in0=pre[pr][:, 1, kk:kk + 256], scalar=cw[:, 1, p, kk:kk + 1],
                    in1=accv[pr][:], op0=ALU.mult, op1=ALU.add),
                    reads=[b_pre[pr], b_c, b_accv[pr]], writes=[b_accv[pr]])
            P.op("dve", lambda e: e.tensor_copy(out=hist[:, :, p, :], in_=pre[pr][:, :, 256:258]),
                 reads=[b_pre[pr]], writes=[b_hist[p]])
            def post():
                P.op("act", lambda e: e.activation(out=sgt[pr][:], in_=accg[pr][:], func=AF.Silu),
                     reads=[b_accg[pr]], writes=[b_sgt[pr]])
                P.op("pool", lambda e: e.tensor_tensor(out=gT[k][:, p, :], in0=sgt[pr][:], in1=accv[pr][:], op=ALU.mult),
                     reads=[b_sgt[pr], b_accv[pr]], writes=[b_gT[k]])
            posts.append(post)
            if p == NP - 1:
                while posts:
                    posts.pop(0)()

        def down_mm(n, m):
            s, t0, k = n // 8, (n % 8) * 256, n % 2
            tt, nh, fc = m // 44, (m // 22) % 2, m % 22
            pi = tt * 2 + nh
            P.op("pe", lambda e: e.matmul(
                pdn[pi][:], lhsT=gT[k][:, fc, tt * 128:(tt + 1) * 128], rhs=w_dn_bf[:, fc, nh * 512:(nh + 1) * 512],
                start=(fc == 0), stop=(fc == NP - 1)),
                reads=[b_gT[k], b_wdn], writes=[b_pdn[pi]], sig=(fc == NP - 1))
            if fc == NP - 1:
                ti = tt
                P.op("dve", lambda e: e.tensor_tensor(
                    out=x2[ti][:, nh * 512:(nh + 1) * 512], in0=pdn[pi][:], in1=x1[tt][:, nh * 512:(nh + 1) * 512],
                    op=ALU.add), reads=[b_pdn[pi], b_x1[tt]], writes=[b_x2[ti]])
                if nh == 1:
                    P.op("act", lambda e: e.activation(out=junk[:], in_=x2[ti][:], func=AF.Square, accum_out=ss[ti][:]),
                         reads=[b_x2[ti]], writes=[b_junk, b_ss[ti]])
                    P.op("dve", lambda e: e.tensor_scalar(out=ssv[ti][:], in0=ss[ti][:], scalar1=1.0 / D, scalar2=EPS,
                                                          op0=ALU.mult, op1=ALU.add), reads=[b_ss[ti]], writes=[b_ssv[ti]])
                    P.op("act", lambda e: e.activation(out=ssl[ti][:], in_=ssv[ti][:], func=AF.Ln),
                         reads=[b_ssv[ti]], writes=[b_ssl[ti]])
                    P.op("act", lambda e: e.activation(out=rstd[ti][:], in_=ssl[ti][:], func=AF.Exp, scale=-0.5),
                         reads=[b_ssl[ti]], writes=[b_rstd[ti]])
                    P.op("dve", lambda e: e.scalar_tensor_tensor(
                        out=x2[ti][:], in0=x2[ti][:], scalar=rstd[ti][:, 0:1], in1=gf[:], op0=ALU.mult, op1=ALU.mult),
                        reads=[b_x2[ti], b_rstd[ti], b_c], writes=[b_x2[ti]])
                    P.dma("sp", T["out"][s, t0 + tt * 128:t0 + (tt + 1) * 128, :], x2[ti][:], f"outo{ti}",
                          reads=[b_x2[ti]])

        def load_x1(n):
            s, t0 = n // 8, (n % 8) * 256
            for tt in range(2):
                P.dma("sp", x1[tt][:], T["out"][s, t0 + tt * 128:t0 + (tt + 1) * 128, :], f"l_x1b{tt}", writes=[b_x1[tt]])

        load(0)
        for n in range(ngroups + 1):
            if n + 1 < ngroups:
                load(n + 1)
            if n >= 1:
                load_x1(n - 1)
            for p in range(NP):
                if n < ngroups:
                    up_pair(n, p)
                if n >= 1:
                    for m in range(4 * p, 4 * p + 4):
                        down_mm(n - 1, m)
        P.barrier()


SCRATCH = {
    "qk": ([16, 64, S], BF16), "v": ([S, 512], BF16), "xs": ([S, D_SSM], BF16), "btok": ([S, 512], BF16),
    "bT": ([512, S], BF16), "cT": ([512, S], BF16), "sz": ([S, D_SSM], BF16), "dt": ([128, 16, NH_SSM], F32),
    "mixed": ([S, 2048], BF16), "h2T": ([D, S], BF16),
}
CONSTS = {
    "ident": ([128, 128], BF16), "cmask": ([128, 2, 256], BF16), "ownmask": ([128, 16, 8], F32), "kbias": ([128, 8, 2], F32),
    "pastmask": ([128, 16, 8], F32), "dmbase": ([128, 16, 8], F32), "karows": ([9, S], BF16), "qrow": ([8, S], BF16),
    "U": ([128, 128], F32), "Ls": ([128, 128], F32), "ones": ([128, 128], F32), "identf": ([128, 128], F32),
}
PARAMS = {
    "ln1_g": [128, D], "ssm_cw": [128, 20, 4], "ssm_cb": [128, 20], "dt_bias": [128, NH_SSM],
    "attn_g": [128, D_ATT], "a_log": [128, NH_SSM], "d_skip": [128, NH_SSM], "ssm_g": [128, D_SSM],
    "ln2_g": [128, D], "lnf_g": [128, D], "ffn_cw": [128, 2, 22, 3], "ffn_cb": [128, 2, 22], "mix_g": [128, 16],
}


def build_program(nseq=4, phases=(1,), debug=False):
    nc = bass.Bass("TRN2", target_bir_lowering=False)
    T = {}
    T["x"] = nc.dram_tensor("x", [nseq, S, D], F32, kind="ExternalInput").ap()
    T["w_in"] = nc.dram_tensor("w_in", [D, D_IN], F32, kind="ExternalInput").ap()
    T["w_out"] = nc.dram_tensor("w_out", [2048, D], F32, kind="ExternalInput").ap()
    T["w_up"] = nc.dram_tensor("w_up", [D, 2 * D_FF], F32, kind="ExternalInput").ap()
    T["w_down"] = nc.dram_tensor("w_down", [D_FF, D], F32, kind="ExternalInput").ap()
    for k, shp in PARAMS.items():
        T[k] = nc.dram_tensor(k, shp, F32, kind="ExternalInput").ap()
    for k, (shp, dt) in CONSTS.items():
        T[k] = nc.dram_tensor(k, shp, dt, kind="ExternalInput").ap()
    for k, (shp, dt) in SCRATCH.items():
        T[k] = nc.dram_tensor(k, [nseq] + shp, dt, kind="ExternalOutput" if debug else "Internal").ap()
    T["out"] = nc.dram_tensor("out", [nseq, S, D], F32, kind="ExternalOutput").ap()
    with contextlib.ExitStack() as es:
        P = Prog(nc, es)
        if 1 in phases:
            phase1(P, T, nseq)
        with contextlib.ExitStack() as es_dn:
            w_dn_bf = Alloc(nc, es_dn).sb([128, 22, D], BF16, "w_dn_bf")
            b_wdn = P.buf("w_dn")
            with contextlib.ExitStack() as es_out:
                w_out_bf = Alloc(nc, es_out).sb([128, 16, D], BF16, "w_out_bf")
                b_wout = P.buf("w_out")
                if 5 in phases:
                    load_weight(P, T, "w_down", w_dn_bf, 22, b_wdn, "w2d")
                if 2 in phases:
                    phase2a(P, T, nseq, w_out_bf if 4 in phases else None, b_wout)
                if 3 in phases:
                    phase2b(P, T, nseq)
                if 4 in phases:
                    phase3a(P, T, nseq, w_out_bf, b_wout)
            if 5 in phases:
                phase3b(P, T, nseq, w_dn_bf, b_wdn)
        P.finish()
    return nc


def host_consts():
    bf = ml_dtypes.bfloat16
    c = {"ident": np.eye(128, dtype=np.float32).astype(bf)}
    sl = np.array(SLOPES, dtype=np.float64)
    p = np.arange(128)
    krel = (np.arange(2)[None, :] * 128 + p[:, None]).astype(np.float64)
    q = np.arange(256, dtype=np.float64)
    d = q[None, None, :] - krel[:, :, None]
    c["cmask"] = np.where(d >= 0, 0.0, -BIG).astype(np.float32).astype(bf)
    c["kbias"] = (sl[None, :, None] * krel[:, None, :]).astype(np.float32)
    t = np.arange(16)
    j = np.arange(8)
    past = (j[None, :] < (t[:, None] // 2))
    c["pastmask"] = np.ascontiguousarray(np.broadcast_to(np.where(past, 0.0, -1e30)[None], (128, 16, 8))).astype(np.float32)
    own = (j[None, :] == (t[:, None] // 2)).astype(np.float32)
    c["ownmask"] = np.ascontiguousarray(np.broadcast_to(own[None], (128, 16, 8))).astype(np.float32)
    dmb = np.where(past, (t[:, None] // 2 - j[None, :]), 0).astype(np.float32)
    c["dmbase"] = np.ascontiguousarray(np.broadcast_to(dmb[None], (128, 16, 8))).astype(np.float32)
    kr = np.zeros((9, S), dtype=np.float32)
    for jj in range(8):
        kr[jj, jj * 256:(jj + 1) * 256] = 1.0
    kr[8, :] = 1.0
    c["karows"] = kr.astype(bf)
    qrel = (np.arange(S) % 256).astype(np.float64)
    c["qrow"] = (-8.0 * sl[:, None] * qrel[None, :]).astype(np.float32).astype(bf)
    li = np.arange(128)
    c["U"] = (li[:, None] <= li[None, :]).astype(np.float32)
    c["Ls"] = (li[:, None] > li[None, :]).astype(np.float32)
    c["ones"] = np.ones((128, 128), dtype=np.float32)
    c["identf"] = np.eye(128, dtype=np.float32)
    return c


def host_params(inp):
    f = np.float32
    p = {}
    p["ln1_g"] = np.ascontiguousarray(np.broadcast_to(inp["ln1_g"][0][None, :], (128, D))).astype(f)
    cw = inp["ssm_conv_w"][0]
    p["ssm_cw"] = np.ascontiguousarray(cw.reshape(4, 20, 128).transpose(2, 1, 0)).astype(f)
    p["ssm_cb"] = np.ascontiguousarray(inp["ssm_conv_b"][0].reshape(20, 128).T).astype(f)
    p["dt_bias"] = np.ascontiguousarray(np.broadcast_to(inp["dt_bias"][0][None, :], (128, NH_SSM))).astype(f)
    p["attn_g"] = np.ascontiguousarray(np.broadcast_to(inp["attn_norm_g"][0][None, :], (128, D_ATT))).astype(f)
    p["a_log"] = np.ascontiguousarray(np.broadcast_to(inp["a_log"][0][None, :], (128, NH_SSM))).astype(f)
    p["d_skip"] = np.ascontiguousarray(np.broadcast_to(inp["d_skip"][0][None, :], (128, NH_SSM))).astype(f)
    p["ssm_g"] = np.ascontiguousarray(np.broadcast_to(inp["ssm_norm_g"][0][None, :], (128, D_SSM))).astype(f)
    p["ln2_g"] = np.ascontiguousarray(np.broadcast_to(inp["ln2_g"][0][None, :], (128, D))).astype(f)
    p["lnf_g"] = np.ascontiguousarray(np.broadcast_to(inp["lnf_g"][None, :], (128, D))).astype(f)
    fw = inp["ffn_conv_w"][0]
    p["ffn_cw"] = np.ascontiguousarray(fw.reshape(3, 2, 22, 128).transpose(3, 1, 2, 0)).astype(f)
    p["ffn_cb"] = np.ascontiguousarray(inp["ffn_conv_b"][0].reshape(2, 22, 128).transpose(2, 0, 1)).astype(f)
    mg = np.concatenate([inp["attn_norm_g"][0], inp["ssm_norm_g"][0]])
    p["mix_g"] = np.ascontiguousarray(mg.reshape(16, 128).T).astype(f)
    return p


def kernel(**inp):
    inp = {k: np.asarray(v) for k, v in inp.items()}
    nseq = 32 // NCORES
    nc = build_program(nseq=nseq, phases=(1, 2, 3, 4, 5))
    base = {"w_in": np.ascontiguousarray(inp["w_in"][0]), "w_out": np.ascontiguousarray(inp["w_out"][0]),
            "w_up": np.ascontiguousarray(inp["w_up"][0]), "w_down": np.ascontiguousarray(inp["w_down"][0])}
    base.update(host_consts())
    base.update(host_params(inp))
    in_maps = []
    for c in range(NCORES):
        m = dict(base)
        m["x"] = np.ascontiguousarray(inp["x"][c * nseq:(c + 1) * nseq])
        in_maps.append(m)
    res = run_bass_kernel_spmd(nc, in_maps, core_ids=list(range(NCORES)))
    return np.concatenate([r["out"] for r in res.results], axis=0)
```
